# Optimizing a Trainium2 kernel written in Bass

```python
import jax
import jax.numpy as jnp
from jax import lax
import numpy as np

D_MODEL = 1024
BATCH = 32
SEQ = 2048
DEPTH = 1

CHUNK = 64
HEAD_DIM = 64
RMS_EPS = 1e-6
NEG_INF = -1e30

SWA_Q_HEADS = 8
SWA_KV_HEADS = 2
SWA_GROUP = SWA_Q_HEADS // SWA_KV_HEADS
SWA_WINDOW = 128
SWA_WINDOW_CHUNKS = SWA_WINDOW // CHUNK
SWA_KEYS = (SWA_WINDOW_CHUNKS + 1) * CHUNK
SWA_WIDTH = SWA_Q_HEADS * HEAD_DIM
SWA_KV_WIDTH = SWA_KV_HEADS * HEAD_DIM

SB_HEADS = 8
SB_WIDTH = SB_HEADS * HEAD_DIM
SB_BLOCK = 128

N_BRANCHES = 2
SPLIT_POINTS = (
    SWA_WIDTH,
    SWA_WIDTH + SWA_KV_WIDTH,
    SWA_WIDTH + 2 * SWA_KV_WIDTH,
    SWA_WIDTH + 2 * SWA_KV_WIDTH + SB_WIDTH,
    SWA_WIDTH + 2 * SWA_KV_WIDTH + 2 * SB_WIDTH,
    SWA_WIDTH + 2 * SWA_KV_WIDTH + 3 * SB_WIDTH,
)
IN_WIDTH = SWA_WIDTH + 2 * SWA_KV_WIDTH + 3 * SB_WIDTH + N_BRANCHES * D_MODEL

PEER_HEADS = 8
PEER_N_KEYS = 128
PEER_N_EXPERTS = PEER_N_KEYS * PEER_N_KEYS
PEER_QUERY_DIM = 256
PEER_HALF = PEER_QUERY_DIM // 2
PEER_TOPK = 16
PEER_BLOCK = 128

kernel_name = 'hybrid_swa_stickbreaking_peer_block'


def rms_norm(x, gain):
    xf = x.astype(jnp.float32)
    y = xf * lax.rsqrt(jnp.mean(xf * xf, axis=-1, keepdims=True) + RMS_EPS)
    return (y * gain.astype(jnp.float32)).astype(x.dtype)


def alibi_slopes(n_heads):
    return jnp.exp2(-8.0 * jnp.arange(1, n_heads + 1, dtype=jnp.float32) / n_heads)


def sliding_window_sink_attention(q, k, v, q_gain, k_gain, sinks):
    b, s = q.shape[0], q.shape[1]
    nc = s // CHUNK
    pad = SWA_WINDOW_CHUNKS * CHUNK
    q = rms_norm(q, q_gain)
    k = rms_norm(k, k_gain)
    kp = jnp.pad(k, ((0, 0), (pad, 0), (0, 0), (0, 0))).reshape(
        b, nc + SWA_WINDOW_CHUNKS, CHUNK, SWA_KV_HEADS, HEAD_DIM)
    vp = jnp.pad(v, ((0, 0), (pad, 0), (0, 0), (0, 0))).reshape(
        b, nc + SWA_WINDOW_CHUNKS, CHUNK, SWA_KV_HEADS, HEAD_DIM)
    kw = jnp.concatenate([kp[:, i:i + nc] for i in range(SWA_WINDOW_CHUNKS + 1)], axis=2)
    vw = jnp.concatenate([vp[:, i:i + nc] for i in range(SWA_WINDOW_CHUNKS + 1)], axis=2)
    qc = q.reshape(b, nc, CHUNK, SWA_KV_HEADS, SWA_GROUP, HEAD_DIM)
    logits = jnp.einsum('bcqhgd,bckhd->bhgcqk', qc, kw).astype(jnp.float32) * (HEAD_DIM ** -0.5)
    dist = jnp.abs(jnp.arange(CHUNK)[:, None] + pad - jnp.arange(SWA_KEYS)[None, :]).astype(jnp.float32)
    slopes = alibi_slopes(SWA_Q_HEADS).reshape(SWA_KV_HEADS, SWA_GROUP)
    logits = logits - slopes[None, :, :, None, None, None] * dist
    key_pos = jnp.arange(nc)[:, None] * CHUNK - pad + jnp.arange(SWA_KEYS)[None, :]
    logits = jnp.where((key_pos >= 0)[:, None, :], logits, NEG_INF)
    sink = jnp.broadcast_to(
        sinks.astype(jnp.float32).reshape(1, SWA_KV_HEADS, SWA_GROUP, 1, 1, 1),
        logits.shape[:-1] + (1,))
    probs = jax.nn.softmax(jnp.concatenate([logits, sink], axis=-1), axis=-1)[..., :-1]
    out = jnp.einsum('bhgcqk,bckhd->bcqhgd', probs.astype(v.dtype), vw)
    return out.reshape(b, s, SWA_WIDTH)


def stick_breaking_attention(q, k, v):
    b, s, h, dh = q.shape
    nb = s // SB_BLOCK
    qb = q.reshape(b, nb, SB_BLOCK, h, dh).transpose(1, 0, 2, 3, 4)
    key_pos = jnp.arange(s)

    def block(args):
        q_blk, blk_idx = args
        z = jnp.einsum('bqhd,bkhd->bhqk', q_blk, k).astype(jnp.float32) * (dh ** -0.5)
        q_pos = blk_idx * SB_BLOCK + jnp.arange(SB_BLOCK)
        before = key_pos[None, :] < q_pos[:, None]
        log_beta = jax.nn.log_sigmoid(z)
        log_keep = jnp.where(before, jax.nn.log_sigmoid(-z), 0.0)
        suffix = lax.cumsum(log_keep, axis=3, reverse=True) - log_keep
        weights = jnp.where(before, jnp.exp(log_beta + suffix), 0.0)
        return jnp.einsum('bhqk,bkhd->bqhd', weights.astype(v.dtype), v)

    out = lax.map(block, (qb, jnp.arange(nb)))
    return out.transpose(1, 0, 2, 3, 4).reshape(b, s, h * dh)


def peer_ffn(x, w_q, sub_keys, u, v):
    b, s, d = x.shape
    tokens = x.reshape(-1, PEER_BLOCK, d)

    def block(xb):
        q = (xb @ w_q).reshape(PEER_BLOCK, PEER_HEADS, 2, PEER_HALF)
        scores = jnp.einsum('thpd,hpkd->thpk', q, sub_keys).astype(jnp.float32)
        top_s, top_i = lax.top_k(scores, PEER_TOPK)
        cand_s = (top_s[:, :, 0, :, None] + top_s[:, :, 1, None, :]).reshape(
            PEER_BLOCK, PEER_HEADS, PEER_TOPK * PEER_TOPK)
        cand_i = (top_i[:, :, 0, :, None] * PEER_N_KEYS + top_i[:, :, 1, None, :]).reshape(
            PEER_BLOCK, PEER_HEADS, PEER_TOPK * PEER_TOPK)
        best_s, best_pos = lax.top_k(cand_s, PEER_TOPK)
        expert = jnp.take_along_axis(cand_i, best_pos, axis=-1)
        gate = jax.nn.softmax(best_s, axis=-1)
        act = jax.nn.gelu(jnp.einsum('thkd,td->thk', u[expert], xb).astype(jnp.float32),
                          approximate=False)
        return jnp.einsum('thk,thkd->td', (gate * act).astype(v.dtype), v[expert])

    return lax.map(block, tokens).reshape(b, s, d)


def setup_inputs(seed: int = 0) -> dict:
    key = jax.random.key(seed)
    ks = jax.random.split(key, 16)

    def normal(k, shape, scale):
        return jax.random.normal(k, shape, jnp.float32) * scale

    return {
        'x': normal(ks[0], (BATCH, SEQ, D_MODEL), 1.0),
        'mix_norm_gain': 1.0 + normal(ks[1], (DEPTH, D_MODEL), 0.02),
        'w_in': normal(ks[2], (DEPTH, D_MODEL, IN_WIDTH), D_MODEL ** -0.5),
        'gate_bias': normal(ks[3], (DEPTH, N_BRANCHES * D_MODEL), 0.1),
        'swa_q_gain': 1.0 + normal(ks[4], (DEPTH, HEAD_DIM), 0.02),
        'swa_k_gain': 1.0 + normal(ks[5], (DEPTH, HEAD_DIM), 0.02),
        'swa_sinks': normal(ks[6], (DEPTH, SWA_Q_HEADS), 0.5),
        'w_up_swa': normal(ks[7], (DEPTH, SWA_WIDTH, D_MODEL), SWA_WIDTH ** -0.5),
        'w_up_sb': normal(ks[8], (DEPTH, SB_WIDTH, D_MODEL), SB_WIDTH ** -0.5),
        'w_out': normal(ks[9], (DEPTH, D_MODEL, D_MODEL), D_MODEL ** -0.5),
        'ffn_norm_gain': 1.0 + normal(ks[10], (DEPTH, D_MODEL), 0.02),
        'peer_w_q': normal(ks[11], (DEPTH, D_MODEL, PEER_HEADS * PEER_QUERY_DIM), D_MODEL ** -0.5),
        'peer_sub_keys': normal(ks[12], (DEPTH, PEER_HEADS, 2, PEER_N_KEYS, PEER_HALF), PEER_HALF ** -0.5),
        'peer_u': normal(ks[13], (DEPTH, PEER_N_EXPERTS, D_MODEL), D_MODEL ** -0.5),
        'peer_v': normal(ks[14], (DEPTH, PEER_N_EXPERTS, D_MODEL), PEER_HEADS ** -0.5),
    }


def reference(x, mix_norm_gain, w_in, gate_bias, swa_q_gain, swa_k_gain, swa_sinks,
              w_up_swa, w_up_sb, w_out, ffn_norm_gain, peer_w_q, peer_sub_keys, peer_u, peer_v):
    b, s, d = x.shape
    for layer in range(DEPTH):
        h = rms_norm(x, mix_norm_gain[layer])
        proj = h @ w_in[layer]
        q_a, k_a, v_a, q_b, k_b, v_b, gate_logits = jnp.split(proj, SPLIT_POINTS, axis=-1)
        y_a = sliding_window_sink_attention(
            q_a.reshape(b, s, SWA_Q_HEADS, HEAD_DIM),
            k_a.reshape(b, s, SWA_KV_HEADS, HEAD_DIM),
            v_a.reshape(b, s, SWA_KV_HEADS, HEAD_DIM),
            swa_q_gain[layer], swa_k_gain[layer], swa_sinks[layer])
        y_b = stick_breaking_attention(
            q_b.reshape(b, s, SB_HEADS, HEAD_DIM),
            k_b.reshape(b, s, SB_HEADS, HEAD_DIM),
            v_b.reshape(b, s, SB_HEADS, HEAD_DIM))
        gates = jax.nn.sigmoid(gate_logits + gate_bias[layer]).reshape(b, s, N_BRANCHES, d)
        merged = gates[:, :, 0] * (y_a @ w_up_swa[layer]) + gates[:, :, 1] * (y_b @ w_up_sb[layer])
        x = x + merged @ w_out[layer]
        h = rms_norm(x, ffn_norm_gain[layer])
        x = x + peer_ffn(h, peer_w_q[layer], peer_sub_keys[layer], peer_u[layer], peer_v[layer])
    return x
```

```python
from contextlib import ExitStack
import numpy as np
import concourse.bass as bass
import concourse.mybir as mybir
from concourse.bass_utils import run_bass_kernel_spmd


F32 = mybir.dt.float32
BF16 = mybir.dt.bfloat16
I32 = mybir.dt.int32
U32 = mybir.dt.uint32
AF = mybir.ActivationFunctionType
ALU = mybir.AluOpType
AX = mybir.AxisListType

SAME_ENGINE_SYNC = True
N_DMA_SEMS = {"sp": 12, "pool": 8, "act": 4}


class Res:
    __slots__ = ("name", "w", "r", "open")

    def __init__(self, name):
        self.name = name
        self.w = None
        self.r = {}
        self.open = False


class Ring:
    def __init__(self, items):
        self.items = list(items)
        self.i = 0

    def get(self):
        it = self.items[self.i % len(self.items)]
        self.i += 1
        res = it[0] if isinstance(it, tuple) else it
        assert not res.open, f"ring slot {res.name} still open"
        res.open = True
        return it


def release(res):
    res.open = False


class Prog:
    ENG = ("pe", "dve", "act", "pool", "sp")

    def __init__(self, nc):
        self.nc = nc
        self.ops = {e: [] for e in self.ENG}
        self.cnt = {e: 0 for e in self.ENG}
        self.seen = {e: {} for e in self.ENG}
        self.sems = {}
        for e in ("pe", "dve", "act", "pool"):
            self.sems[e] = nc.alloc_semaphore(f"s_{e}")
        self.dma_pool = {}
        for q, n in N_DMA_SEMS.items():
            self.dma_pool[q] = {"i": 0, "sems": []}
            for k in range(n):
                key = ("dma", q, k)
                self.sems[key] = nc.alloc_semaphore(f"s_dma_{q}_{k}")
                self.dma_pool[q]["sems"].append([key, 0])
        self.n_wait = 0

    def _deps(self, eng, reads, writes):
        deps = {}

        def add(tok):
            if tok is None:
                return
            k, v = tok
            if deps.get(k, 0) < v:
                deps[k] = v
        for r in reads:
            add(r.w)
        for w in writes:
            add(w.w)
            for k, v in w.r.items():
                add((k, v))
        out = []
        for k, v in deps.items():
            if k == eng and (eng == "pe" or not SAME_ENGINE_SYNC):
                continue
            if self.seen[eng].get(k, 0) >= v:
                continue
            self.seen[eng][k] = v
            out.append((k, v))
        return out

    def _update(self, tok, reads, writes):
        k, v = tok
        for r in reads:
            if r.r.get(k, 0) < v:
                r.r[k] = v
        for w in writes:
            w.w = tok
            w.r = {}

    def op(self, eng, fn, reads=(), writes=()):
        waits = self._deps(eng, reads, writes)
        self.cnt[eng] += 1
        tok = (eng, self.cnt[eng])
        self.ops[eng].append((waits, fn, (eng, 1)))
        self._update(tok, reads, writes)
        self.n_wait += len(waits)
        return tok

    def dma(self, q, fn, reads=(), writes=()):
        pool = self.dma_pool[q]
        slot = pool["sems"][pool["i"] % len(pool["sems"])]
        pool["i"] += 1
        key, cnt = slot
        waits = self._deps(q, reads, writes)
        if cnt > 0 and self.seen[q].get(key, 0) < 16 * cnt:
            self.seen[q][key] = 16 * cnt
            waits.append((key, 16 * cnt))
        slot[1] = cnt + 1
        tok = (key, 16 * (cnt + 1))
        self.ops[q].append((waits, fn, (key, 16)))
        self._update(tok, reads, writes)
        return tok

    def wait_tokens(self, eng, toks):
        waits = []
        for k, v in toks:
            if self.seen[eng].get(k, 0) >= v:
                continue
            self.seen[eng][k] = v
            waits.append((k, v))
        if waits:
            self.ops[eng].append((waits, None, None))

    def all_tokens(self):
        toks = []
        for e in ("pe", "dve", "act", "pool"):
            if self.cnt[e] > 0:
                toks.append((e, self.cnt[e]))
        for q, pool in self.dma_pool.items():
            for key, cnt in pool["sems"]:
                if cnt > 0:
                    toks.append((key, 16 * cnt))
        return toks

    def barrier(self):
        toks = self.all_tokens()
        for e in self.ENG:
            self.wait_tokens(e, [t for t in toks if not (e == 'pe' and t[0] == 'pe')])

    def replay(self):
        nc = self.nc
        P = self
        with nc.Block() as block:
            def run(e, name):
                for waits, fn, inc in P.ops[name]:
                    for k, v in waits:
                        e.wait_ge(P.sems[k], v)
                    if fn is not None:
                        ins = fn(e)
                        ins.then_inc(P.sems[inc[0]], inc[1])

            @block.sync
            def _(e):
                run(e, "sp")

            @block.tensor
            def _(e):
                run(e, "pe")

            @block.vector
            def _(e):
                run(e, "dve")

            @block.scalar
            def _(e):
                run(e, "act")

            @block.gpsimd
            def _(e):
                run(e, "pool")
        for e in self.ENG:
            self.ops[e] = []


STACK = [None]


def sb(nc, name, shape, dt):
    return STACK[0].enter_context(nc.sbuf_tensor(name, list(shape), dt))


def ps(nc, name, shape, dt):
    return STACK[0].enter_context(nc.psum_tensor(name, list(shape), dt))


def consts():
    j = np.arange(128)[:, None]; t = np.arange(128)[None, :]
    ident = np.eye(128, dtype=np.float32)
    tri = (j >= t).astype(np.float32)
    ones = np.ones((128, 128), np.float32)
    maskL = (j < t).astype(np.float32)
    maskb = np.where(j < t, 0.0, 30000.0).astype(np.float32)
    cst = np.stack([ident, tri, ones, maskL, maskb]).astype(np.float32)
    slopes = 2.0 ** (-8.0 * np.arange(1, 9) / 8)
    k = np.arange(128)[:, None]; q = np.arange(128)[None, :]
    swab = np.zeros((128, 8, 2, 128), np.float32)
    for kt in range(2):
        spos = (kt - 1) * 128 + k
        tpos = q
        ck = spos // 64
        cq = tpos // 64
        vis = (ck <= cq) & (ck >= cq - 2)
        dist = np.abs(tpos - spos).astype(np.float64)
        for h in range(8):
            b = np.where(vis, -slopes[h] * dist * 8.0, -240000.0)
            swab[:, h, kt, :] = b
    return cst, np.ascontiguousarray(swab.reshape(128, 2048))
def col(v, n):
    return np.ascontiguousarray(np.asarray(v, np.float32).reshape(n, 128).T)
def bc(v):
    v = np.asarray(v, np.float32).reshape(1, -1)
    return np.ascontiguousarray(np.broadcast_to(v, (128, v.shape[1])))


D = 1024
S = 2048
NT = 16
BATCH = 2
SKIP_SB = set()
SKIP_SWA = set()


def phase_a1(P, nc, dr, nseq):
    R = lambda n: Res(n)
    win = sb(nc, "a_win", [128, 8, 2304], BF16)
    stg = [sb(nc, f"a_stg{i}", [128, 1152], F32) for i in range(2)]
    g1col = sb(nc, "a_g1col", [128, 8], F32)
    gainqk = sb(nc, "a_gainqk", [128, 640], F32)
    esink = sb(nc, "a_esink", [128, 8], F32)
    epsc = sb(nc, "a_eps", [128, 1], F32)
    cstf = sb(nc, "a_cstf", [128, 5, 128], F32)
    cstb = sb(nc, "a_cstb", [128, 5, 128], BF16)
    identb, trib, onesb, maskLb, maskbb = (cstb[:, k, :] for k in range(5))
    swabf = sb(nc, "a_swabf", [128, 2048], F32)
    swab = sb(nc, "a_swab", [128, 8, 2, 128], BF16)
    kaT = sb(nc, "a_kaT", [64, 2, S], BF16)
    kbT = sb(nc, "a_kbT", [128, 4, S], BF16)
    vb = sb(nc, "a_vb", [128, NT, 512], BF16)
    vaug = sb(nc, "a_vaug", [128, NT, 2, 65], BF16)
    xa = [sb(nc, f"a_xa{i}", [128, D], F32) for i in range(2)]
    xs = sb(nc, "a_xs", [128, D], BF16)
    junk = sb(nc, "a_junk", [128, D], BF16)
    hT = sb(nc, "a_hT", [128, 8, 128], BF16)
    sq = sb(nc, "a_sq", [128, 640], F32)
    qtmp = sb(nc, "a_qtmp", [128, 640], F32)
    qn = sb(nc, "a_qn", [128, 640], BF16)
    ssq = sb(nc, "a_ssq", [128, 10], F32)
    rq = sb(nc, "a_rq", [128, 10], F32)
    ss = sb(nc, "a_ss", [128, 1], F32)
    lnv = sb(nc, "a_lnv", [128, 1], F32)
    rstd = sb(nc, "a_rstd", [128, 1], F32)
    qaT = sb(nc, "a_qaT", [64, 8, 128], BF16)
    qbT = sb(nc, "a_qbT", [128, 4, 128], BF16)
    qbTn = sb(nc, "a_qbTn", [128, 4, 128], BF16)
    PT = [sb(nc, f"a_PT{i}", [128, 2, 2, 128], BF16) for i in range(2)]
    dn = sb(nc, "a_dn", [128, 4, 1], F32)
    ya = sb(nc, "a_ya", [128, 512], BF16)
    yaT = [sb(nc, f"a_yaT{i}", [128, 4, 128], BF16) for i in range(2)]
    ybT = [sb(nc, f"a_ybT{i}", [64, 8, 128], BF16) for i in range(2)]
    Eb = [sb(nc, f"a_E{i}", [128, 512], F32) for i in range(2)]
    Lb = [sb(nc, f"a_L{i}", [128, 512], BF16) for i in range(2)]
    Wb = [sb(nc, f"a_W{i}", [128, 512], BF16) for i in range(2)]
    Sb = sb(nc, "a_S", [128, 17, 128], BF16)
    TR = ps(nc, "a_TR", [128, 1024], BF16)
    PSR = [ps(nc, f"a_PS{i}", [128, 512], F32) for i in range(4)]
    OUT = [ps(nc, f"a_OUT{i}", [128, 512], F32) for i in range(2)]

    r_win, r_const = R("win"), R("const")
    r_stg = [R("stg0"), R("stg1")]
    r_kaT, r_kbT, r_vb, r_vaug = R("kaT"), R("kbT"), R("vb"), R("vaug")
    r_xa = [R("xa0"), R("xa1")]
    r_xs, r_junk, r_hT, r_sq, r_qtmp, r_qn, r_norm, r_qnorm = (R(n) for n in
                                                               ("xs", "junk", "hT", "sq", "qtmp", "qn", "norm", "qnorm"))
    r_qaT, r_qbT = R("qaT"), R("qbT")
    r_PT = [R("PT0"), R("PT1")]
    r_dn, r_ya = R("dn"), R("ya")
    r_yaT = [R("yaT0"), R("yaT1")]
    r_ybT = [R("ybT0"), R("ybT1")]
    r_S = [R(f"S{k}") for k in range(17)]
    r_TR = R("TR")
    r_OUT = [R("OUT0"), R("OUT1")]
    psr = Ring([(R(f"PS{i}"), PSR[i]) for i in range(4)])
    ering = Ring([(R(f"E{i}"), Eb[i]) for i in range(2)])
    lring = Ring([(R(f"L{i}"), Lb[i]) for i in range(2)])
    wring = Ring([(R(f"W{i}"), Wb[i]) for i in range(2)])

    P.dma("sp", lambda e: e.dma_start(out=g1col[:], in_=dr["g1col"]), writes=[r_const])
    P.dma("sp", lambda e: e.dma_start(out=gainqk[:], in_=dr["gainqk"]), writes=[r_const])
    P.dma("sp", lambda e: e.dma_start(out=esink[:], in_=dr["sinks_bc"]), writes=[r_const])
    P.dma("sp", lambda e: e.dma_start(out=cstf[:], in_=dr["cst"][0:5].rearrange("k p n -> p k n")), writes=[r_const])
    P.dma("sp", lambda e: e.dma_start(out=swabf[:], in_=dr["swab"]), writes=[r_const])
    P.op("act", lambda e: e.activation(out=esink[:], in_=esink[:], func=AF.Exp), reads=[r_const], writes=[r_const])
    P.op("dve", lambda e: e.tensor_copy(out=cstb[:], in_=cstf[:]), reads=[r_const], writes=[r_const])
    P.op("dve", lambda e: e.tensor_copy(out=swab[:].rearrange("p a b c -> p (a b c)"), in_=swabf[:]),
         reads=[r_const], writes=[r_const])
    P.op("dve", lambda e: e.memset(epsc[:], 1e-6), writes=[r_const])
    P.op("dve", lambda e: e.memset(vaug[:], 1.0), writes=[r_vaug])
    k = 0
    for kc in range(8):
        for hf in range(2):
            s = k % 2
            k += 1
            P.dma("sp", lambda e, kc=kc, hf=hf, s=s: e.dma_start(
                out=stg[s][:], in_=dr["w_in"][kc * 128:(kc + 1) * 128, hf * 1152:(hf + 1) * 1152]), writes=[r_stg[s]])
            P.op("dve", lambda e, kc=kc, hf=hf, s=s: e.tensor_scalar(
                out=win[:, kc, hf * 1152:(hf + 1) * 1152], in0=stg[s][:], scalar1=g1col[:, kc:kc + 1], scalar2=None,
                op0=ALU.mult), reads=[r_stg[s], r_const], writes=[r_win])

    def front(sq_i, tt):
        gt = sq_i * NT + tt
        b = gt % 2
        xap = dr["x"][gt * 128:(gt + 1) * 128, :]
        P.dma("sp", lambda e: e.dma_start(out=xa[b][:], in_=xap), writes=[r_xa[b]])
        P.op("act", lambda e: e.activation(out=junk[:], in_=xa[b][:], func=AF.Square, accum_out=ss[:]),
             reads=[r_xa[b]], writes=[r_junk, r_norm])
        P.op("act", lambda e: e.activation(out=lnv[:], in_=ss[:], func=AF.Ln, scale=1.0 / D, bias=epsc[:]),
             reads=[r_norm, r_const], writes=[r_norm])
        P.op("act", lambda e: e.activation(out=rstd[:], in_=lnv[:], func=AF.Exp, scale=-0.5), reads=[r_norm], writes=[r_norm])
        P.op("dve", lambda e: e.tensor_scalar(out=xs[:], in0=xa[b][:], scalar1=rstd[:], scalar2=None, op0=ALU.mult),
             reads=[r_xa[b], r_norm], writes=[r_xs])
        for kc in range(8):
            P.op("pe", lambda e, kc=kc: e.transpose(out=TR[:, kc * 128:(kc + 1) * 128], in_=xs[:, kc * 128:(kc + 1) * 128],
                                                    identity=identb), reads=[r_xs, r_const], writes=[r_TR])
        P.op("act", lambda e: e.activation(out=hT[:].rearrange("p a b -> p (a b)"), in_=TR[:], func=AF.Copy),
             reads=[r_TR], writes=[r_hT])
        rA1, A1 = psr.get()
        rA2, A2 = psr.get()
        rA3, A3 = psr.get()
        for (rr, bank, c0, n) in ((rA1, A1, 0, 512), (rA2, A2, 512, 256), (rA3, A3, 1792, 512)):
            for kc in range(8):
                P.op("pe", lambda e, bank=bank, c0=c0, n=n, kc=kc: e.matmul(
                    bank[:, 0:n], lhsT=hT[:, kc, :], rhs=win[:, kc, c0:c0 + n], start=(kc == 0), stop=(kc == 7)),
                    reads=[r_hT, r_win], writes=[rr])
        P.op("act", lambda e: e.activation(out=vb[:, tt, :], in_=A3[:, 0:512], func=AF.Copy), reads=[rA3], writes=[r_vb])
        release(rA3)
        P.op("act", lambda e: e.activation(out=vaug[:, tt, :, 0:64], in_=A2[:, 128:256].rearrange("p (g d) -> p g d", d=64),
                                           func=AF.Copy), reads=[rA2], writes=[r_vaug])
        P.op("act", lambda e: e.activation(out=sq[:, 0:512], in_=A1[:, 0:512], func=AF.Square), reads=[rA1], writes=[r_sq])
        P.op("act", lambda e: e.activation(out=sq[:, 512:640], in_=A2[:, 0:128], func=AF.Square), reads=[rA2], writes=[r_sq])
        P.op("dve", lambda e: e.tensor_reduce(out=ssq[:], in_=sq[:].rearrange("p (h d) -> p h d", d=64), axis=AX.X, op=ALU.add),
             reads=[r_sq], writes=[r_qnorm])
        P.op("act", lambda e: e.activation(out=rq[:], in_=ssq[:], func=AF.Ln, scale=1.0 / 64, bias=epsc[:]),
             reads=[r_qnorm, r_const], writes=[r_qnorm])
        P.op("act", lambda e: e.activation(out=rq[:], in_=rq[:], func=AF.Exp, scale=-0.5), reads=[r_qnorm], writes=[r_qnorm])
        P.op("dve", lambda e: e.tensor_tensor(out=qtmp[:, 0:512].rearrange("p (h d) -> p h d", d=64),
                                              in0=A1[:, 0:512].rearrange("p (h d) -> p h d", d=64),
                                              in1=rq[:, 0:8].unsqueeze(2).to_broadcast([128, 8, 64]), op=ALU.mult),
             reads=[rA1, r_qnorm], writes=[r_qtmp])
        P.op("dve", lambda e: e.tensor_tensor(out=qtmp[:, 512:640].rearrange("p (h d) -> p h d", d=64),
                                              in0=A2[:, 0:128].rearrange("p (h d) -> p h d", d=64),
                                              in1=rq[:, 8:10].unsqueeze(2).to_broadcast([128, 2, 64]), op=ALU.mult),
             reads=[rA2, r_qnorm], writes=[r_qtmp])
        release(rA1)
        release(rA2)
        P.op("dve", lambda e: e.tensor_tensor(out=qn[:], in0=qtmp[:], in1=gainqk[:], op=ALU.mult),
             reads=[r_qtmp, r_const], writes=[r_qn])
        for h in range(8):
            P.op("pe", lambda e, h=h: e.transpose(out=TR[0:64, h * 128:(h + 1) * 128], in_=qn[:, h * 64:(h + 1) * 64],
                                                  identity=identb), reads=[r_qn, r_const], writes=[r_TR])
        P.op("act", lambda e: e.activation(out=qaT[:].rearrange("p a b -> p (a b)"), in_=TR[0:64, 0:1024], func=AF.Copy),
             reads=[r_TR], writes=[r_qaT])

    def front_k(tt):
        for g in range(2):
            P.op("pe", lambda e, g=g: e.transpose(out=TR[0:64, g * 128:(g + 1) * 128], in_=qn[:, 512 + g * 64:512 + (g + 1) * 64],
                                                  identity=identb), reads=[r_qn, r_const], writes=[r_TR])
        P.op("act", lambda e: e.activation(out=kaT[:, :, tt * 128:(tt + 1) * 128],
                                           in_=TR[0:64, 0:256].rearrange("p (g t) -> p g t", t=128), func=AF.Copy),
             reads=[r_TR], writes=[r_kaT])

    def front_b(tt):
        for half, c0 in ((0, 768), (1, 1280)):
            rB, B = psr.get()
            for c in range(4):
                for kc in range(8):
                    P.op("pe", lambda e, B=B, c=c, kc=kc, c0=c0: e.matmul(
                        B[:, c * 128:(c + 1) * 128], lhsT=win[:, kc, c0 + c * 128:c0 + (c + 1) * 128], rhs=hT[:, kc, :],
                        start=(kc == 0), stop=(kc == 7)), reads=[r_win, r_hT], writes=[rB])
            if half == 0:
                P.op("act", lambda e, B=B: e.activation(out=qbT[:].rearrange("p a b -> p (a b)"), in_=B[:], func=AF.Copy,
                                                        scale=0.125), reads=[rB], writes=[r_qbT])
                P.op("act", lambda e, B=B: e.activation(out=qbTn[:].rearrange("p a b -> p (a b)"), in_=B[:], func=AF.Copy,
                                                        scale=-0.125), reads=[rB], writes=[r_qbT])
            else:
                P.op("act", lambda e, B=B: e.activation(out=kbT[:, :, tt * 128:(tt + 1) * 128],
                                                        in_=B[:].rearrange("p (c t) -> p c t", t=128), func=AF.Copy),
                     reads=[rB], writes=[r_kbT])
            release(rB)

    def swa(gt, tt):
        yb = gt % 2
        kts = [1] if tt == 0 else [0, 1]
        rOA = OAb = None
        for hb in range(4):
            g = hb // 2
            rST, ST = psr.get()
            ptb = hb % 2
            for hh in range(2):
                h = 2 * hb + hh
                for kt in kts:
                    ktile = tt - 1 + kt
                    o = (hh * 2 + kt) * 128
                    P.op("pe", lambda e, ST=ST, o=o, g=g, h=h, ktile=ktile: e.matmul(
                        ST[:, o:o + 128], lhsT=kaT[0:64, g, ktile * 128:(ktile + 1) * 128], rhs=qaT[0:64, h, :],
                        start=True, stop=False), reads=[r_kaT, r_qaT], writes=[rST])
                    P.op("pe", lambda e, ST=ST, o=o, h=h, kt=kt: e.matmul(
                        ST[:, o:o + 128], lhsT=identb, rhs=swab[:, h, kt, :], start=False, stop=True),
                        reads=[r_const], writes=[rST])
            if tt == 0:
                for hh in range(2):
                    o = (hh * 2 + 1) * 128
                    P.op("act", lambda e, ST=ST, o=o, hh=hh, ptb=ptb: e.activation(
                        out=PT[ptb][:, hh, 1, :], in_=ST[:, o:o + 128], func=AF.Exp, scale=0.125),
                        reads=[rST], writes=[r_PT[ptb]])
            else:
                P.op("act", lambda e, ST=ST, ptb=ptb: e.activation(
                    out=PT[ptb][:].rearrange("p a b c -> p (a b c)"), in_=ST[:], func=AF.Exp, scale=0.125),
                    reads=[rST], writes=[r_PT[ptb]])
            release(rST)
            if hb % 2 == 0:
                rOA, OAb = psr.get()
            for hh in range(2):
                h = 2 * hb + hh
                o = (h % 4) * 65
                for ki, kt in enumerate(kts):
                    ktile = tt - 1 + kt
                    P.op("pe", lambda e, OAb=OAb, o=o, hh=hh, kt=kt, ktile=ktile, g=g, ptb=ptb, ki=ki: e.matmul(
                        OAb[:, o:o + 65], lhsT=PT[ptb][:, hh, kt, :], rhs=vaug[:, ktile, g, :],
                        start=(ki == 0), stop=(ki == len(kts) - 1)), reads=[r_PT[ptb], r_vaug], writes=[rOA])
            if hb % 2 == 1:
                hs = (hb // 2) * 4
                oav = OAb[:, 0:260].rearrange("p (h d) -> p h d", d=65)
                P.op("dve", lambda e, oav=oav, hs=hs: e.tensor_tensor(
                    out=dn[:], in0=oav[:, :, 64:65], in1=esink[:, hs:hs + 4].unsqueeze(2), op=ALU.add),
                    reads=[rOA, r_const], writes=[r_dn])
                P.op("dve", lambda e: e.reciprocal(out=dn[:], in_=dn[:]), reads=[r_dn], writes=[r_dn])
                P.op("dve", lambda e, oav=oav, hs=hs: e.tensor_tensor(
                    out=ya[:, hs * 64:(hs + 4) * 64].rearrange("p (h d) -> p h d", d=64), in0=oav[:, :, 0:64],
                    in1=dn[:].to_broadcast([128, 4, 64]), op=ALU.mult), reads=[rOA, r_dn], writes=[r_ya])
                release(rOA)
        for c in range(4):
            P.op("pe", lambda e, c=c: e.transpose(out=TR[:, c * 128:(c + 1) * 128], in_=ya[:, c * 128:(c + 1) * 128],
                                                  identity=identb), reads=[r_ya, r_const], writes=[r_TR])
        P.op("act", lambda e: e.activation(out=yaT[yb][:].rearrange("p a b -> p (a b)"), in_=TR[:, 0:512], func=AF.Copy),
             reads=[r_TR], writes=[r_yaT[yb]])
        P.dma("sp", lambda e: e.dma_start(out=dr["ysa"][gt], in_=yaT[yb][:].rearrange("p a b -> p (a b)")),
              reads=[r_yaT[yb]])

    def sbattn(gt, tt):
        yb = gt % 2
        for h in range(8):
            c = h // 2
            pb = (h % 2) * 64
            OUTb, rOUT = OUT[h % 2], r_OUT[h % 2]
            kbs = list(range(tt, -1, -1))
            batches = [kbs[i:i + BATCH] for i in range(0, len(kbs), BATCH)]
            nmm = len(kbs)
            mmi = 0
            for batch in batches:
                nb = len(batch)
                rZ, Z = psr.get()
                for b, kb in enumerate(batch):
                    P.op("pe", lambda e, Z=Z, b=b, kb=kb, c=c, pb=pb: e.matmul(
                        Z[:, b * 128:(b + 1) * 128], lhsT=kbT[pb:pb + 64, c, kb * 128:(kb + 1) * 128],
                        rhs=qbT[pb:pb + 64, c, :], start=True, stop=True), reads=[r_kbT, r_qbT], writes=[rZ])
                rE, E = ering.get()
                P.op("act", lambda e, Z=Z, E=E, nb=nb: e.activation(out=E[:, 0:nb * 128], in_=Z[:, 0:nb * 128], func=AF.Exp),
                     reads=[rZ], writes=[rE])
                release(rZ)
                rL, L = lring.get()
                P.op("act", lambda e, E=E, L=L, nb=nb: e.activation(out=L[:, 0:nb * 128], in_=E[:, 0:nb * 128], func=AF.Ln,
                                                                     bias=1.0), reads=[rE], writes=[rL])
                release(rE)
                for b, kb in enumerate(batch):
                    if kb == tt:
                        P.op("dve", lambda e, L=L, b=b: e.tensor_tensor(out=Sb[:, tt, :], in0=L[:, b * 128:(b + 1) * 128],
                                                                        in1=maskLb, op=ALU.mult),
                             reads=[rL, r_const], writes=[r_S[tt]])
                    elif kb >= 1:
                        P.op("dve", lambda e, L=L, b=b, kb=kb: e.tensor_tensor(out=Sb[:, kb, :], in0=Sb[:, kb + 1, :],
                                                                               in1=L[:, b * 128:(b + 1) * 128], op=ALU.add),
                             reads=[rL, r_S[kb + 1]], writes=[r_S[kb]])
                rC, C = psr.get()
                for b, kb in enumerate(batch):
                    o = b * 128
                    if kb == tt:
                        P.op("pe", lambda e, C=C, o=o: e.matmul(C[:, o:o + 128], lhsT=trib, rhs=Sb[:, tt, :], start=True, stop=False),
                             reads=[r_const, r_S[tt]], writes=[rC])
                    else:
                        P.op("pe", lambda e, C=C, o=o, L=L: e.matmul(C[:, o:o + 128], lhsT=trib, rhs=L[:, o:o + 128],
                                                                     start=True, stop=False), reads=[r_const, rL], writes=[rC])
                        P.op("pe", lambda e, C=C, o=o, kb=kb: e.matmul(C[:, o:o + 128], lhsT=onesb, rhs=Sb[:, kb + 1, :],
                                                                       start=False, stop=False),
                             reads=[r_const, r_S[kb + 1]], writes=[rC])
                    P.op("pe", lambda e, C=C, o=o, kb=kb, c=c, pb=pb: e.matmul(
                        C[:, o:o + 128], lhsT=kbT[pb:pb + 64, c, kb * 128:(kb + 1) * 128], rhs=qbTn[pb:pb + 64, c, :],
                        start=False, stop=(kb != tt)), reads=[r_kbT, r_qbT], writes=[rC])
                    if kb == tt:
                        P.op("pe", lambda e, C=C, o=o: e.matmul(C[:, o:o + 128], lhsT=identb, rhs=maskbb, start=False, stop=True),
                             reads=[r_const], writes=[rC])
                release(rL)
                rW, W = wring.get()
                P.op("act", lambda e, C=C, W=W, nb=nb: e.activation(out=W[:, 0:nb * 128], in_=C[:, 0:nb * 128], func=AF.Exp,
                                                                     scale=-1.0), reads=[rC], writes=[rW])
                release(rC)
                for b, kb in enumerate(batch):
                    P.op("pe", lambda e, W=W, b=b, kb=kb, h=h, OUTb=OUTb, mmi=mmi: e.matmul(
                        OUTb[0:64, 0:128], lhsT=vb[:, kb, h * 64:(h + 1) * 64], rhs=W[:, b * 128:(b + 1) * 128],
                        start=(mmi == 0), stop=(mmi == nmm - 1)), reads=[r_vb, rW], writes=[rOUT])
                    mmi += 1
                release(rW)
            P.op("act", lambda e, OUTb=OUTb, h=h: e.activation(out=ybT[yb][:, h, :], in_=OUTb[0:64, 0:128], func=AF.Copy),
                 reads=[rOUT], writes=[r_ybT[yb]])
        P.dma("sp", lambda e: e.dma_start(out=dr["ysb"][gt], in_=ybT[yb][:].rearrange("p a b -> p (a b)")),
              reads=[r_ybT[yb]])

    for sq_i in range(nseq):
        for tt in range(NT):
            gt = sq_i * NT + tt
            front(sq_i, tt)
            front_k(tt)
            front_b(tt)
            if tt not in SKIP_SWA:
                swa(gt, tt)
            if tt not in SKIP_SB:
                sbattn(gt, tt)
            if tt in SKIP_SWA:
                pass


def phase_a2(P, nc, dr, ntiles):
    R = lambda n: Res(n)
    wg = sb(nc, "m_wg", [128, 8, 2048], BF16)
    wupa = sb(nc, "m_wupa", [128, 4, 1024], BF16)
    wupb = sb(nc, "m_wupb", [64, 8, 1024], BF16)
    wout = sb(nc, "m_wout", [128, 8, 1024], BF16)
    stg = [sb(nc, f"m_stg{i}", [128, 1024], F32) for i in range(2)]
    g1col = sb(nc, "m_g1col", [128, 8], F32)
    ngb = sb(nc, "m_ngb", [128, 16], F32)
    identf = sb(nc, "m_identf", [128, 128], F32)
    identb = sb(nc, "m_identb", [128, 128], BF16)
    epsc = sb(nc, "m_eps", [128, 1], F32)
    xa = [sb(nc, f"m_xa{i}", [128, D], F32) for i in range(2)]
    xs = sb(nc, "m_xs", [128, D], BF16)
    junk = sb(nc, "m_junk", [128, D], BF16)
    hT = sb(nc, "m_hT", [128, 8, 128], BF16)
    ss = sb(nc, "m_ss", [128, 1], F32)
    lnv = sb(nc, "m_lnv", [128, 1], F32)
    rstd = sb(nc, "m_rstd", [128, 1], F32)
    yaT = [sb(nc, f"m_yaT{i}", [128, 4, 128], BF16) for i in range(2)]
    ybT = [sb(nc, f"m_ybT{i}", [64, 8, 128], BF16) for i in range(2)]
    eg = [sb(nc, f"m_eg{i}", [128, 2, 128], F32) for i in range(2)]
    tmp = [sb(nc, f"m_tmp{i}", [128, 2, 128], F32) for i in range(2)]
    mT = sb(nc, "m_mT", [128, 8, 128], BF16)
    x1 = [sb(nc, f"m_x1{i}", [128, D], F32) for i in range(2)]
    TR = ps(nc, "m_TR", [128, 1024], BF16)
    PSR = [ps(nc, f"m_PS{i}", [128, 512], F32) for i in range(6)]

    r_w, r_const = R("w"), R("const")
    r_stg = [R("stg0"), R("stg1")]
    r_xa = [R("xa0"), R("xa1")]
    r_xs, r_junk, r_hT, r_norm, r_mT, r_TR = (R(n) for n in ("xs", "junk", "hT", "norm", "mT", "TR"))
    r_yaT = [R("yaT0"), R("yaT1")]
    r_ybT = [R("ybT0"), R("ybT1")]
    r_eg = [R("eg0"), R("eg1")]
    r_tmp = [R("tmp0"), R("tmp1")]
    r_x1 = [R("x10"), R("x11")]
    psr = Ring([(R(f"PS{i}"), PSR[i]) for i in range(6)])

    P.dma("sp", lambda e: e.dma_start(out=g1col[:], in_=dr["g1col"]), writes=[r_const])
    P.dma("sp", lambda e: e.dma_start(out=ngb[:], in_=dr["gbcol"]), writes=[r_const])
    P.dma("sp", lambda e: e.dma_start(out=identf[:], in_=dr["ident"]), writes=[r_const])
    P.op("dve", lambda e: e.tensor_copy(out=identb[:], in_=identf[:]), reads=[r_const], writes=[r_const])
    P.op("dve", lambda e: e.tensor_scalar(out=ngb[:], in0=ngb[:], scalar1=-1.0, scalar2=None, op0=ALU.mult),
         reads=[r_const], writes=[r_const])
    P.op("dve", lambda e: e.memset(epsc[:], 1e-6), writes=[r_const])
    k = 0

    def load(dst_ap, src_ap, npart, scalar=None):
        nonlocal k
        s = k % 2
        k += 1
        P.dma("sp", lambda e: e.dma_start(out=stg[s][0:npart, :], in_=src_ap), writes=[r_stg[s]])
        if scalar is None:
            P.op("dve", lambda e: e.tensor_copy(out=dst_ap, in_=stg[s][0:npart, :]), reads=[r_stg[s]], writes=[r_w])
        else:
            P.op("dve", lambda e: e.tensor_scalar(out=dst_ap, in0=stg[s][0:npart, :], scalar1=scalar, scalar2=None,
                                                  op0=ALU.mult), reads=[r_stg[s], r_const], writes=[r_w])
    for kc in range(8):
        for hf in range(2):
            load(wg[:, kc, hf * 1024:(hf + 1) * 1024], dr["w_in"][kc * 128:(kc + 1) * 128, 2304 + hf * 1024:2304 + (hf + 1) * 1024],
                 128, g1col[:, kc:kc + 1])
    for i in range(4):
        load(wupa[:, i, :], dr["w_up_swa"][i * 128:(i + 1) * 128, :], 128)
    for h in range(8):
        load(wupb[:, h, :], dr["w_up_sb"][h * 64:(h + 1) * 64, :], 64)
    for dc in range(8):
        load(wout[:, dc, :], dr["w_out"][dc * 128:(dc + 1) * 128, :], 128)

    toks = []
    for gt in range(ntiles):
        b = gt % 2
        xap = dr["x"][gt * 128:(gt + 1) * 128, :]
        P.dma("sp", lambda e, xap=xap, b=b: e.dma_start(out=xa[b][:], in_=xap), writes=[r_xa[b]])
        P.dma("sp", lambda e, gt=gt, b=b: e.dma_start(out=yaT[b][:].rearrange("p a b -> p (a b)"), in_=dr["ysa"][gt]),
              writes=[r_yaT[b]])
        P.dma("sp", lambda e, gt=gt, b=b: e.dma_start(out=ybT[b][:].rearrange("p a b -> p (a b)"), in_=dr["ysb"][gt]),
              writes=[r_ybT[b]])
        P.op("act", lambda e, b=b: e.activation(out=junk[:], in_=xa[b][:], func=AF.Square, accum_out=ss[:]),
             reads=[r_xa[b]], writes=[r_junk, r_norm])
        P.op("act", lambda e: e.activation(out=lnv[:], in_=ss[:], func=AF.Ln, scale=1.0 / D, bias=epsc[:]),
             reads=[r_norm, r_const], writes=[r_norm])
        P.op("act", lambda e: e.activation(out=rstd[:], in_=lnv[:], func=AF.Exp, scale=-0.5), reads=[r_norm], writes=[r_norm])
        P.op("dve", lambda e, b=b: e.tensor_scalar(out=xs[:], in0=xa[b][:], scalar1=rstd[:], scalar2=None, op0=ALU.mult),
             reads=[r_xa[b], r_norm], writes=[r_xs])
        for kc in range(8):
            P.op("pe", lambda e, kc=kc: e.transpose(out=TR[:, kc * 128:(kc + 1) * 128], in_=xs[:, kc * 128:(kc + 1) * 128],
                                                    identity=identb[:]), reads=[r_xs, r_const], writes=[r_TR])
        P.op("act", lambda e: e.activation(out=hT[:].rearrange("p a b -> p (a b)"), in_=TR[:], func=AF.Copy),
             reads=[r_TR], writes=[r_hT])
        for dc in range(8):
            rM, M = psr.get()
            cs = slice(dc * 128, (dc + 1) * 128)
            for i in range(4):
                P.op("pe", lambda e, M=M, i=i, cs=cs, b=b: e.matmul(M[:, 0:128], lhsT=wupa[:, i, cs], rhs=yaT[b][:, i, :],
                                                                   start=(i == 0), stop=(i == 3)),
                     reads=[r_w, r_yaT[b]], writes=[rM])
            for h in range(8):
                P.op("pe", lambda e, M=M, h=h, cs=cs, b=b: e.matmul(M[:, 128:256], lhsT=wupb[:, h, cs], rhs=ybT[b][:, h, :],
                                                                   start=(h == 0), stop=(h == 7)),
                     reads=[r_w, r_ybT[b]], writes=[rM])
            for gi in range(2):
                gs = slice(gi * 1024 + dc * 128, gi * 1024 + (dc + 1) * 128)
                for kc in range(8):
                    P.op("pe", lambda e, M=M, gi=gi, gs=gs, kc=kc: e.matmul(M[:, (2 + gi) * 128:(3 + gi) * 128], lhsT=wg[:, kc, gs],
                                                                           rhs=hT[:, kc, :], start=(kc == 0), stop=(kc == 7)),
                         reads=[r_w, r_hT], writes=[rM])
            eb = dc % 2
            for gi in range(2):
                P.op("act", lambda e, M=M, gi=gi, dc=dc, eb=eb: e.activation(
                    out=eg[eb][:, gi, :], in_=M[:, (2 + gi) * 128:(3 + gi) * 128], func=AF.Exp, scale=-1.0,
                    bias=ngb[:, gi * 8 + dc:gi * 8 + dc + 1]), reads=[rM, r_const], writes=[r_eg[eb]])
            P.op("dve", lambda e, eb=eb: e.tensor_scalar(out=eg[eb][:], in0=eg[eb][:], scalar1=1.0, scalar2=None, op0=ALU.add),
                 reads=[r_eg[eb]], writes=[r_eg[eb]])
            P.op("dve", lambda e, eb=eb: e.reciprocal(out=eg[eb][:], in_=eg[eb][:]), reads=[r_eg[eb]], writes=[r_eg[eb]])
            P.op("dve", lambda e, M=M, eb=eb: e.tensor_tensor(out=tmp[eb][:].rearrange("p a b -> p (a b)"),
                                                              in0=eg[eb][:].rearrange("p a b -> p (a b)"), in1=M[:, 0:256],
                                                              op=ALU.mult), reads=[r_eg[eb], rM], writes=[r_tmp[eb]])
            release(rM)
            P.op("dve", lambda e, eb=eb, dc=dc: e.tensor_tensor(out=mT[:, dc, :], in0=tmp[eb][:, 0, :], in1=tmp[eb][:, 1, :],
                                                                op=ALU.add), reads=[r_tmp[eb]], writes=[r_mT])
        for hf in range(2):
            rO, O = psr.get()
            for dc in range(8):
                P.op("pe", lambda e, O=O, dc=dc, hf=hf: e.matmul(O[:, 0:512], lhsT=mT[:, dc, :],
                                                                rhs=wout[:, dc, hf * 512:(hf + 1) * 512],
                                                                start=(dc == 0), stop=(dc == 7)), reads=[r_mT, r_w], writes=[rO])
            P.op("dve", lambda e, O=O, hf=hf, b=b: e.tensor_tensor(out=x1[b][:, hf * 512:(hf + 1) * 512],
                                                                   in0=xa[b][:, hf * 512:(hf + 1) * 512], in1=O[:, 0:512],
                                                                   op=ALU.add), reads=[r_xa[b], rO], writes=[r_x1[b]])
            release(rO)
        yap = dr["y"][gt * 128:(gt + 1) * 128, :]
        toks.append(P.dma("sp", lambda e, yap=yap, b=b: e.dma_start(out=yap, in_=x1[b][:]), reads=[r_x1[b]]))
    return toks


NG = 6


def phase_b(P, nc, dr, ntiles, x1_res):
    wq = sb(nc, "b_wq", [128, 8, 2048], BF16)
    skT = sb(nc, "b_skT", [128, 16, 128], F32)
    g2bc = sb(nc, "b_g2bc", [128, D], F32)
    g2col = sb(nc, "b_g2col", [128, 8], F32)
    iota16 = sb(nc, "b_iota", [128, 16], F32)
    identf = sb(nc, "b_identf", [128, 128], F32)
    identb = sb(nc, "b_identb", [128, 128], BF16)
    epsc = sb(nc, "b_eps", [128, 1], F32)
    stg = [sb(nc, f"b_stg{i}", [128, 2048], F32) for i in range(2)]
    xt = [sb(nc, f"b_xt{i}", [128, D], F32) for i in range(2)]
    xs = sb(nc, "b_xs", [128, D], BF16)
    junk = sb(nc, "b_junk", [128, D], BF16)
    h2 = [sb(nc, f"b_h2{i}", [128, D], F32) for i in range(2)]
    h2T = sb(nc, "b_h2T", [128, 8, 128], BF16)
    qT = sb(nc, "b_qT", [128, 16, 128], F32)
    sc = sb(nc, "b_sc", [128, 16, 128], F32)
    sc2 = sb(nc, "b_sc2", [128, 16, 128], F32)
    cand = sb(nc, "b_cand", [128, 8, 256], F32)
    cand2 = sb(nc, "b_cand2", [128, 8, 256], F32)
    oh = sb(nc, "b_oh", [128, 8, 16, 16], F32)
    tops = sb(nc, "b_tops", [128, 16, 16], F32)
    topi = sb(nc, "b_topi", [128, 16, 16], U32)
    topif = sb(nc, "b_topif", [128, 16, 16], F32)
    bests = sb(nc, "b_bests", [128, 8, 16], F32)
    bestp = sb(nc, "b_bestp", [128, 8, 16], U32)
    hiu = sb(nc, "b_hiu", [128, 8, 16], U32)
    lou = sb(nc, "b_lou", [128, 8, 16], U32)
    hif = sb(nc, "b_hif", [128, 8, 16], F32)
    lof = sb(nc, "b_lof", [128, 8, 16], F32)
    i0s = sb(nc, "b_i0s", [128, 8, 16], F32)
    i1s = sb(nc, "b_i1s", [128, 8, 16], F32)
    expf = sb(nc, "b_expf", [128, 128], F32)
    idx = [sb(nc, f"b_idx{i}", [128, 128], I32) for i in range(2)]
    gd = sb(nc, "b_gd", [128, 8, 16], F32)
    gsum = sb(nc, "b_gsum", [128, 8], F32)
    gate = [sb(nc, f"b_gate{i}", [128, 8, 16], F32) for i in range(2)]
    dots = sb(nc, "b_dots", [128, 128], F32)
    act = sb(nc, "b_act", [128, 128], F32)
    wgt = sb(nc, "b_wgt", [128, 128], F32)
    ss = sb(nc, "b_ss", [128, 1], F32)
    lnv = sb(nc, "b_lnv", [128, 1], F32)
    rstd = sb(nc, "b_rstd", [128, 1], F32)
    G = [sb(nc, f"b_G{i}", [128, D], F32) for i in range(NG)]
    acc = [sb(nc, f"b_acc{i}", [128, D], F32) for i in range(2)]
    TR = ps(nc, "b_TR", [128, 1024], BF16)
    PS = [ps(nc, f"b_PS{i}", [128, 512], F32) for i in range(4)]

    R = lambda n: Res(n)
    r_wq, r_skT, r_const = R("wq"), R("skT"), R("const")
    r_stg = [R("stg0"), R("stg1")]
    r_xt = [R("xt0"), R("xt1")]
    r_xs, r_junk, r_h2T, r_qT, r_sc, r_sc2 = R("xs"), R("junk"), R("h2T"), R("qT"), R("sc"), R("sc2")
    r_h2 = [R("h2a"), R("h2b")]
    r_cand, r_cand2, r_oh, r_top, r_best, r_sel = R("cand"), R("cand2"), R("oh"), R("top"), R("best"), R("sel")
    r_idx = [R("idx0"), R("idx1")]
    r_gate = [R("gate0"), R("gate1")]
    r_gtmp = R("gtmp")
    r_dots, r_wgt, r_norm = R("dots"), R("wgt"), R("norm")
    r_G = [R(f"G{i}") for i in range(NG)]
    r_acc = [R("acc0"), R("acc1")]
    r_TR = R("TR")
    r_PS = [R(f"PS{i}") for i in range(4)]
    r_y = [R(f"y{i}") for i in range(ntiles)]

    P.dma("sp", lambda e: e.dma_start(out=skT[:], in_=dr["skT"]), writes=[r_skT])
    P.dma("sp", lambda e: e.dma_start(out=g2bc[:], in_=dr["g2bc"]), writes=[r_const])
    P.dma("sp", lambda e: e.dma_start(out=g2col[:], in_=dr["g2col"]), writes=[r_const])
    P.dma("sp", lambda e: e.dma_start(out=iota16[:], in_=dr["iota16"]), writes=[r_const])
    P.dma("sp", lambda e: e.dma_start(out=identf[:], in_=dr["ident"]), writes=[r_const])
    P.op("dve", lambda e: e.tensor_copy(out=identb[:], in_=identf[:]), reads=[r_const], writes=[r_const])
    P.op("dve", lambda e: e.memset(epsc[:], 1e-6), writes=[r_const])
    for kc in range(8):
        s = kc % 2
        P.dma("sp", lambda e, kc=kc, s=s: e.dma_start(out=stg[s][:], in_=dr["wq"][kc * 128:(kc + 1) * 128, :]),
              writes=[r_stg[s]])
        P.op("dve", lambda e, kc=kc, s=s: e.tensor_scalar(out=wq[:, kc, :], in0=stg[s][:], scalar1=g2col[:, kc:kc + 1],
                                                         scalar2=None, op0=ALU.mult),
             reads=[r_stg[s], r_const], writes=[r_wq])

    def front(i):
        b = i % 2
        x1ap = dr["x1"][i * 128:(i + 1) * 128, :]
        rd = [x1_res[i]] if x1_res is not None else []
        P.dma("sp", lambda e: e.dma_start(out=xt[b][:], in_=x1ap), reads=rd, writes=[r_xt[b]])
        P.op("act", lambda e: e.activation(out=junk[:], in_=xt[b][:], func=AF.Square, accum_out=ss[:]),
             reads=[r_xt[b]], writes=[r_junk, r_norm])
        P.op("act", lambda e: e.activation(out=lnv[:], in_=ss[:], func=AF.Ln, scale=1.0 / D, bias=epsc[:]),
             reads=[r_norm, r_const], writes=[r_norm])
        P.op("act", lambda e: e.activation(out=rstd[:], in_=lnv[:], func=AF.Exp, scale=-0.5),
             reads=[r_norm], writes=[r_norm])
        P.op("dve", lambda e: e.tensor_scalar(out=xs[:], in0=xt[b][:], scalar1=rstd[:], scalar2=None, op0=ALU.mult),
             reads=[r_xt[b], r_norm], writes=[r_xs])
        P.op("dve", lambda e: e.scalar_tensor_tensor(out=h2[b][:], in0=xt[b][:], scalar=rstd[:], in1=g2bc[:],
                                                     op0=ALU.mult, op1=ALU.mult),
             reads=[r_xt[b], r_norm, r_const], writes=[r_h2[b]])
        for kc in range(8):
            P.op("pe", lambda e, kc=kc: e.transpose(out=TR[:, kc * 128:(kc + 1) * 128], in_=xs[:, kc * 128:(kc + 1) * 128],
                                                    identity=identb[:]),
                 reads=[r_xs, r_const], writes=[r_TR])
        P.op("act", lambda e: e.activation(out=h2T[:].rearrange("p a b -> p (a b)"), in_=TR[:], func=AF.Copy),
             reads=[r_TR], writes=[r_h2T])
        for g in range(4):
            for j in range(4):
                hp = g * 4 + j
                for kc in range(8):
                    P.op("pe", lambda e, g=g, j=j, hp=hp, kc=kc: e.matmul(
                        PS[g][:, j * 128:(j + 1) * 128], lhsT=wq[:, kc, hp * 128:(hp + 1) * 128], rhs=h2T[:, kc, :],
                        start=(kc == 0), stop=(kc == 7)),
                        reads=[r_wq, r_h2T], writes=[r_PS[g]])
            P.op("act", lambda e, g=g: e.activation(out=qT[:, g * 4:(g + 1) * 4, :].rearrange("p a b -> p (a b)"),
                                                    in_=PS[g][:], func=AF.Copy),
                 reads=[r_PS[g]], writes=[r_qT])
        for g in range(4):
            for j in range(4):
                hp = g * 4 + j
                P.op("pe", lambda e, g=g, j=j, hp=hp: e.matmul(
                    PS[g][:, j * 128:(j + 1) * 128], lhsT=qT[:, hp, :], rhs=skT[:, hp, :], start=True, stop=True),
                    reads=[r_qT, r_skT], writes=[r_PS[g]])
            P.op("act", lambda e, g=g: e.activation(out=sc[:, g * 4:(g + 1) * 4, :].rearrange("p a b -> p (a b)"),
                                                    in_=PS[g][:], func=AF.Copy),
                 reads=[r_PS[g]], writes=[r_sc])
        for hp in range(16):
            P.op("dve", lambda e, hp=hp: e.max(out=tops[:, hp, 0:8], in_=sc[:, hp, :]), reads=[r_sc], writes=[r_top])
            P.op("dve", lambda e, hp=hp: e.max_index(out=topi[:, hp, 0:8], in_max=tops[:, hp, 0:8], in_values=sc[:, hp, :]),
                 reads=[r_sc, r_top], writes=[r_top])
            P.op("dve", lambda e, hp=hp: e.match_replace(out=sc2[:, hp, :], in_to_replace=tops[:, hp, 0:8],
                                                         in_values=sc[:, hp, :], imm_value=-1e30),
                 reads=[r_sc, r_top], writes=[r_sc2])
            P.op("dve", lambda e, hp=hp: e.max(out=tops[:, hp, 8:16], in_=sc2[:, hp, :]), reads=[r_sc2], writes=[r_top])
            P.op("dve", lambda e, hp=hp: e.max_index(out=topi[:, hp, 8:16], in_max=tops[:, hp, 8:16], in_values=sc2[:, hp, :]),
                 reads=[r_sc2, r_top], writes=[r_top])
        P.op("dve", lambda e: e.tensor_copy(out=topif[:], in_=topi[:]), reads=[r_top], writes=[r_top])
        tv = tops[:].rearrange("p (h t) k -> p h t k", t=2)
        in0 = tv[:, :, 0, :].unsqueeze(3).to_broadcast([128, 8, 16, 16])
        in1 = tv[:, :, 1, :].unsqueeze(2).to_broadcast([128, 8, 16, 16])
        candv = cand[:].rearrange("p h (i j) -> p h i j", j=16)
        P.op("dve", lambda e: e.tensor_tensor(out=candv, in0=in0, in1=in1, op=ALU.add), reads=[r_top], writes=[r_cand])
        for h in range(8):
            P.op("dve", lambda e, h=h: e.max(out=bests[:, h, 0:8], in_=cand[:, h, :]), reads=[r_cand], writes=[r_best])
            P.op("dve", lambda e, h=h: e.max_index(out=bestp[:, h, 0:8], in_max=bests[:, h, 0:8], in_values=cand[:, h, :]),
                 reads=[r_cand, r_best], writes=[r_best])
            P.op("dve", lambda e, h=h: e.match_replace(out=cand2[:, h, :], in_to_replace=bests[:, h, 0:8],
                                                       in_values=cand[:, h, :], imm_value=-1e30),
                 reads=[r_cand, r_best], writes=[r_cand2])
            P.op("dve", lambda e, h=h: e.max(out=bests[:, h, 8:16], in_=cand2[:, h, :]), reads=[r_cand2], writes=[r_best])
            P.op("dve", lambda e, h=h: e.max_index(out=bestp[:, h, 8:16], in_max=bests[:, h, 8:16], in_values=cand2[:, h, :]),
                 reads=[r_cand2, r_best], writes=[r_best])
        P.op("dve", lambda e: e.tensor_single_scalar(out=hiu[:], in_=bestp[:], scalar=4, op=ALU.logical_shift_right),
             reads=[r_best], writes=[r_sel])
        P.op("dve", lambda e: e.tensor_single_scalar(out=lou[:], in_=bestp[:], scalar=15, op=ALU.bitwise_and),
             reads=[r_best], writes=[r_sel])
        P.op("dve", lambda e: e.tensor_copy(out=hif[:], in_=hiu[:]), reads=[r_sel], writes=[r_sel])
        P.op("dve", lambda e: e.tensor_copy(out=lof[:], in_=lou[:]), reads=[r_sel], writes=[r_sel])
        iota_bc = iota16[:].unsqueeze(1).unsqueeze(1).to_broadcast([128, 8, 16, 16])
        tiv = topif[:].rearrange("p (h t) k -> p h t k", t=2)
        for (selin, tsel, outsel) in ((hif, 0, i0s), (lof, 1, i1s)):
            P.op("dve", lambda e, selin=selin: e.tensor_tensor(
                out=oh[:], in0=iota_bc, in1=selin[:].unsqueeze(3).to_broadcast([128, 8, 16, 16]), op=ALU.is_equal),
                reads=[r_sel, r_const], writes=[r_oh])
            P.op("dve", lambda e, tsel=tsel: e.tensor_tensor(
                out=oh[:], in0=oh[:], in1=tiv[:, :, tsel, :].unsqueeze(2).to_broadcast([128, 8, 16, 16]), op=ALU.mult),
                reads=[r_oh, r_top], writes=[r_oh])
            P.op("dve", lambda e, outsel=outsel: e.tensor_reduce(out=outsel[:], in_=oh[:], axis=AX.X, op=ALU.add),
                 reads=[r_oh], writes=[r_sel])
        P.op("dve", lambda e: e.scalar_tensor_tensor(out=expf[:].rearrange("p (h k) -> p h k", k=16), in0=i0s[:],
                                                     scalar=128.0, in1=i1s[:], op0=ALU.mult, op1=ALU.add),
             reads=[r_sel], writes=[r_sel])
        P.op("dve", lambda e: e.tensor_copy(out=idx[b][:], in_=expf[:]), reads=[r_sel], writes=[r_idx[b]])
        P.op("dve", lambda e: e.tensor_tensor(out=gd[:], in0=bests[:], in1=bests[:, :, 0:1].to_broadcast([128, 8, 16]),
                                              op=ALU.subtract), reads=[r_best], writes=[r_gtmp])
        P.op("act", lambda e: e.activation(out=gd[:], in_=gd[:], func=AF.Exp), reads=[r_gtmp], writes=[r_gtmp])
        P.op("dve", lambda e: e.tensor_reduce(out=gsum[:], in_=gd[:], axis=AX.X, op=ALU.add), reads=[r_gtmp], writes=[r_gtmp])
        P.op("dve", lambda e: e.reciprocal(out=gsum[:], in_=gsum[:]), reads=[r_gtmp], writes=[r_gtmp])
        P.op("dve", lambda e: e.tensor_tensor(out=gate[b][:], in0=gd[:], in1=gsum[:].unsqueeze(2).to_broadcast([128, 8, 16]),
                                              op=ALU.mult), reads=[r_gtmp], writes=[r_gate[b]])

    gring = {"i": 0}

    def back(i):
        b = i % 2
        for j in range(128):
            g = gring["i"] % NG
            gring["i"] += 1
            P.dma("pool", lambda e, g=g, j=j: e.indirect_dma_start(
                out=G[g][:], out_offset=None, in_=dr["u"],
                in_offset=bass.IndirectOffsetOnAxis(ap=idx[b][:, j:j + 1], axis=0)),
                reads=[r_idx[b]], writes=[r_G[g]])
            P.op("dve", lambda e, g=g, j=j: e.scalar_tensor_tensor(
                out=junk[:], in0=G[g][:], scalar=1.0, in1=h2[b][:], op0=ALU.mult, op1=ALU.mult,
                accum_out=dots[:, j:j + 1]),
                reads=[r_G[g], r_h2[b]], writes=[r_junk, r_dots])
        P.op("act", lambda e: e.activation(out=act[:], in_=dots[:], func=AF.Gelu), reads=[r_dots], writes=[r_wgt])
        P.op("dve", lambda e: e.tensor_tensor(out=wgt[:], in0=act[:], in1=gate[b][:].rearrange("p h k -> p (h k)"),
                                              op=ALU.mult), reads=[r_wgt, r_gate[b]], writes=[r_wgt])
        if i == 0 and "dbg" in dr:
            dbg = dr["dbg"]
            P.dma("sp", lambda e: e.dma_start(out=dbg[0], in_=dots[:]), reads=[r_dots])
            P.dma("sp", lambda e: e.dma_start(out=dbg[1], in_=wgt[:]), reads=[r_wgt])
            P.dma("sp", lambda e: e.dma_start(out=dbg[2], in_=expf[:]), reads=[r_sel])
            P.dma("sp", lambda e: e.dma_start(out=dbg[3], in_=gate[b][:].rearrange("p h k -> p (h k)")), reads=[r_gate[b]])
            P.dma("sp", lambda e: e.dma_start(out=dbg[4], in_=bests[:].rearrange("p h k -> p (h k)")), reads=[r_best])
            P.dma("sp", lambda e: e.dma_start(out=dbg[5], in_=topif[:, 0:8, :].rearrange("p h k -> p (h k)")), reads=[r_top])
            P.dma("sp", lambda e: e.dma_start(out=dbg[6], in_=tops[:, 0:8, :].rearrange("p h k -> p (h k)")), reads=[r_top])
            P.dma("sp", lambda e: e.dma_start(out=dbg[7], in_=sc[:, 0, :]), reads=[r_sc])
        for j in range(128):
            g = gring["i"] % NG
            gring["i"] += 1
            P.dma("pool", lambda e, g=g, j=j: e.indirect_dma_start(
                out=G[g][:], out_offset=None, in_=dr["v"],
                in_offset=bass.IndirectOffsetOnAxis(ap=idx[b][:, j:j + 1], axis=0)),
                reads=[r_idx[b]], writes=[r_G[g]])
            src = xt[b] if j == 0 else acc[b]
            rsrc = r_xt[b] if j == 0 else r_acc[b]
            P.op("dve", lambda e, g=g, j=j, src=src: e.scalar_tensor_tensor(
                out=acc[b][:], in0=G[g][:], scalar=wgt[:, j:j + 1], in1=src[:], op0=ALU.mult, op1=ALU.add),
                reads=[r_G[g], r_wgt, rsrc], writes=[r_acc[b]])
        yap = dr["y"][i * 128:(i + 1) * 128, :]
        wr = [r_y[i]] + ([x1_res[i]] if x1_res is not None else [])
        return P.dma("sp", lambda e: e.dma_start(out=yap, in_=acc[b][:]), reads=[r_acc[b]], writes=wr)

    out_toks = []
    front(0)
    for i in range(ntiles):
        if i + 1 < ntiles:
            front(i + 1)
        out_toks.append(back(i))
    return out_toks


NCORES = 8
SEQ_PER_CORE = 4


def build_program(nseq):
    nc = bass.Bass("TRN2", target_bir_lowering=False)
    nt = nseq * 16

    def din(name, shape, dt=F32):
        return nc.dram_tensor(name, list(shape), dt, kind="ExternalInput").ap()
    dr = {"x": din("x", [nt * 128, 1024]), "w_in": din("w_in", [1024, 4352]), "g1col": din("g1col", [128, 8]),
          "gainqk": din("gainqk", [128, 640]), "sinks_bc": din("sinks_bc", [128, 8]), "cst": din("cst", [5, 128, 128]),
          "swab": din("swab", [128, 2048]), "gbcol": din("gbcol", [128, 16]), "w_up_swa": din("w_up_swa", [512, 1024]),
          "w_up_sb": din("w_up_sb", [512, 1024]), "w_out": din("w_out", [1024, 1024]),
          "wq": din("wq", [1024, 2048]), "g2col": din("g2col", [128, 8]), "g2bc": din("g2bc", [128, 1024]),
          "skT": din("skT", [128, 16, 128]), "u": din("u", [16384, 1024]), "v": din("v", [16384, 1024]),
          "iota16": din("iota16", [128, 16])}
    dr["ident"] = dr["cst"][0]
    dr["y"] = nc.dram_tensor("y", [nt * 128, 1024], F32, kind="ExternalOutput").ap()
    dr["x1"] = dr["y"]
    dr["ysa"] = nc.dram_tensor("ysa", [nt, 128, 512], BF16).ap()
    dr["ysb"] = nc.dram_tensor("ysb", [nt, 64, 1024], BF16).ap()
    P = Prog(nc)
    with ExitStack() as st:
        STACK[0] = st
        phase_a1(P, nc, dr, nseq)
        P.barrier()
        P.replay()
    with ExitStack() as st:
        STACK[0] = st
        phase_a2(P, nc, dr, nt)
        P.barrier()
        P.replay()
    with ExitStack() as st:
        STACK[0] = st
        toks = phase_b(P, nc, dr, nt, None)
        P.wait_tokens("sp", toks)
        P.barrier()
        P.replay()
    return nc


def host_inputs(inputs):
    f = lambda k: np.asarray(inputs[k], np.float32)
    cst, swab = consts()
    g1 = f("mix_norm_gain")[0]
    g2 = f("ffn_norm_gain")[0]
    sk = f("peer_sub_keys")[0]
    shared = {
        "w_in": np.ascontiguousarray(f("w_in")[0]), "g1col": col(g1, 8),
        "gainqk": bc(np.concatenate([np.tile(f("swa_q_gain")[0], 8), np.tile(f("swa_k_gain")[0], 2)])),
        "sinks_bc": bc(f("swa_sinks")[0]), "cst": cst, "swab": swab, "gbcol": col(f("gate_bias")[0], 16),
        "w_up_swa": np.ascontiguousarray(f("w_up_swa")[0]), "w_up_sb": np.ascontiguousarray(f("w_up_sb")[0]),
        "w_out": np.ascontiguousarray(f("w_out")[0]), "wq": np.ascontiguousarray(f("peer_w_q")[0]),
        "g2col": col(g2, 8), "g2bc": bc(g2),
        "skT": np.ascontiguousarray(sk.reshape(16, 128, 128).transpose(2, 0, 1)),
        "u": np.ascontiguousarray(f("peer_u")[0]), "v": np.ascontiguousarray(f("peer_v")[0]),
        "iota16": bc(np.arange(16, dtype=np.float32)),
    }
    return shared


def kernel(**inputs):
    x = np.asarray(inputs["x"], np.float32)
    B, S_, D_ = x.shape
    nseq = B // NCORES
    shared = host_inputs(inputs)
    nc = build_program(nseq)
    in_maps = []
    for c in range(NCORES):
        m = dict(shared)
        m["x"] = np.ascontiguousarray(x[c * nseq:(c + 1) * nseq].reshape(nseq * S_, D_))
        in_maps.append(m)
    res = run_bass_kernel_spmd(nc, in_maps, core_ids=list(range(NCORES)))
    out = np.concatenate([np.asarray(r["y"], np.float32).reshape(nseq, S_, D_) for r in res.results], axis=0)
    return out
```

```python
from contextlib import ExitStack
import numpy as np
import concourse.bass as bass
import concourse.mybir as mybir
from concourse.bass_utils import run_bass_kernel_spmd


F32 = mybir.dt.float32
BF16 = mybir.dt.bfloat16
I32 = mybir.dt.int32
U32 = mybir.dt.uint32
AF = mybir.ActivationFunctionType
ALU = mybir.AluOpType
AX = mybir.AxisListType

SAME_ENGINE_SYNC = True
N_DMA_SEMS = {"sp": 12, "pool": 8, "act": 4}


class Res:
    __slots__ = ("name", "w", "r", "open")

    def __init__(self, name):
        self.name = name
        self.w = None
        self.r = {}
        self.open = False


class Ring:
    def __init__(self, items):
        self.items = list(items)
        self.i = 0

    def get(self):
        it = self.items[self.i % len(self.items)]
        self.i += 1
        res = it[0] if isinstance(it, tuple) else it
        assert not res.open, f"ring slot {res.name} still open"
        res.open = True
        return it


def release(res):
    res.open = False


class Prog:
    ENG = ("pe", "dve", "act", "pool", "sp")

    def __init__(self, nc):
        self.nc = nc
        self.ops = {e: [] for e in self.ENG}
        self.cnt = {e: 0 for e in self.ENG}
        self.seen = {e: {} for e in self.ENG}
        self.sems = {}
        for e in ("pe", "dve", "act", "pool"):
            self.sems[e] = nc.alloc_semaphore(f"s_{e}")
        self.dma_pool = {}
        for q, n in N_DMA_SEMS.items():
            self.dma_pool[q] = {"i": 0, "sems": []}
            for k in range(n):
                key = ("dma", q, k)
                self.sems[key] = nc.alloc_semaphore(f"s_dma_{q}_{k}")
                self.dma_pool[q]["sems"].append([key, 0])
        self.n_wait = 0

    def _deps(self, eng, reads, writes):
        deps = {}

        def add(tok):
            if tok is None:
                return
            k, v = tok
            if deps.get(k, 0) < v:
                deps[k] = v
        for r in reads:
            add(r.w)
        for w in writes:
            add(w.w)
            for k, v in w.r.items():
                add((k, v))
        out = []
        for k, v in deps.items():
            if k == eng and (eng == "pe" or not SAME_ENGINE_SYNC):
                continue
            if self.seen[eng].get(k, 0) >= v:
                continue
            self.seen[eng][k] = v
            out.append((k, v))
        return out

    def _update(self, tok, reads, writes):
        k, v = tok
        for r in reads:
            if r.r.get(k, 0) < v:
                r.r[k] = v
        for w in writes:
            w.w = tok
            w.r = {}

    def op(self, eng, fn, reads=(), writes=()):
        waits = self._deps(eng, reads, writes)
        self.cnt[eng] += 1
        tok = (eng, self.cnt[eng])
        self.ops[eng].append((waits, fn, (eng, 1)))
        self._update(tok, reads, writes)
        self.n_wait += len(waits)
        return tok

    def dma(self, q, fn, reads=(), writes=()):
        pool = self.dma_pool[q]
        slot = pool["sems"][pool["i"] % len(pool["sems"])]
        pool["i"] += 1
        key, cnt = slot
        waits = self._deps(q, reads, writes)
        if cnt > 0 and self.seen[q].get(key, 0) < 16 * cnt:
            self.seen[q][key] = 16 * cnt
            waits.append((key, 16 * cnt))
        slot[1] = cnt + 1
        tok = (key, 16 * (cnt + 1))
        self.ops[q].append((waits, fn, (key, 16)))
        self._update(tok, reads, writes)
        return tok

    def wait_tokens(self, eng, toks):
        waits = []
        for k, v in toks:
            if self.seen[eng].get(k, 0) >= v:
                continue
            self.seen[eng][k] = v
            waits.append((k, v))
        if waits:
            self.ops[eng].append((waits, None, None))

    def all_tokens(self):
        toks = []
        for e in ("pe", "dve", "act", "pool"):
            if self.cnt[e] > 0:
                toks.append((e, self.cnt[e]))
        for q, pool in self.dma_pool.items():
            for key, cnt in pool["sems"]:
                if cnt > 0:
                    toks.append((key, 16 * cnt))
        return toks

    def barrier(self):
        toks = self.all_tokens()
        for e in self.ENG:
            self.wait_tokens(e, [t for t in toks if not (e == 'pe' and t[0] == 'pe')])

    def replay(self):
        nc = self.nc
        P = self
        with nc.Block() as block:
            def run(e, name):
                for waits, fn, inc in P.ops[name]:
                    for k, v in waits:
                        e.wait_ge(P.sems[k], v)
                    if fn is not None:
                        ins = fn(e)
                        ins.then_inc(P.sems[inc[0]], inc[1])

            @block.sync
            def _(e):
                run(e, "sp")

            @block.tensor
            def _(e):
                run(e, "pe")

            @block.vector
            def _(e):
                run(e, "dve")

            @block.scalar
            def _(e):
                run(e, "act")

            @block.gpsimd
            def _(e):
                run(e, "pool")
        for e in self.ENG:
            self.ops[e] = []


STACK = [None]


def sb(nc, name, shape, dt):
    return STACK[0].enter_context(nc.sbuf_tensor(name, list(shape), dt))


def ps(nc, name, shape, dt):
    return STACK[0].enter_context(nc.psum_tensor(name, list(shape), dt))


def consts():
    j = np.arange(128)[:, None]; t = np.arange(128)[None, :]
    ident = np.eye(128, dtype=np.float32)
    tri = (j >= t).astype(np.float32)
    ones = np.ones((128, 128), np.float32)
    maskL = (j < t).astype(np.float32)
    maskb = np.where(j < t, 0.0, 30000.0).astype(np.float32)
    cst = np.stack([ident, tri, ones, maskL, maskb]).astype(np.float32)
    slopes = 2.0 ** (-8.0 * np.arange(1, 9) / 8)
    k = np.arange(128)[:, None]; q = np.arange(128)[None, :]
    swab = np.zeros((128, 8, 2, 128), np.float32)
    for kt in range(2):
        spos = (kt - 1) * 128 + k
        tpos = q
        ck = spos // 64
        cq = tpos // 64
        vis = (ck <= cq) & (ck >= cq - 2)
        dist = np.abs(tpos - spos).astype(np.float64)
        for h in range(8):
            b = np.where(vis, -slopes[h] * dist * 8.0, -240000.0)
            swab[:, h, kt, :] = b
    return cst, np.ascontiguousarray(swab.reshape(128, 2048))
def col(v, n):
    return np.ascontiguousarray(np.asarray(v, np.float32).reshape(n, 128).T)
def bc(v):
    v = np.asarray(v, np.float32).reshape(1, -1)
    return np.ascontiguousarray(np.broadcast_to(v, (128, v.shape[1])))


D = 1024
S = 2048
NT = 16
BATCH = 2
SKIP_SB = set()
SKIP_SWA = set()


def phase_a1(P, nc, dr, nseq):
    R = lambda n: Res(n)
    win = sb(nc, "a_win", [128, 8, 2304], BF16)
    stg = [sb(nc, f"a_stg{i}", [128, 1152], F32) for i in range(2)]
    g1col = sb(nc, "a_g1col", [128, 8], F32)
    gainqk = sb(nc, "a_gainqk", [128, 640], F32)
    esink = sb(nc, "a_esink", [128, 8], F32)
    epsc = sb(nc, "a_eps", [128, 1], F32)
    cstf = sb(nc, "a_cstf", [128, 5, 128], F32)
    cstb = sb(nc, "a_cstb", [128, 5, 128], BF16)
    identb, trib, onesb, maskLb, maskbb = (cstb[:, k, :] for k in range(5))
    swabf = sb(nc, "a_swabf", [128, 2048], F32)
    swab = sb(nc, "a_swab", [128, 8, 2, 128], BF16)
    kaT = sb(nc, "a_kaT", [64, 2, S], BF16)
    kbT = sb(nc, "a_kbT", [128, 4, S], BF16)
    vb = sb(nc, "a_vb", [128, NT, 512], BF16)
    vaug = sb(nc, "a_vaug", [128, NT, 2, 65], BF16)
    xa = [sb(nc, f"a_xa{i}", [128, D], F32) for i in range(2)]
    xs = sb(nc, "a_xs", [128, D], BF16)
    junk = sb(nc, "a_junk", [128, D], BF16)
    hT = sb(nc, "a_hT", [128, 8, 128], BF16)
    sq = sb(nc, "a_sq", [128, 640], F32)
    qtmp = sb(nc, "a_qtmp", [128, 640], F32)
    qn = sb(nc, "a_qn", [128, 640], BF16)
    ssq = sb(nc, "a_ssq", [128, 10], F32)
    rq = sb(nc, "a_rq", [128, 10], F32)
    ss = sb(nc, "a_ss", [128, 1], F32)
    lnv = sb(nc, "a_lnv", [128, 1], F32)
    rstd = sb(nc, "a_rstd", [128, 1], F32)
    qaT = sb(nc, "a_qaT", [64, 8, 128], BF16)
    qbT = sb(nc, "a_qbT", [128, 4, 128], BF16)
    qbTn = sb(nc, "a_qbTn", [128, 4, 128], BF16)
    PT = [sb(nc, f"a_PT{i}", [128, 2, 2, 128], BF16) for i in range(2)]
    dn = sb(nc, "a_dn", [128, 4, 1], F32)
    ya = sb(nc, "a_ya", [128, 512], BF16)
    yaT = [sb(nc, f"a_yaT{i}", [128, 4, 128], BF16) for i in range(2)]
    ybT = [sb(nc, f"a_ybT{i}", [64, 8, 128], BF16) for i in range(2)]
    Eb = [sb(nc, f"a_E{i}", [128, 512], F32) for i in range(2)]
    Lb = [sb(nc, f"a_L{i}", [128, 512], BF16) for i in range(2)]
    Wb = [sb(nc, f"a_W{i}", [128, 512], BF16) for i in range(2)]
    Sb = sb(nc, "a_S", [128, 17, 128], BF16)
    TR = ps(nc, "a_TR", [128, 1024], BF16)
    PSR = [ps(nc, f"a_PS{i}", [128, 512], F32) for i in range(4)]
    OUT = [ps(nc, f"a_OUT{i}", [128, 512], F32) for i in range(2)]
    cin = [sb(nc, f"a_cin{i}", [128, 2048], F32) for i in range(2)]
    cout = [sb(nc, f"a_cout{i}", [128, 2048], BF16) for i in range(2)]
    r_cin = [R("cin0"), R("cin1")]
    r_cout = [R("cout0"), R("cout1")]
    conv_state = {"k": 0}

    def convert_chunks(n):
        for _ in range(n):
            k = conv_state["k"]
            if k >= 128:
                return
            conv_state["k"] = k + 1
            src, dst = (dr["u"], dr["ub"]) if k < 64 else (dr["v"], dr["vb"])
            r0 = (k % 64) * 256
            s_ap = src[r0:r0 + 256, :].rearrange("(p i) d -> p (i d)", i=2)
            d_ap = dst[r0:r0 + 256, :].rearrange("(p i) d -> p (i d)", i=2)
            bi = k % 2
            P.dma("pool", lambda e, s_ap=s_ap, bi=bi: e.dma_start(out=cin[bi][:], in_=s_ap), writes=[r_cin[bi]])
            P.op("pool", lambda e, bi=bi: e.tensor_copy(out=cout[bi][:], in_=cin[bi][:]), reads=[r_cin[bi]], writes=[r_cout[bi]])
            P.dma("pool", lambda e, d_ap=d_ap, bi=bi: e.dma_start(out=d_ap, in_=cout[bi][:]), reads=[r_cout[bi]])

    r_win, r_const = R("win"), R("const")
    r_stg = [R("stg0"), R("stg1")]
    r_kaT, r_kbT, r_vb, r_vaug = R("kaT"), R("kbT"), R("vb"), R("vaug")
    r_xa = [R("xa0"), R("xa1")]
    r_xs, r_junk, r_hT, r_sq, r_qtmp, r_qn, r_norm, r_qnorm = (R(n) for n in
                                                               ("xs", "junk", "hT", "sq", "qtmp", "qn", "norm", "qnorm"))
    r_qaT, r_qbT = R("qaT"), R("qbT")
    r_PT = [R("PT0"), R("PT1")]
    r_dn, r_ya = R("dn"), R("ya")
    r_yaT = [R("yaT0"), R("yaT1")]
    r_ybT = [R("ybT0"), R("ybT1")]
    r_S = [R(f"S{k}") for k in range(17)]
    r_TR = R("TR")
    r_OUT = [R("OUT0"), R("OUT1")]
    psr = Ring([(R(f"PS{i}"), PSR[i]) for i in range(4)])
    ering = Ring([(R(f"E{i}"), Eb[i]) for i in range(2)])
    lring = Ring([(R(f"L{i}"), Lb[i]) for i in range(2)])
    wring = Ring([(R(f"W{i}"), Wb[i]) for i in range(2)])

    P.dma("sp", lambda e: e.dma_start(out=g1col[:], in_=dr["g1col"]), writes=[r_const])
    P.dma("sp", lambda e: e.dma_start(out=gainqk[:], in_=dr["gainqk"]), writes=[r_const])
    P.dma("sp", lambda e: e.dma_start(out=esink[:], in_=dr["sinks_bc"]), writes=[r_const])
    P.dma("sp", lambda e: e.dma_start(out=cstf[:], in_=dr["cst"][0:5].rearrange("k p n -> p k n")), writes=[r_const])
    P.dma("sp", lambda e: e.dma_start(out=swabf[:], in_=dr["swab"]), writes=[r_const])
    P.op("act", lambda e: e.activation(out=esink[:], in_=esink[:], func=AF.Exp), reads=[r_const], writes=[r_const])
    P.op("dve", lambda e: e.tensor_copy(out=cstb[:], in_=cstf[:]), reads=[r_const], writes=[r_const])
    P.op("dve", lambda e: e.tensor_copy(out=swab[:].rearrange("p a b c -> p (a b c)"), in_=swabf[:]),
         reads=[r_const], writes=[r_const])
    P.op("dve", lambda e: e.memset(epsc[:], 1e-6), writes=[r_const])
    P.op("dve", lambda e: e.memset(vaug[:], 1.0), writes=[r_vaug])
    k = 0
    for kc in range(8):
        for hf in range(2):
            s = k % 2
            k += 1
            P.dma("sp", lambda e, kc=kc, hf=hf, s=s: e.dma_start(
                out=stg[s][:], in_=dr["w_in"][kc * 128:(kc + 1) * 128, hf * 1152:(hf + 1) * 1152]), writes=[r_stg[s]])
            P.op("dve", lambda e, kc=kc, hf=hf, s=s: e.tensor_scalar(
                out=win[:, kc, hf * 1152:(hf + 1) * 1152], in0=stg[s][:], scalar1=g1col[:, kc:kc + 1], scalar2=None,
                op0=ALU.mult), reads=[r_stg[s], r_const], writes=[r_win])

    def front(sq_i, tt):
        gt = sq_i * NT + tt
        b = gt % 2
        xap = dr["x"][gt * 128:(gt + 1) * 128, :]
        P.dma("sp", lambda e: e.dma_start(out=xa[b][:], in_=xap), writes=[r_xa[b]])
        P.op("act", lambda e: e.activation(out=junk[:], in_=xa[b][:], func=AF.Square, accum_out=ss[:]),
             reads=[r_xa[b]], writes=[r_junk, r_norm])
        P.op("act", lambda e: e.activation(out=lnv[:], in_=ss[:], func=AF.Ln, scale=1.0 / D, bias=epsc[:]),
             reads=[r_norm, r_const], writes=[r_norm])
        P.op("act", lambda e: e.activation(out=rstd[:], in_=lnv[:], func=AF.Exp, scale=-0.5), reads=[r_norm], writes=[r_norm])
        P.op("dve", lambda e: e.tensor_scalar(out=xs[:], in0=xa[b][:], scalar1=rstd[:], scalar2=None, op0=ALU.mult),
             reads=[r_xa[b], r_norm], writes=[r_xs])
        for kc in range(8):
            P.op("pe", lambda e, kc=kc: e.transpose(out=TR[:, kc * 128:(kc + 1) * 128], in_=xs[:, kc * 128:(kc + 1) * 128],
                                                    identity=identb), reads=[r_xs, r_const], writes=[r_TR])
        P.op("act", lambda e: e.activation(out=hT[:].rearrange("p a b -> p (a b)"), in_=TR[:], func=AF.Copy),
             reads=[r_TR], writes=[r_hT])
        rA1, A1 = psr.get()
        rA2, A2 = psr.get()
        rA3, A3 = psr.get()
        for (rr, bank, c0, n) in ((rA1, A1, 0, 512), (rA2, A2, 512, 256), (rA3, A3, 1792, 512)):
            for kc in range(8):
                P.op("pe", lambda e, bank=bank, c0=c0, n=n, kc=kc: e.matmul(
                    bank[:, 0:n], lhsT=hT[:, kc, :], rhs=win[:, kc, c0:c0 + n], start=(kc == 0), stop=(kc == 7)),
                    reads=[r_hT, r_win], writes=[rr])
        P.op("act", lambda e: e.activation(out=vb[:, tt, :], in_=A3[:, 0:512], func=AF.Copy), reads=[rA3], writes=[r_vb])
        release(rA3)
        P.op("act", lambda e: e.activation(out=vaug[:, tt, :, 0:64], in_=A2[:, 128:256].rearrange("p (g d) -> p g d", d=64),
                                           func=AF.Copy), reads=[rA2], writes=[r_vaug])
        P.op("act", lambda e: e.activation(out=sq[:, 0:512], in_=A1[:, 0:512], func=AF.Square), reads=[rA1], writes=[r_sq])
        P.op("act", lambda e: e.activation(out=sq[:, 512:640], in_=A2[:, 0:128], func=AF.Square), reads=[rA2], writes=[r_sq])
        P.op("dve", lambda e: e.tensor_reduce(out=ssq[:], in_=sq[:].rearrange("p (h d) -> p h d", d=64), axis=AX.X, op=ALU.add),
             reads=[r_sq], writes=[r_qnorm])
        P.op("act", lambda e: e.activation(out=rq[:], in_=ssq[:], func=AF.Ln, scale=1.0 / 64, bias=epsc[:]),
             reads=[r_qnorm, r_const], writes=[r_qnorm])
        P.op("act", lambda e: e.activation(out=rq[:], in_=rq[:], func=AF.Exp, scale=-0.5), reads=[r_qnorm], writes=[r_qnorm])
        P.op("dve", lambda e: e.tensor_tensor(out=qtmp[:, 0:512].rearrange("p (h d) -> p h d", d=64),
                                              in0=A1[:, 0:512].rearrange("p (h d) -> p h d", d=64),
                                              in1=rq[:, 0:8].unsqueeze(2).to_broadcast([128, 8, 64]), op=ALU.mult),
             reads=[rA1, r_qnorm], writes=[r_qtmp])
        P.op("dve", lambda e: e.tensor_tensor(out=qtmp[:, 512:640].rearrange("p (h d) -> p h d", d=64),
                                              in0=A2[:, 0:128].rearrange("p (h d) -> p h d", d=64),
                                              in1=rq[:, 8:10].unsqueeze(2).to_broadcast([128, 2, 64]), op=ALU.mult),
             reads=[rA2, r_qnorm], writes=[r_qtmp])
        release(rA1)
        release(rA2)
        P.op("dve", lambda e: e.tensor_tensor(out=qn[:], in0=qtmp[:], in1=gainqk[:], op=ALU.mult),
             reads=[r_qtmp, r_const], writes=[r_qn])
        for h in range(8):
            P.op("pe", lambda e, h=h: e.transpose(out=TR[0:64, h * 128:(h + 1) * 128], in_=qn[:, h * 64:(h + 1) * 64],
                                                  identity=identb), reads=[r_qn, r_const], writes=[r_TR])
        P.op("act", lambda e: e.activation(out=qaT[:].rearrange("p a b -> p (a b)"), in_=TR[0:64, 0:1024], func=AF.Copy),
             reads=[r_TR], writes=[r_qaT])

    def front_k(tt):
        for g in range(2):
            P.op("pe", lambda e, g=g: e.transpose(out=TR[0:64, g * 128:(g + 1) * 128], in_=qn[:, 512 + g * 64:512 + (g + 1) * 64],
                                                  identity=identb), reads=[r_qn, r_const], writes=[r_TR])
        P.op("act", lambda e: e.activation(out=kaT[:, :, tt * 128:(tt + 1) * 128],
                                           in_=TR[0:64, 0:256].rearrange("p (g t) -> p g t", t=128), func=AF.Copy),
             reads=[r_TR], writes=[r_kaT])

    def front_b(tt):
        for half, c0 in ((0, 768), (1, 1280)):
            rB, B = psr.get()
            for c in range(4):
                for kc in range(8):
                    P.op("pe", lambda e, B=B, c=c, kc=kc, c0=c0: e.matmul(
                        B[:, c * 128:(c + 1) * 128], lhsT=win[:, kc, c0 + c * 128:c0 + (c + 1) * 128], rhs=hT[:, kc, :],
                        start=(kc == 0), stop=(kc == 7)), reads=[r_win, r_hT], writes=[rB])
            if half == 0:
                P.op("act", lambda e, B=B: e.activation(out=qbT[:].rearrange("p a b -> p (a b)"), in_=B[:], func=AF.Copy,
                                                        scale=0.125), reads=[rB], writes=[r_qbT])
                P.op("act", lambda e, B=B: e.activation(out=qbTn[:].rearrange("p a b -> p (a b)"), in_=B[:], func=AF.Copy,
                                                        scale=-0.125), reads=[rB], writes=[r_qbT])
            else:
                P.op("act", lambda e, B=B: e.activation(out=kbT[:, :, tt * 128:(tt + 1) * 128],
                                                        in_=B[:].rearrange("p (c t) -> p c t", t=128), func=AF.Copy),
                     reads=[rB], writes=[r_kbT])
            release(rB)

    def swa(gt, tt):
        yb = gt % 2
        kts = [1] if tt == 0 else [0, 1]
        rOA = OAb = None
        for hb in range(4):
            g = hb // 2
            rST, ST = psr.get()
            ptb = hb % 2
            for hh in range(2):
                h = 2 * hb + hh
                for kt in kts:
                    ktile = tt - 1 + kt
                    o = (hh * 2 + kt) * 128
                    P.op("pe", lambda e, ST=ST, o=o, g=g, h=h, ktile=ktile: e.matmul(
                        ST[:, o:o + 128], lhsT=kaT[0:64, g, ktile * 128:(ktile + 1) * 128], rhs=qaT[0:64, h, :],
                        start=True, stop=False), reads=[r_kaT, r_qaT], writes=[rST])
                    P.op("pe", lambda e, ST=ST, o=o, h=h, kt=kt: e.matmul(
                        ST[:, o:o + 128], lhsT=identb, rhs=swab[:, h, kt, :], start=False, stop=True),
                        reads=[r_const], writes=[rST])
            if tt == 0:
                for hh in range(2):
                    o = (hh * 2 + 1) * 128
                    P.op("act", lambda e, ST=ST, o=o, hh=hh, ptb=ptb: e.activation(
                        out=PT[ptb][:, hh, 1, :], in_=ST[:, o:o + 128], func=AF.Exp, scale=0.125),
                        reads=[rST], writes=[r_PT[ptb]])
            else:
                P.op("act", lambda e, ST=ST, ptb=ptb: e.activation(
                    out=PT[ptb][:].rearrange("p a b c -> p (a b c)"), in_=ST[:], func=AF.Exp, scale=0.125),
                    reads=[rST], writes=[r_PT[ptb]])
            release(rST)
            if hb % 2 == 0:
                rOA, OAb = psr.get()
            for hh in range(2):
                h = 2 * hb + hh
                o = (h % 4) * 65
                for ki, kt in enumerate(kts):
                    ktile = tt - 1 + kt
                    P.op("pe", lambda e, OAb=OAb, o=o, hh=hh, kt=kt, ktile=ktile, g=g, ptb=ptb, ki=ki: e.matmul(
                        OAb[:, o:o + 65], lhsT=PT[ptb][:, hh, kt, :], rhs=vaug[:, ktile, g, :],
                        start=(ki == 0), stop=(ki == len(kts) - 1)), reads=[r_PT[ptb], r_vaug], writes=[rOA])
            if hb % 2 == 1:
                hs = (hb // 2) * 4
                oav = OAb[:, 0:260].rearrange("p (h d) -> p h d", d=65)
                P.op("dve", lambda e, oav=oav, hs=hs: e.tensor_tensor(
                    out=dn[:], in0=oav[:, :, 64:65], in1=esink[:, hs:hs + 4].unsqueeze(2), op=ALU.add),
                    reads=[rOA, r_const], writes=[r_dn])
                P.op("dve", lambda e: e.reciprocal(out=dn[:], in_=dn[:]), reads=[r_dn], writes=[r_dn])
                P.op("dve", lambda e, oav=oav, hs=hs: e.tensor_tensor(
                    out=ya[:, hs * 64:(hs + 4) * 64].rearrange("p (h d) -> p h d", d=64), in0=oav[:, :, 0:64],
                    in1=dn[:].to_broadcast([128, 4, 64]), op=ALU.mult), reads=[rOA, r_dn], writes=[r_ya])
                release(rOA)
        for c in range(4):
            P.op("pe", lambda e, c=c: e.transpose(out=TR[:, c * 128:(c + 1) * 128], in_=ya[:, c * 128:(c + 1) * 128],
                                                  identity=identb), reads=[r_ya, r_const], writes=[r_TR])
        P.op("act", lambda e: e.activation(out=yaT[yb][:].rearrange("p a b -> p (a b)"), in_=TR[:, 0:512], func=AF.Copy),
             reads=[r_TR], writes=[r_yaT[yb]])
        P.dma("sp", lambda e: e.dma_start(out=dr["ysa"][gt], in_=yaT[yb][:].rearrange("p a b -> p (a b)")),
              reads=[r_yaT[yb]])

    def sbattn(gt, tt):
        yb = gt % 2
        for h in range(8):
            c = h // 2
            pb = (h % 2) * 64
            OUTb, rOUT = OUT[h % 2], r_OUT[h % 2]
            kbs = list(range(tt, -1, -1))
            batches = [kbs[i:i + BATCH] for i in range(0, len(kbs), BATCH)]
            nmm = len(kbs)
            mmi = 0
            for batch in batches:
                nb = len(batch)
                rZ, Z = psr.get()
                for b, kb in enumerate(batch):
                    P.op("pe", lambda e, Z=Z, b=b, kb=kb, c=c, pb=pb: e.matmul(
                        Z[:, b * 128:(b + 1) * 128], lhsT=kbT[pb:pb + 64, c, kb * 128:(kb + 1) * 128],
                        rhs=qbT[pb:pb + 64, c, :], start=True, stop=True), reads=[r_kbT, r_qbT], writes=[rZ])
                rE, E = ering.get()
                P.op("act", lambda e, Z=Z, E=E, nb=nb: e.activation(out=E[:, 0:nb * 128], in_=Z[:, 0:nb * 128], func=AF.Exp),
                     reads=[rZ], writes=[rE])
                release(rZ)
                rL, L = lring.get()
                P.op("act", lambda e, E=E, L=L, nb=nb: e.activation(out=L[:, 0:nb * 128], in_=E[:, 0:nb * 128], func=AF.Ln,
                                                                     bias=1.0), reads=[rE], writes=[rL])
                release(rE)
                for b, kb in enumerate(batch):
                    if kb == tt:
                        P.op("dve", lambda e, L=L, b=b: e.tensor_tensor(out=Sb[:, tt, :], in0=L[:, b * 128:(b + 1) * 128],
                                                                        in1=maskLb, op=ALU.mult),
                             reads=[rL, r_const], writes=[r_S[tt]])
                    elif kb >= 1:
                        P.op("dve", lambda e, L=L, b=b, kb=kb: e.tensor_tensor(out=Sb[:, kb, :], in0=Sb[:, kb + 1, :],
                                                                               in1=L[:, b * 128:(b + 1) * 128], op=ALU.add),
                             reads=[rL, r_S[kb + 1]], writes=[r_S[kb]])
                rC, C = psr.get()
                for b, kb in enumerate(batch):
                    o = b * 128
                    if kb == tt:
                        P.op("pe", lambda e, C=C, o=o: e.matmul(C[:, o:o + 128], lhsT=trib, rhs=Sb[:, tt, :], start=True, stop=False),
                             reads=[r_const, r_S[tt]], writes=[rC])
                    else:
                        P.op("pe", lambda e, C=C, o=o, L=L: e.matmul(C[:, o:o + 128], lhsT=trib, rhs=L[:, o:o + 128],
                                                                     start=True, stop=False), reads=[r_const, rL], writes=[rC])
                        P.op("pe", lambda e, C=C, o=o, kb=kb: e.matmul(C[:, o:o + 128], lhsT=onesb, rhs=Sb[:, kb + 1, :],
                                                                       start=False, stop=False),
                             reads=[r_const, r_S[kb + 1]], writes=[rC])
                    P.op("pe", lambda e, C=C, o=o, kb=kb, c=c, pb=pb: e.matmul(
                        C[:, o:o + 128], lhsT=kbT[pb:pb + 64, c, kb * 128:(kb + 1) * 128], rhs=qbTn[pb:pb + 64, c, :],
                        start=False, stop=(kb != tt)), reads=[r_kbT, r_qbT], writes=[rC])
                    if kb == tt:
                        P.op("pe", lambda e, C=C, o=o: e.matmul(C[:, o:o + 128], lhsT=identb, rhs=maskbb, start=False, stop=True),
                             reads=[r_const], writes=[rC])
                release(rL)
                rW, W = wring.get()
                P.op("act", lambda e, C=C, W=W, nb=nb: e.activation(out=W[:, 0:nb * 128], in_=C[:, 0:nb * 128], func=AF.Exp,
                                                                     scale=-1.0), reads=[rC], writes=[rW])
                release(rC)
                for b, kb in enumerate(batch):
                    P.op("pe", lambda e, W=W, b=b, kb=kb, h=h, OUTb=OUTb, mmi=mmi: e.matmul(
                        OUTb[0:64, 0:128], lhsT=vb[:, kb, h * 64:(h + 1) * 64], rhs=W[:, b * 128:(b + 1) * 128],
                        start=(mmi == 0), stop=(mmi == nmm - 1)), reads=[r_vb, rW], writes=[rOUT])
                    mmi += 1
                release(rW)
            P.op("act", lambda e, OUTb=OUTb, h=h: e.activation(out=ybT[yb][:, h, :], in_=OUTb[0:64, 0:128], func=AF.Copy),
                 reads=[rOUT], writes=[r_ybT[yb]])
        P.dma("sp", lambda e: e.dma_start(out=dr["ysb"][gt], in_=ybT[yb][:].rearrange("p a b -> p (a b)")),
              reads=[r_ybT[yb]])

    for sq_i in range(nseq):
        for tt in range(NT):
            gt = sq_i * NT + tt
            convert_chunks((128 + nseq * NT - 1) // (nseq * NT))
            front(sq_i, tt)
            front_k(tt)
            front_b(tt)
            if tt not in SKIP_SWA:
                swa(gt, tt)
            if tt not in SKIP_SB:
                sbattn(gt, tt)
            if tt in SKIP_SWA:
                pass


def phase_a2(P, nc, dr, ntiles):
    R = lambda n: Res(n)
    wg = sb(nc, "m_wg", [128, 8, 2048], BF16)
    wupa = sb(nc, "m_wupa", [128, 4, 1024], BF16)
    wupb = sb(nc, "m_wupb", [64, 8, 1024], BF16)
    wout = sb(nc, "m_wout", [128, 8, 1024], BF16)
    stg = [sb(nc, f"m_stg{i}", [128, 1024], F32) for i in range(2)]
    g1col = sb(nc, "m_g1col", [128, 8], F32)
    ngb = sb(nc, "m_ngb", [128, 16], F32)
    identf = sb(nc, "m_identf", [128, 128], F32)
    identb = sb(nc, "m_identb", [128, 128], BF16)
    epsc = sb(nc, "m_eps", [128, 1], F32)
    xa = [sb(nc, f"m_xa{i}", [128, D], F32) for i in range(2)]
    xs = sb(nc, "m_xs", [128, D], BF16)
    junk = sb(nc, "m_junk", [128, D], BF16)
    hT = sb(nc, "m_hT", [128, 8, 128], BF16)
    ss = sb(nc, "m_ss", [128, 1], F32)
    lnv = sb(nc, "m_lnv", [128, 1], F32)
    rstd = sb(nc, "m_rstd", [128, 1], F32)
    yaT = [sb(nc, f"m_yaT{i}", [128, 4, 128], BF16) for i in range(2)]
    ybT = [sb(nc, f"m_ybT{i}", [64, 8, 128], BF16) for i in range(2)]
    eg = [sb(nc, f"m_eg{i}", [128, 2, 128], F32) for i in range(2)]
    tmp = [sb(nc, f"m_tmp{i}", [128, 2, 128], F32) for i in range(2)]
    mT = sb(nc, "m_mT", [128, 8, 128], BF16)
    x1 = [sb(nc, f"m_x1{i}", [128, D], F32) for i in range(2)]
    TR = ps(nc, "m_TR", [128, 1024], BF16)
    PSR = [ps(nc, f"m_PS{i}", [128, 512], F32) for i in range(6)]

    r_w, r_const = R("w"), R("const")
    r_stg = [R("stg0"), R("stg1")]
    r_xa = [R("xa0"), R("xa1")]
    r_xs, r_junk, r_hT, r_norm, r_mT, r_TR = (R(n) for n in ("xs", "junk", "hT", "norm", "mT", "TR"))
    r_yaT = [R("yaT0"), R("yaT1")]
    r_ybT = [R("ybT0"), R("ybT1")]
    r_eg = [R("eg0"), R("eg1")]
    r_tmp = [R("tmp0"), R("tmp1")]
    r_x1 = [R("x10"), R("x11")]
    psr = Ring([(R(f"PS{i}"), PSR[i]) for i in range(6)])

    P.dma("sp", lambda e: e.dma_start(out=g1col[:], in_=dr["g1col"]), writes=[r_const])
    P.dma("sp", lambda e: e.dma_start(out=ngb[:], in_=dr["gbcol"]), writes=[r_const])
    P.dma("sp", lambda e: e.dma_start(out=identf[:], in_=dr["ident"]), writes=[r_const])
    P.op("dve", lambda e: e.tensor_copy(out=identb[:], in_=identf[:]), reads=[r_const], writes=[r_const])
    P.op("dve", lambda e: e.tensor_scalar(out=ngb[:], in0=ngb[:], scalar1=-1.0, scalar2=None, op0=ALU.mult),
         reads=[r_const], writes=[r_const])
    P.op("dve", lambda e: e.memset(epsc[:], 1e-6), writes=[r_const])
    k = 0

    def load(dst_ap, src_ap, npart, scalar=None):
        nonlocal k
        s = k % 2
        k += 1
        P.dma("sp", lambda e: e.dma_start(out=stg[s][0:npart, :], in_=src_ap), writes=[r_stg[s]])
        if scalar is None:
            P.op("dve", lambda e: e.tensor_copy(out=dst_ap, in_=stg[s][0:npart, :]), reads=[r_stg[s]], writes=[r_w])
        else:
            P.op("dve", lambda e: e.tensor_scalar(out=dst_ap, in0=stg[s][0:npart, :], scalar1=scalar, scalar2=None,
                                                  op0=ALU.mult), reads=[r_stg[s], r_const], writes=[r_w])
    for kc in range(8):
        for hf in range(2):
            load(wg[:, kc, hf * 1024:(hf + 1) * 1024], dr["w_in"][kc * 128:(kc + 1) * 128, 2304 + hf * 1024:2304 + (hf + 1) * 1024],
                 128, g1col[:, kc:kc + 1])
    for i in range(4):
        load(wupa[:, i, :], dr["w_up_swa"][i * 128:(i + 1) * 128, :], 128)
    for h in range(8):
        load(wupb[:, h, :], dr["w_up_sb"][h * 64:(h + 1) * 64, :], 64)
    for dc in range(8):
        load(wout[:, dc, :], dr["w_out"][dc * 128:(dc + 1) * 128, :], 128)

    toks = []
    for gt in range(ntiles):
        b = gt % 2
        xap = dr["x"][gt * 128:(gt + 1) * 128, :]
        P.dma("sp", lambda e, xap=xap, b=b: e.dma_start(out=xa[b][:], in_=xap), writes=[r_xa[b]])
        P.dma("sp", lambda e, gt=gt, b=b: e.dma_start(out=yaT[b][:].rearrange("p a b -> p (a b)"), in_=dr["ysa"][gt]),
              writes=[r_yaT[b]])
        P.dma("sp", lambda e, gt=gt, b=b: e.dma_start(out=ybT[b][:].rearrange("p a b -> p (a b)"), in_=dr["ysb"][gt]),
              writes=[r_ybT[b]])
        P.op("act", lambda e, b=b: e.activation(out=junk[:], in_=xa[b][:], func=AF.Square, accum_out=ss[:]),
             reads=[r_xa[b]], writes=[r_junk, r_norm])
        P.op("act", lambda e: e.activation(out=lnv[:], in_=ss[:], func=AF.Ln, scale=1.0 / D, bias=epsc[:]),
             reads=[r_norm, r_const], writes=[r_norm])
        P.op("act", lambda e: e.activation(out=rstd[:], in_=lnv[:], func=AF.Exp, scale=-0.5), reads=[r_norm], writes=[r_norm])
        P.op("dve", lambda e, b=b: e.tensor_scalar(out=xs[:], in0=xa[b][:], scalar1=rstd[:], scalar2=None, op0=ALU.mult),
             reads=[r_xa[b], r_norm], writes=[r_xs])
        for kc in range(8):
            P.op("pe", lambda e, kc=kc: e.transpose(out=TR[:, kc * 128:(kc + 1) * 128], in_=xs[:, kc * 128:(kc + 1) * 128],
                                                    identity=identb[:]), reads=[r_xs, r_const], writes=[r_TR])
        P.op("act", lambda e: e.activation(out=hT[:].rearrange("p a b -> p (a b)"), in_=TR[:], func=AF.Copy),
             reads=[r_TR], writes=[r_hT])
        for dc in range(8):
            rM, M = psr.get()
            cs = slice(dc * 128, (dc + 1) * 128)
            for i in range(4):
                P.op("pe", lambda e, M=M, i=i, cs=cs, b=b: e.matmul(M[:, 0:128], lhsT=wupa[:, i, cs], rhs=yaT[b][:, i, :],
                                                                   start=(i == 0), stop=(i == 3)),
                     reads=[r_w, r_yaT[b]], writes=[rM])
            for h in range(8):
                P.op("pe", lambda e, M=M, h=h, cs=cs, b=b: e.matmul(M[:, 128:256], lhsT=wupb[:, h, cs], rhs=ybT[b][:, h, :],
                                                                   start=(h == 0), stop=(h == 7)),
                     reads=[r_w, r_ybT[b]], writes=[rM])
            for gi in range(2):
                gs = slice(gi * 1024 + dc * 128, gi * 1024 + (dc + 1) * 128)
                for kc in range(8):
                    P.op("pe", lambda e, M=M, gi=gi, gs=gs, kc=kc: e.matmul(M[:, (2 + gi) * 128:(3 + gi) * 128], lhsT=wg[:, kc, gs],
                                                                           rhs=hT[:, kc, :], start=(kc == 0), stop=(kc == 7)),
                         reads=[r_w, r_hT], writes=[rM])
            eb = dc % 2
            for gi in range(2):
                P.op("act", lambda e, M=M, gi=gi, dc=dc, eb=eb: e.activation(
                    out=eg[eb][:, gi, :], in_=M[:, (2 + gi) * 128:(3 + gi) * 128], func=AF.Exp, scale=-1.0,
                    bias=ngb[:, gi * 8 + dc:gi * 8 + dc + 1]), reads=[rM, r_const], writes=[r_eg[eb]])
            P.op("dve", lambda e, eb=eb: e.tensor_scalar(out=eg[eb][:], in0=eg[eb][:], scalar1=1.0, scalar2=None, op0=ALU.add),
                 reads=[r_eg[eb]], writes=[r_eg[eb]])
            P.op("dve", lambda e, eb=eb: e.reciprocal(out=eg[eb][:], in_=eg[eb][:]), reads=[r_eg[eb]], writes=[r_eg[eb]])
            P.op("dve", lambda e, M=M, eb=eb: e.tensor_tensor(out=tmp[eb][:].rearrange("p a b -> p (a b)"),
                                                              in0=eg[eb][:].rearrange("p a b -> p (a b)"), in1=M[:, 0:256],
                                                              op=ALU.mult), reads=[r_eg[eb], rM], writes=[r_tmp[eb]])
            release(rM)
            P.op("dve", lambda e, eb=eb, dc=dc: e.tensor_tensor(out=mT[:, dc, :], in0=tmp[eb][:, 0, :], in1=tmp[eb][:, 1, :],
                                                                op=ALU.add), reads=[r_tmp[eb]], writes=[r_mT])
        for hf in range(2):
            rO, O = psr.get()
            for dc in range(8):
                P.op("pe", lambda e, O=O, dc=dc, hf=hf: e.matmul(O[:, 0:512], lhsT=mT[:, dc, :],
                                                                rhs=wout[:, dc, hf * 512:(hf + 1) * 512],
                                                                start=(dc == 0), stop=(dc == 7)), reads=[r_mT, r_w], writes=[rO])
            P.op("dve", lambda e, O=O, hf=hf, b=b: e.tensor_tensor(out=x1[b][:, hf * 512:(hf + 1) * 512],
                                                                   in0=xa[b][:, hf * 512:(hf + 1) * 512], in1=O[:, 0:512],
                                                                   op=ALU.add), reads=[r_xa[b], rO], writes=[r_x1[b]])
            release(rO)
        yap = dr["y"][gt * 128:(gt + 1) * 128, :]
        toks.append(P.dma("sp", lambda e, yap=yap, b=b: e.dma_start(out=yap, in_=x1[b][:]), reads=[r_x1[b]]))
    return toks


NG = 12


def phase_b(P, nc, dr, ntiles, x1_res):
    wq = sb(nc, "b_wq", [128, 8, 2048], BF16)
    skT = sb(nc, "b_skT", [128, 16, 128], F32)
    g2bc = sb(nc, "b_g2bc", [128, D], F32)
    g2col = sb(nc, "b_g2col", [128, 8], F32)
    iota16 = sb(nc, "b_iota", [128, 16], F32)
    identf = sb(nc, "b_identf", [128, 128], F32)
    identb = sb(nc, "b_identb", [128, 128], BF16)
    epsc = sb(nc, "b_eps", [128, 1], F32)
    stg = [sb(nc, f"b_stg{i}", [128, 2048], F32) for i in range(2)]
    xt = [sb(nc, f"b_xt{i}", [128, D], F32) for i in range(2)]
    xs = sb(nc, "b_xs", [128, D], BF16)
    junk = sb(nc, "b_junk", [128, D], BF16)
    h2 = [sb(nc, f"b_h2{i}", [128, D], F32) for i in range(2)]
    h2T = sb(nc, "b_h2T", [128, 8, 128], BF16)
    qT = sb(nc, "b_qT", [128, 16, 128], F32)
    sc = sb(nc, "b_sc", [128, 16, 128], F32)
    sc2 = sb(nc, "b_sc2", [128, 16, 128], F32)
    cand = sb(nc, "b_cand", [128, 8, 256], F32)
    cand2 = sb(nc, "b_cand2", [128, 8, 256], F32)
    oh = sb(nc, "b_oh", [128, 8, 16, 16], F32)
    tops = sb(nc, "b_tops", [128, 16, 16], F32)
    topi = sb(nc, "b_topi", [128, 16, 16], U32)
    topif = sb(nc, "b_topif", [128, 16, 16], F32)
    bests = sb(nc, "b_bests", [128, 8, 16], F32)
    bestp = sb(nc, "b_bestp", [128, 8, 16], U32)
    hiu = sb(nc, "b_hiu", [128, 8, 16], U32)
    lou = sb(nc, "b_lou", [128, 8, 16], U32)
    hif = sb(nc, "b_hif", [128, 8, 16], F32)
    lof = sb(nc, "b_lof", [128, 8, 16], F32)
    i0s = sb(nc, "b_i0s", [128, 8, 16], F32)
    i1s = sb(nc, "b_i1s", [128, 8, 16], F32)
    expf = sb(nc, "b_expf", [128, 128], F32)
    idx = [sb(nc, f"b_idx{i}", [128, 128], I32) for i in range(2)]
    gd = sb(nc, "b_gd", [128, 8, 16], F32)
    gsum = sb(nc, "b_gsum", [128, 8], F32)
    gate = [sb(nc, f"b_gate{i}", [128, 8, 16], F32) for i in range(2)]
    dots = sb(nc, "b_dots", [128, 128], F32)
    act = sb(nc, "b_act", [128, 128], F32)
    wgt = sb(nc, "b_wgt", [128, 128], F32)
    ss = sb(nc, "b_ss", [128, 1], F32)
    lnv = sb(nc, "b_lnv", [128, 1], F32)
    rstd = sb(nc, "b_rstd", [128, 1], F32)
    G = [sb(nc, f"b_G{i}", [128, D], BF16) for i in range(NG)]
    acc = [sb(nc, f"b_acc{i}", [128, D], F32) for i in range(2)]
    scl = [sb(nc, f"b_scl{i}", [128, D], BF16) for i in range(4)]
    r_scl = [Res(f"scl{i}") for i in range(4)]
    ACC = [ps(nc, f"b_ACC{i}", [128, 512], F32) for i in range(2)]
    r_ACC = Res("ACC")
    TR = ps(nc, "b_TR", [128, 1024], BF16)
    PS = [ps(nc, f"b_PS{i}", [128, 512], F32) for i in range(4)]

    R = lambda n: Res(n)
    r_wq, r_skT, r_const = R("wq"), R("skT"), R("const")
    r_stg = [R("stg0"), R("stg1")]
    r_xt = [R("xt0"), R("xt1")]
    r_xs, r_junk, r_h2T, r_qT, r_sc, r_sc2 = R("xs"), R("junk"), R("h2T"), R("qT"), R("sc"), R("sc2")
    r_h2 = [R("h2a"), R("h2b")]
    r_cand, r_cand2, r_oh, r_top, r_best, r_sel = R("cand"), R("cand2"), R("oh"), R("top"), R("best"), R("sel")
    r_idx = [R("idx0"), R("idx1")]
    r_gate = [R("gate0"), R("gate1")]
    r_gtmp = R("gtmp")
    r_dots, r_wgt, r_norm = R("dots"), R("wgt"), R("norm")
    r_G = [R(f"G{i}") for i in range(NG)]
    r_acc = [R("acc0"), R("acc1")]
    r_TR = R("TR")
    r_PS = [R(f"PS{i}") for i in range(4)]
    r_y = [R(f"y{i}") for i in range(ntiles)]

    P.dma("sp", lambda e: e.dma_start(out=skT[:], in_=dr["skT"]), writes=[r_skT])
    P.dma("sp", lambda e: e.dma_start(out=g2bc[:], in_=dr["g2bc"]), writes=[r_const])
    P.dma("sp", lambda e: e.dma_start(out=g2col[:], in_=dr["g2col"]), writes=[r_const])
    P.dma("sp", lambda e: e.dma_start(out=iota16[:], in_=dr["iota16"]), writes=[r_const])
    P.dma("sp", lambda e: e.dma_start(out=identf[:], in_=dr["ident"]), writes=[r_const])
    P.op("dve", lambda e: e.tensor_copy(out=identb[:], in_=identf[:]), reads=[r_const], writes=[r_const])
    P.op("dve", lambda e: e.memset(epsc[:], 1e-6), writes=[r_const])
    for kc in range(8):
        s = kc % 2
        P.dma("sp", lambda e, kc=kc, s=s: e.dma_start(out=stg[s][:], in_=dr["wq"][kc * 128:(kc + 1) * 128, :]),
              writes=[r_stg[s]])
        P.op("dve", lambda e, kc=kc, s=s: e.tensor_scalar(out=wq[:, kc, :], in0=stg[s][:], scalar1=g2col[:, kc:kc + 1],
                                                         scalar2=None, op0=ALU.mult),
             reads=[r_stg[s], r_const], writes=[r_wq])

    def front(i):
        b = i % 2
        x1ap = dr["x1"][i * 128:(i + 1) * 128, :]
        rd = [x1_res[i]] if x1_res is not None else []
        P.dma("sp", lambda e: e.dma_start(out=xt[b][:], in_=x1ap), reads=rd, writes=[r_xt[b]])
        P.op("act", lambda e: e.activation(out=junk[:], in_=xt[b][:], func=AF.Square, accum_out=ss[:]),
             reads=[r_xt[b]], writes=[r_junk, r_norm])
        P.op("act", lambda e: e.activation(out=lnv[:], in_=ss[:], func=AF.Ln, scale=1.0 / D, bias=epsc[:]),
             reads=[r_norm, r_const], writes=[r_norm])
        P.op("act", lambda e: e.activation(out=rstd[:], in_=lnv[:], func=AF.Exp, scale=-0.5),
             reads=[r_norm], writes=[r_norm])
        P.op("dve", lambda e: e.tensor_scalar(out=xs[:], in0=xt[b][:], scalar1=rstd[:], scalar2=None, op0=ALU.mult),
             reads=[r_xt[b], r_norm], writes=[r_xs])
        P.op("dve", lambda e: e.scalar_tensor_tensor(out=h2[b][:], in0=xt[b][:], scalar=rstd[:], in1=g2bc[:],
                                                     op0=ALU.mult, op1=ALU.mult),
             reads=[r_xt[b], r_norm, r_const], writes=[r_h2[b]])
        for kc in range(8):
            P.op("pe", lambda e, kc=kc: e.transpose(out=TR[:, kc * 128:(kc + 1) * 128], in_=xs[:, kc * 128:(kc + 1) * 128],
                                                    identity=identb[:]),
                 reads=[r_xs, r_const], writes=[r_TR])
        P.op("act", lambda e: e.activation(out=h2T[:].rearrange("p a b -> p (a b)"), in_=TR[:], func=AF.Copy),
             reads=[r_TR], writes=[r_h2T])
        for g in range(4):
            for j in range(4):
                hp = g * 4 + j
                for kc in range(8):
                    P.op("pe", lambda e, g=g, j=j, hp=hp, kc=kc: e.matmul(
                        PS[g][:, j * 128:(j + 1) * 128], lhsT=wq[:, kc, hp * 128:(hp + 1) * 128], rhs=h2T[:, kc, :],
                        start=(kc == 0), stop=(kc == 7)),
                        reads=[r_wq, r_h2T], writes=[r_PS[g]])
            P.op("act", lambda e, g=g: e.activation(out=qT[:, g * 4:(g + 1) * 4, :].rearrange("p a b -> p (a b)"),
                                                    in_=PS[g][:], func=AF.Copy),
                 reads=[r_PS[g]], writes=[r_qT])
        for g in range(4):
            for j in range(4):
                hp = g * 4 + j
                P.op("pe", lambda e, g=g, j=j, hp=hp: e.matmul(
                    PS[g][:, j * 128:(j + 1) * 128], lhsT=qT[:, hp, :], rhs=skT[:, hp, :], start=True, stop=True),
                    reads=[r_qT, r_skT], writes=[r_PS[g]])
            P.op("act", lambda e, g=g: e.activation(out=sc[:, g * 4:(g + 1) * 4, :].rearrange("p a b -> p (a b)"),
                                                    in_=PS[g][:], func=AF.Copy),
                 reads=[r_PS[g]], writes=[r_sc])
        for hp in range(16):
            P.op("dve", lambda e, hp=hp: e.max(out=tops[:, hp, 0:8], in_=sc[:, hp, :]), reads=[r_sc], writes=[r_top])
            P.op("dve", lambda e, hp=hp: e.max_index(out=topi[:, hp, 0:8], in_max=tops[:, hp, 0:8], in_values=sc[:, hp, :]),
                 reads=[r_sc, r_top], writes=[r_top])
            P.op("dve", lambda e, hp=hp: e.match_replace(out=sc2[:, hp, :], in_to_replace=tops[:, hp, 0:8],
                                                         in_values=sc[:, hp, :], imm_value=-1e30),
                 reads=[r_sc, r_top], writes=[r_sc2])
            P.op("dve", lambda e, hp=hp: e.max(out=tops[:, hp, 8:16], in_=sc2[:, hp, :]), reads=[r_sc2], writes=[r_top])
            P.op("dve", lambda e, hp=hp: e.max_index(out=topi[:, hp, 8:16], in_max=tops[:, hp, 8:16], in_values=sc2[:, hp, :]),
                 reads=[r_sc2, r_top], writes=[r_top])
        P.op("dve", lambda e: e.tensor_copy(out=topif[:], in_=topi[:]), reads=[r_top], writes=[r_top])
        tv = tops[:].rearrange("p (h t) k -> p h t k", t=2)
        in0 = tv[:, :, 0, :].unsqueeze(3).to_broadcast([128, 8, 16, 16])
        in1 = tv[:, :, 1, :].unsqueeze(2).to_broadcast([128, 8, 16, 16])
        candv = cand[:].rearrange("p h (i j) -> p h i j", j=16)
        P.op("dve", lambda e: e.tensor_tensor(out=candv, in0=in0, in1=in1, op=ALU.add), reads=[r_top], writes=[r_cand])
        for h in range(8):
            P.op("dve", lambda e, h=h: e.max(out=bests[:, h, 0:8], in_=cand[:, h, :]), reads=[r_cand], writes=[r_best])
            P.op("dve", lambda e, h=h: e.max_index(out=bestp[:, h, 0:8], in_max=bests[:, h, 0:8], in_values=cand[:, h, :]),
                 reads=[r_cand, r_best], writes=[r_best])
            P.op("dve", lambda e, h=h: e.match_replace(out=cand2[:, h, :], in_to_replace=bests[:, h, 0:8],
                                                       in_values=cand[:, h, :], imm_value=-1e30),
                 reads=[r_cand, r_best], writes=[r_cand2])
            P.op("dve", lambda e, h=h: e.max(out=bests[:, h, 8:16], in_=cand2[:, h, :]), reads=[r_cand2], writes=[r_best])
            P.op("dve", lambda e, h=h: e.max_index(out=bestp[:, h, 8:16], in_max=bests[:, h, 8:16], in_values=cand2[:, h, :]),
                 reads=[r_cand2, r_best], writes=[r_best])
        P.op("dve", lambda e: e.tensor_single_scalar(out=hiu[:], in_=bestp[:], scalar=4, op=ALU.logical_shift_right),
             reads=[r_best], writes=[r_sel])
        P.op("dve", lambda e: e.tensor_single_scalar(out=lou[:], in_=bestp[:], scalar=15, op=ALU.bitwise_and),
             reads=[r_best], writes=[r_sel])
        P.op("dve", lambda e: e.tensor_copy(out=hif[:], in_=hiu[:]), reads=[r_sel], writes=[r_sel])
        P.op("dve", lambda e: e.tensor_copy(out=lof[:], in_=lou[:]), reads=[r_sel], writes=[r_sel])
        iota_bc = iota16[:].unsqueeze(1).unsqueeze(1).to_broadcast([128, 8, 16, 16])
        tiv = topif[:].rearrange("p (h t) k -> p h t k", t=2)
        for (selin, tsel, outsel) in ((hif, 0, i0s), (lof, 1, i1s)):
            P.op("dve", lambda e, selin=selin: e.tensor_tensor(
                out=oh[:], in0=iota_bc, in1=selin[:].unsqueeze(3).to_broadcast([128, 8, 16, 16]), op=ALU.is_equal),
                reads=[r_sel, r_const], writes=[r_oh])
            P.op("dve", lambda e, tsel=tsel: e.tensor_tensor(
                out=oh[:], in0=oh[:], in1=tiv[:, :, tsel, :].unsqueeze(2).to_broadcast([128, 8, 16, 16]), op=ALU.mult),
                reads=[r_oh, r_top], writes=[r_oh])
            P.op("dve", lambda e, outsel=outsel: e.tensor_reduce(out=outsel[:], in_=oh[:], axis=AX.X, op=ALU.add),
                 reads=[r_oh], writes=[r_sel])
        P.op("dve", lambda e: e.scalar_tensor_tensor(out=expf[:].rearrange("p (h k) -> p h k", k=16), in0=i0s[:],
                                                     scalar=128.0, in1=i1s[:], op0=ALU.mult, op1=ALU.add),
             reads=[r_sel], writes=[r_sel])
        P.op("dve", lambda e: e.tensor_copy(out=idx[b][:], in_=expf[:]), reads=[r_sel], writes=[r_idx[b]])
        P.op("dve", lambda e: e.tensor_tensor(out=gd[:], in0=bests[:], in1=bests[:, :, 0:1].to_broadcast([128, 8, 16]),
                                              op=ALU.subtract), reads=[r_best], writes=[r_gtmp])
        P.op("act", lambda e: e.activation(out=gd[:], in_=gd[:], func=AF.Exp), reads=[r_gtmp], writes=[r_gtmp])
        P.op("dve", lambda e: e.tensor_reduce(out=gsum[:], in_=gd[:], axis=AX.X, op=ALU.add), reads=[r_gtmp], writes=[r_gtmp])
        P.op("dve", lambda e: e.reciprocal(out=gsum[:], in_=gsum[:]), reads=[r_gtmp], writes=[r_gtmp])
        P.op("dve", lambda e: e.tensor_tensor(out=gate[b][:], in0=gd[:], in1=gsum[:].unsqueeze(2).to_broadcast([128, 8, 16]),
                                              op=ALU.mult), reads=[r_gtmp], writes=[r_gate[b]])

    gring = {"i": 0}

    def back(i):
        b = i % 2
        for j in range(128):
            g = gring["i"] % NG
            gring["i"] += 1
            P.dma("pool", lambda e, g=g, j=j: e.indirect_dma_start(
                out=G[g][:], out_offset=None, in_=dr["ub"],
                in_offset=bass.IndirectOffsetOnAxis(ap=idx[b][:, j:j + 1], axis=0)),
                reads=[r_idx[b]], writes=[r_G[g]])
            P.op("dve", lambda e, g=g, j=j: e.scalar_tensor_tensor(
                out=junk[:], in0=G[g][:], scalar=1.0, in1=h2[b][:], op0=ALU.mult, op1=ALU.mult,
                accum_out=dots[:, j:j + 1]),
                reads=[r_G[g], r_h2[b]], writes=[r_junk, r_dots])
        P.op("act", lambda e: e.activation(out=act[:], in_=dots[:], func=AF.Gelu), reads=[r_dots], writes=[r_wgt])
        P.op("dve", lambda e: e.tensor_tensor(out=wgt[:], in0=act[:], in1=gate[b][:].rearrange("p h k -> p (h k)"),
                                              op=ALU.mult), reads=[r_wgt, r_gate[b]], writes=[r_wgt])
        if i == 0 and "dbg" in dr:
            dbg = dr["dbg"]
            P.dma("sp", lambda e: e.dma_start(out=dbg[0], in_=dots[:]), reads=[r_dots])
            P.dma("sp", lambda e: e.dma_start(out=dbg[1], in_=wgt[:]), reads=[r_wgt])
            P.dma("sp", lambda e: e.dma_start(out=dbg[2], in_=expf[:]), reads=[r_sel])
            P.dma("sp", lambda e: e.dma_start(out=dbg[3], in_=gate[b][:].rearrange("p h k -> p (h k)")), reads=[r_gate[b]])
            P.dma("sp", lambda e: e.dma_start(out=dbg[4], in_=bests[:].rearrange("p h k -> p (h k)")), reads=[r_best])
            P.dma("sp", lambda e: e.dma_start(out=dbg[5], in_=topif[:, 0:8, :].rearrange("p h k -> p (h k)")), reads=[r_top])
            P.dma("sp", lambda e: e.dma_start(out=dbg[6], in_=tops[:, 0:8, :].rearrange("p h k -> p (h k)")), reads=[r_top])
            P.dma("sp", lambda e: e.dma_start(out=dbg[7], in_=sc[:, 0, :]), reads=[r_sc])
        for j in range(128):
            g = gring["i"] % NG
            gring["i"] += 1
            P.dma("pool", lambda e, g=g, j=j: e.indirect_dma_start(
                out=G[g][:], out_offset=None, in_=dr["vb"],
                in_offset=bass.IndirectOffsetOnAxis(ap=idx[b][:, j:j + 1], axis=0)),
                reads=[r_idx[b]], writes=[r_G[g]])
            si = j % 4
            P.op("act", lambda e, g=g, j=j, si=si: e.activation(out=scl[si][:], in_=G[g][:], func=AF.Copy,
                                                               scale=wgt[:, j:j + 1]),
                 reads=[r_G[g], r_wgt], writes=[r_scl[si]])
            for hf in range(2):
                P.op("pe", lambda e, j=j, si=si, hf=hf: e.matmul(ACC[hf][:, 0:512], lhsT=identb[:],
                                                                 rhs=scl[si][:, hf * 512:(hf + 1) * 512],
                                                                 start=(j == 0), stop=(j == 127)),
                     reads=[r_scl[si], r_const], writes=[r_ACC])
        for hf in range(2):
            P.op("dve", lambda e, hf=hf: e.tensor_tensor(out=acc[b][:, hf * 512:(hf + 1) * 512],
                                                         in0=xt[b][:, hf * 512:(hf + 1) * 512], in1=ACC[hf][:, 0:512],
                                                         op=ALU.add), reads=[r_xt[b], r_ACC], writes=[r_acc[b]])
        yap = dr["y"][i * 128:(i + 1) * 128, :]
        wr = [r_y[i]] + ([x1_res[i]] if x1_res is not None else [])
        return P.dma("sp", lambda e: e.dma_start(out=yap, in_=acc[b][:]), reads=[r_acc[b]], writes=wr)

    out_toks = []
    front(0)
    for i in range(ntiles):
        if i + 1 < ntiles:
            front(i + 1)
        out_toks.append(back(i))
    return out_toks


NCORES = 8
SEQ_PER_CORE = 4


def build_program(nseq):
    nc = bass.Bass("TRN2", target_bir_lowering=False)
    nt = nseq * 16

    def din(name, shape, dt=F32):
        return nc.dram_tensor(name, list(shape), dt, kind="ExternalInput").ap()
    dr = {"x": din("x", [nt * 128, 1024]), "w_in": din("w_in", [1024, 4352]), "g1col": din("g1col", [128, 8]),
          "gainqk": din("gainqk", [128, 640]), "sinks_bc": din("sinks_bc", [128, 8]), "cst": din("cst", [5, 128, 128]),
          "swab": din("swab", [128, 2048]), "gbcol": din("gbcol", [128, 16]), "w_up_swa": din("w_up_swa", [512, 1024]),
          "w_up_sb": din("w_up_sb", [512, 1024]), "w_out": din("w_out", [1024, 1024]),
          "wq": din("wq", [1024, 2048]), "g2col": din("g2col", [128, 8]), "g2bc": din("g2bc", [128, 1024]),
          "skT": din("skT", [128, 16, 128]), "u": din("u", [16384, 1024]), "v": din("v", [16384, 1024]),
          "iota16": din("iota16", [128, 16])}
    dr["ident"] = dr["cst"][0]
    dr["y"] = nc.dram_tensor("y", [nt * 128, 1024], F32, kind="ExternalOutput").ap()
    dr["x1"] = dr["y"]
    dr["ysa"] = nc.dram_tensor("ysa", [nt, 128, 512], BF16).ap()
    dr["ysb"] = nc.dram_tensor("ysb", [nt, 64, 1024], BF16).ap()
    dr["ub"] = nc.dram_tensor("ub16", [16384, 1024], BF16).ap()
    dr["vb"] = nc.dram_tensor("vb16", [16384, 1024], BF16).ap()
    P = Prog(nc)
    with ExitStack() as st:
        STACK[0] = st
        phase_a1(P, nc, dr, nseq)
        P.barrier()
        P.replay()
    with ExitStack() as st:
        STACK[0] = st
        phase_a2(P, nc, dr, nt)
        P.barrier()
        P.replay()
    with ExitStack() as st:
        STACK[0] = st
        toks = phase_b(P, nc, dr, nt, None)
        P.wait_tokens("sp", toks)
        P.barrier()
        P.replay()
    return nc


def host_inputs(inputs):
    f = lambda k: np.asarray(inputs[k], np.float32)
    cst, swab = consts()
    g1 = f("mix_norm_gain")[0]
    g2 = f("ffn_norm_gain")[0]
    sk = f("peer_sub_keys")[0]
    shared = {
        "w_in": np.ascontiguousarray(f("w_in")[0]), "g1col": col(g1, 8),
        "gainqk": bc(np.concatenate([np.tile(f("swa_q_gain")[0], 8), np.tile(f("swa_k_gain")[0], 2)])),
        "sinks_bc": bc(f("swa_sinks")[0]), "cst": cst, "swab": swab, "gbcol": col(f("gate_bias")[0], 16),
        "w_up_swa": np.ascontiguousarray(f("w_up_swa")[0]), "w_up_sb": np.ascontiguousarray(f("w_up_sb")[0]),
        "w_out": np.ascontiguousarray(f("w_out")[0]), "wq": np.ascontiguousarray(f("peer_w_q")[0]),
        "g2col": col(g2, 8), "g2bc": bc(g2),
        "skT": np.ascontiguousarray(sk.reshape(16, 128, 128).transpose(2, 0, 1)),
        "u": np.ascontiguousarray(f("peer_u")[0]), "v": np.ascontiguousarray(f("peer_v")[0]),
        "iota16": bc(np.arange(16, dtype=np.float32)),
    }
    return shared


def kernel(**inputs):
    x = np.asarray(inputs["x"], np.float32)
    B, S_, D_ = x.shape
    nseq = B // NCORES
    shared = host_inputs(inputs)
    nc = build_program(nseq)
    in_maps = []
    for c in range(NCORES):
        m = dict(shared)
        m["x"] = np.ascontiguousarray(x[c * nseq:(c + 1) * nseq].reshape(nseq * S_, D_))
        in_maps.append(m)
    res = run_bass_kernel_spmd(nc, in_maps, core_ids=list(range(NCORES)))
    out = np.concatenate([np.asarray(r["y"], np.float32).reshape(nseq, S_, D_) for r in res.results], axis=0)
    return out
```

```python
from contextlib import ExitStack
import numpy as np
import concourse.bass as bass
import concourse.mybir as mybir
from concourse.bass_utils import run_bass_kernel_spmd


F32 = mybir.dt.float32
BF16 = mybir.dt.bfloat16
I32 = mybir.dt.int32
U32 = mybir.dt.uint32
AF = mybir.ActivationFunctionType
ALU = mybir.AluOpType
AX = mybir.AxisListType

SAME_ENGINE_SYNC = True
N_DMA_SEMS = {"sp": 12, "pool": 24, "act": 2}


class Res:
    __slots__ = ("name", "w", "r", "open")

    def __init__(self, name):
        self.name = name
        self.w = None
        self.r = {}
        self.open = False


class Ring:
    def __init__(self, items):
        self.items = list(items)
        self.i = 0

    def get(self):
        it = self.items[self.i % len(self.items)]
        self.i += 1
        res = it[0] if isinstance(it, tuple) else it
        assert not res.open, f"ring slot {res.name} still open"
        res.open = True
        return it


def release(res):
    res.open = False


class Prog:
    ENG = ("pe", "dve", "act", "pool", "sp")

    def __init__(self, nc):
        self.nc = nc
        self.ops = {e: [] for e in self.ENG}
        self.cnt = {e: 0 for e in self.ENG}
        self.seen = {e: {} for e in self.ENG}
        self.sems = {}
        for e in ("pe", "dve", "act", "pool"):
            self.sems[e] = nc.alloc_semaphore(f"s_{e}")
        self.dma_pool = {}
        for q, n in N_DMA_SEMS.items():
            self.dma_pool[q] = {"i": 0, "sems": []}
            for k in range(n):
                key = ("dma", q, k)
                self.sems[key] = nc.alloc_semaphore(f"s_dma_{q}_{k}")
                self.dma_pool[q]["sems"].append([key, 0])
        self.n_wait = 0

    def _deps(self, eng, reads, writes):
        deps = {}

        def add(tok):
            if tok is None:
                return
            k, v = tok
            if deps.get(k, 0) < v:
                deps[k] = v
        for r in reads:
            add(r.w)
        for w in writes:
            add(w.w)
            for k, v in w.r.items():
                add((k, v))
        out = []
        for k, v in deps.items():
            if k == eng and (eng == "pe" or not SAME_ENGINE_SYNC):
                continue
            if self.seen[eng].get(k, 0) >= v:
                continue
            self.seen[eng][k] = v
            out.append((k, v))
        return out

    def _update(self, tok, reads, writes):
        k, v = tok
        for r in reads:
            if r.r.get(k, 0) < v:
                r.r[k] = v
        for w in writes:
            w.w = tok
            w.r = {}

    def op(self, eng, fn, reads=(), writes=()):
        waits = self._deps(eng, reads, writes)
        self.cnt[eng] += 1
        tok = (eng, self.cnt[eng])
        self.ops[eng].append((waits, fn, (eng, 1)))
        self._update(tok, reads, writes)
        self.n_wait += len(waits)
        return tok

    def dma(self, q, fn, reads=(), writes=()):
        pool = self.dma_pool[q]
        slot = pool["sems"][pool["i"] % len(pool["sems"])]
        pool["i"] += 1
        key, cnt = slot
        waits = self._deps(q, reads, writes)
        if cnt > 0 and self.seen[q].get(key, 0) < 16 * cnt:
            self.seen[q][key] = 16 * cnt
            waits.append((key, 16 * cnt))
        slot[1] = cnt + 1
        tok = (key, 16 * (cnt + 1))
        self.ops[q].append((waits, fn, (key, 16)))
        self._update(tok, reads, writes)
        return tok

    def wait_tokens(self, eng, toks):
        waits = []
        for k, v in toks:
            if self.seen[eng].get(k, 0) >= v:
                continue
            self.seen[eng][k] = v
            waits.append((k, v))
        if waits:
            self.ops[eng].append((waits, None, None))

    def all_tokens(self):
        toks = []
        for e in ("pe", "dve", "act", "pool"):
            if self.cnt[e] > 0:
                toks.append((e, self.cnt[e]))
        for q, pool in self.dma_pool.items():
            for key, cnt in pool["sems"]:
                if cnt > 0:
                    toks.append((key, 16 * cnt))
        return toks

    def barrier(self):
        toks = self.all_tokens()
        for e in self.ENG:
            self.wait_tokens(e, [t for t in toks if not (e == 'pe' and t[0] == 'pe')])

    def replay(self):
        nc = self.nc
        P = self
        with nc.Block() as block:
            def run(e, name):
                for waits, fn, inc in P.ops[name]:
                    for k, v in waits:
                        e.wait_ge(P.sems[k], v)
                    if fn is not None:
                        ins = fn(e)
                        ins.then_inc(P.sems[inc[0]], inc[1])

            @block.sync
            def _(e):
                run(e, "sp")

            @block.tensor
            def _(e):
                run(e, "pe")

            @block.vector
            def _(e):
                run(e, "dve")

            @block.scalar
            def _(e):
                run(e, "act")

            @block.gpsimd
            def _(e):
                run(e, "pool")
        for e in self.ENG:
            self.ops[e] = []


STACK = [None]


def sb(nc, name, shape, dt):
    return STACK[0].enter_context(nc.sbuf_tensor(name, list(shape), dt))


def ps(nc, name, shape, dt):
    return STACK[0].enter_context(nc.psum_tensor(name, list(shape), dt))


def consts():
    j = np.arange(128)[:, None]; t = np.arange(128)[None, :]
    ident = np.eye(128, dtype=np.float32)
    tri = (j >= t).astype(np.float32)
    ones = np.ones((128, 128), np.float32)
    maskL = (j < t).astype(np.float32)
    maskb = np.where(j < t, 0.0, 30000.0).astype(np.float32)
    cst = np.stack([ident, tri, ones, maskL, maskb]).astype(np.float32)
    slopes = 2.0 ** (-8.0 * np.arange(1, 9) / 8)
    k = np.arange(128)[:, None]; q = np.arange(128)[None, :]
    swab = np.zeros((128, 8, 2, 128), np.float32)
    for kt in range(2):
        spos = (kt - 1) * 128 + k
        tpos = q
        ck = spos // 64
        cq = tpos // 64
        vis = (ck <= cq) & (ck >= cq - 2)
        dist = np.abs(tpos - spos).astype(np.float64)
        for h in range(8):
            b = np.where(vis, -slopes[h] * dist * 8.0, -240000.0)
            swab[:, h, kt, :] = b
    return cst, np.ascontiguousarray(swab.reshape(128, 2048))
def col(v, n):
    return np.ascontiguousarray(np.asarray(v, np.float32).reshape(n, 128).T)
def bc(v):
    v = np.asarray(v, np.float32).reshape(1, -1)
    return np.ascontiguousarray(np.broadcast_to(v, (128, v.shape[1])))


D = 1024
S = 2048
NT = 16
BATCH = 2
SKIP_SB = set()
SKIP_SWA = set()


def phase_a1(P, nc, dr, nseq):
    R = lambda n: Res(n)
    win = sb(nc, "a_win", [128, 8, 2304], BF16)
    stg = [sb(nc, f"a_stg{i}", [128, 1152], F32) for i in range(2)]
    g1col = sb(nc, "a_g1col", [128, 8], F32)
    gainqk = sb(nc, "a_gainqk", [128, 640], F32)
    esink = sb(nc, "a_esink", [128, 8], F32)
    epsc = sb(nc, "a_eps", [128, 1], F32)
    cstf = sb(nc, "a_cstf", [128, 5, 128], F32)
    cstb = sb(nc, "a_cstb", [128, 5, 128], BF16)
    identb, trib, onesb, maskLb, maskbb = (cstb[:, k, :] for k in range(5))
    swabf = sb(nc, "a_swabf", [128, 2048], F32)
    swab = sb(nc, "a_swab", [128, 8, 2, 128], BF16)
    kaT = sb(nc, "a_kaT", [64, 2, S], BF16)
    kbT = sb(nc, "a_kbT", [128, 4, S], BF16)
    vb = sb(nc, "a_vb", [128, NT, 512], BF16)
    vaug = sb(nc, "a_vaug", [128, NT, 2, 65], BF16)
    xa = [sb(nc, f"a_xa{i}", [128, D], F32) for i in range(2)]
    xs = sb(nc, "a_xs", [128, D], BF16)
    junk = sb(nc, "a_junk", [128, D], BF16)
    hT = sb(nc, "a_hT", [128, 8, 128], BF16)
    sq = sb(nc, "a_sq", [128, 640], F32)
    qtmp = sb(nc, "a_qtmp", [128, 640], F32)
    qn = sb(nc, "a_qn", [128, 640], BF16)
    ssq = sb(nc, "a_ssq", [128, 10], F32)
    rq = sb(nc, "a_rq", [128, 10], F32)
    ss = sb(nc, "a_ss", [128, 1], F32)
    lnv = sb(nc, "a_lnv", [128, 1], F32)
    rstd = sb(nc, "a_rstd", [128, 1], F32)
    qaT = sb(nc, "a_qaT", [64, 8, 128], BF16)
    qbT = sb(nc, "a_qbT", [128, 4, 128], BF16)
    qbTn = sb(nc, "a_qbTn", [128, 4, 128], BF16)
    PT = [sb(nc, f"a_PT{i}", [128, 2, 2, 128], BF16) for i in range(2)]
    dn = sb(nc, "a_dn", [128, 4, 1], F32)
    ya = sb(nc, "a_ya", [128, 512], BF16)
    yaT = [sb(nc, f"a_yaT{i}", [128, 4, 128], BF16) for i in range(2)]
    ybT = [sb(nc, f"a_ybT{i}", [64, 8, 128], BF16) for i in range(2)]
    Eb = [sb(nc, f"a_E{i}", [128, 512], F32) for i in range(2)]
    Lb = [sb(nc, f"a_L{i}", [128, 512], BF16) for i in range(2)]
    Wb = [sb(nc, f"a_W{i}", [128, 512], BF16) for i in range(2)]
    Sb = sb(nc, "a_S", [128, 17, 128], BF16)
    TR = ps(nc, "a_TR", [128, 1024], BF16)
    PSR = [ps(nc, f"a_PS{i}", [128, 512], F32) for i in range(4)]
    OUT = [ps(nc, f"a_OUT{i}", [128, 512], F32) for i in range(2)]
    cin = [sb(nc, f"a_cin{i}", [128, 2048], F32) for i in range(2)]
    cout = [sb(nc, f"a_cout{i}", [128, 2048], BF16) for i in range(2)]
    r_cin = [R("cin0"), R("cin1")]
    r_cout = [R("cout0"), R("cout1")]
    conv_state = {"k": 0}

    def convert_chunks(n):
        for _ in range(n):
            k = conv_state["k"]
            if k >= 128:
                return
            conv_state["k"] = k + 1
            src, dst = (dr["u"], dr["ub"]) if k < 64 else (dr["v"], dr["vb"])
            r0 = (k % 64) * 256
            s_ap = src[r0:r0 + 256, :].rearrange("(p i) d -> p (i d)", i=2)
            d_ap = dst[r0:r0 + 256, :].rearrange("(p i) d -> p (i d)", i=2)
            bi = k % 2
            P.dma("pool", lambda e, s_ap=s_ap, bi=bi: e.dma_start(out=cin[bi][:], in_=s_ap), writes=[r_cin[bi]])
            P.op("pool", lambda e, bi=bi: e.tensor_copy(out=cout[bi][:], in_=cin[bi][:]), reads=[r_cin[bi]], writes=[r_cout[bi]])
            P.dma("pool", lambda e, d_ap=d_ap, bi=bi: e.dma_start(out=d_ap, in_=cout[bi][:]), reads=[r_cout[bi]])

    r_win, r_const = R("win"), R("const")
    r_stg = [R("stg0"), R("stg1")]
    r_kaT, r_kbT, r_vb, r_vaug = R("kaT"), R("kbT"), R("vb"), R("vaug")
    r_xa = [R("xa0"), R("xa1")]
    r_xs, r_junk, r_hT, r_sq, r_qtmp, r_qn, r_norm, r_qnorm = (R(n) for n in
                                                               ("xs", "junk", "hT", "sq", "qtmp", "qn", "norm", "qnorm"))
    r_qaT, r_qbT = R("qaT"), R("qbT")
    r_PT = [R("PT0"), R("PT1")]
    r_dn, r_ya = R("dn"), R("ya")
    r_yaT = [R("yaT0"), R("yaT1")]
    r_ybT = [R("ybT0"), R("ybT1")]
    r_S = [R(f"S{k}") for k in range(17)]
    r_TR = R("TR")
    r_OUT = [R("OUT0"), R("OUT1")]
    psr = Ring([(R(f"PS{i}"), PSR[i]) for i in range(4)])
    ering = Ring([(R(f"E{i}"), Eb[i]) for i in range(2)])
    lring = Ring([(R(f"L{i}"), Lb[i]) for i in range(2)])
    wring = Ring([(R(f"W{i}"), Wb[i]) for i in range(2)])

    P.dma("sp", lambda e: e.dma_start(out=g1col[:], in_=dr["g1col"]), writes=[r_const])
    P.dma("sp", lambda e: e.dma_start(out=gainqk[:], in_=dr["gainqk"]), writes=[r_const])
    P.dma("sp", lambda e: e.dma_start(out=esink[:], in_=dr["sinks_bc"]), writes=[r_const])
    P.dma("sp", lambda e: e.dma_start(out=cstf[:], in_=dr["cst"][0:5].rearrange("k p n -> p k n")), writes=[r_const])
    P.dma("sp", lambda e: e.dma_start(out=swabf[:], in_=dr["swab"]), writes=[r_const])
    P.op("act", lambda e: e.activation(out=esink[:], in_=esink[:], func=AF.Exp), reads=[r_const], writes=[r_const])
    P.op("dve", lambda e: e.tensor_copy(out=cstb[:], in_=cstf[:]), reads=[r_const], writes=[r_const])
    P.op("dve", lambda e: e.tensor_copy(out=swab[:].rearrange("p a b c -> p (a b c)"), in_=swabf[:]),
         reads=[r_const], writes=[r_const])
    P.op("dve", lambda e: e.memset(epsc[:], 1e-6), writes=[r_const])
    P.op("dve", lambda e: e.memset(vaug[:], 1.0), writes=[r_vaug])
    k = 0
    for kc in range(8):
        for hf in range(2):
            s = k % 2
            k += 1
            P.dma("sp", lambda e, kc=kc, hf=hf, s=s: e.dma_start(
                out=stg[s][:], in_=dr["w_in"][kc * 128:(kc + 1) * 128, hf * 1152:(hf + 1) * 1152]), writes=[r_stg[s]])
            P.op("dve", lambda e, kc=kc, hf=hf, s=s: e.tensor_scalar(
                out=win[:, kc, hf * 1152:(hf + 1) * 1152], in0=stg[s][:], scalar1=g1col[:, kc:kc + 1], scalar2=None,
                op0=ALU.mult), reads=[r_stg[s], r_const], writes=[r_win])

    def front(sq_i, tt):
        gt = sq_i * NT + tt
        b = gt % 2
        xap = dr["x"][gt * 128:(gt + 1) * 128, :]
        P.dma("sp", lambda e: e.dma_start(out=xa[b][:], in_=xap), writes=[r_xa[b]])
        P.op("act", lambda e: e.activation(out=junk[:], in_=xa[b][:], func=AF.Square, accum_out=ss[:]),
             reads=[r_xa[b]], writes=[r_junk, r_norm])
        P.op("act", lambda e: e.activation(out=lnv[:], in_=ss[:], func=AF.Ln, scale=1.0 / D, bias=epsc[:]),
             reads=[r_norm, r_const], writes=[r_norm])
        P.op("act", lambda e: e.activation(out=rstd[:], in_=lnv[:], func=AF.Exp, scale=-0.5), reads=[r_norm], writes=[r_norm])
        P.op("dve", lambda e: e.tensor_scalar(out=xs[:], in0=xa[b][:], scalar1=rstd[:], scalar2=None, op0=ALU.mult),
             reads=[r_xa[b], r_norm], writes=[r_xs])
        for kc in range(8):
            P.op("pe", lambda e, kc=kc: e.transpose(out=TR[:, kc * 128:(kc + 1) * 128], in_=xs[:, kc * 128:(kc + 1) * 128],
                                                    identity=identb), reads=[r_xs, r_const], writes=[r_TR])
        P.op("act", lambda e: e.activation(out=hT[:].rearrange("p a b -> p (a b)"), in_=TR[:], func=AF.Copy),
             reads=[r_TR], writes=[r_hT])
        rA1, A1 = psr.get()
        rA2, A2 = psr.get()
        rA3, A3 = psr.get()
        for (rr, bank, c0, n) in ((rA1, A1, 0, 512), (rA2, A2, 512, 256), (rA3, A3, 1792, 512)):
            for kc in range(8):
                P.op("pe", lambda e, bank=bank, c0=c0, n=n, kc=kc: e.matmul(
                    bank[:, 0:n], lhsT=hT[:, kc, :], rhs=win[:, kc, c0:c0 + n], start=(kc == 0), stop=(kc == 7)),
                    reads=[r_hT, r_win], writes=[rr])
        P.op("act", lambda e: e.activation(out=vb[:, tt, :], in_=A3[:, 0:512], func=AF.Copy), reads=[rA3], writes=[r_vb])
        release(rA3)
        P.op("act", lambda e: e.activation(out=vaug[:, tt, :, 0:64], in_=A2[:, 128:256].rearrange("p (g d) -> p g d", d=64),
                                           func=AF.Copy), reads=[rA2], writes=[r_vaug])
        P.op("act", lambda e: e.activation(out=sq[:, 0:512], in_=A1[:, 0:512], func=AF.Square), reads=[rA1], writes=[r_sq])
        P.op("act", lambda e: e.activation(out=sq[:, 512:640], in_=A2[:, 0:128], func=AF.Square), reads=[rA2], writes=[r_sq])
        P.op("dve", lambda e: e.tensor_reduce(out=ssq[:], in_=sq[:].rearrange("p (h d) -> p h d", d=64), axis=AX.X, op=ALU.add),
             reads=[r_sq], writes=[r_qnorm])
        P.op("act", lambda e: e.activation(out=rq[:], in_=ssq[:], func=AF.Ln, scale=1.0 / 64, bias=epsc[:]),
             reads=[r_qnorm, r_const], writes=[r_qnorm])
        P.op("act", lambda e: e.activation(out=rq[:], in_=rq[:], func=AF.Exp, scale=-0.5), reads=[r_qnorm], writes=[r_qnorm])
        P.op("dve", lambda e: e.tensor_tensor(out=qtmp[:, 0:512].rearrange("p (h d) -> p h d", d=64),
                                              in0=A1[:, 0:512].rearrange("p (h d) -> p h d", d=64),
                                              in1=rq[:, 0:8].unsqueeze(2).to_broadcast([128, 8, 64]), op=ALU.mult),
             reads=[rA1, r_qnorm], writes=[r_qtmp])
        P.op("dve", lambda e: e.tensor_tensor(out=qtmp[:, 512:640].rearrange("p (h d) -> p h d", d=64),
                                              in0=A2[:, 0:128].rearrange("p (h d) -> p h d", d=64),
                                              in1=rq[:, 8:10].unsqueeze(2).to_broadcast([128, 2, 64]), op=ALU.mult),
             reads=[rA2, r_qnorm], writes=[r_qtmp])
        release(rA1)
        release(rA2)
        P.op("dve", lambda e: e.tensor_tensor(out=qn[:], in0=qtmp[:], in1=gainqk[:], op=ALU.mult),
             reads=[r_qtmp, r_const], writes=[r_qn])
        for h in range(8):
            P.op("pe", lambda e, h=h: e.transpose(out=TR[0:64, h * 128:(h + 1) * 128], in_=qn[:, h * 64:(h + 1) * 64],
                                                  identity=identb), reads=[r_qn, r_const], writes=[r_TR])
        P.op("act", lambda e: e.activation(out=qaT[:].rearrange("p a b -> p (a b)"), in_=TR[0:64, 0:1024], func=AF.Copy),
             reads=[r_TR], writes=[r_qaT])

    def front_k(tt):
        for g in range(2):
            P.op("pe", lambda e, g=g: e.transpose(out=TR[0:64, g * 128:(g + 1) * 128], in_=qn[:, 512 + g * 64:512 + (g + 1) * 64],
                                                  identity=identb), reads=[r_qn, r_const], writes=[r_TR])
        P.op("act", lambda e: e.activation(out=kaT[:, :, tt * 128:(tt + 1) * 128],
                                           in_=TR[0:64, 0:256].rearrange("p (g t) -> p g t", t=128), func=AF.Copy),
             reads=[r_TR], writes=[r_kaT])

    def front_b(tt):
        for half, c0 in ((0, 768), (1, 1280)):
            rB, B = psr.get()
            for c in range(4):
                for kc in range(8):
                    P.op("pe", lambda e, B=B, c=c, kc=kc, c0=c0: e.matmul(
                        B[:, c * 128:(c + 1) * 128], lhsT=win[:, kc, c0 + c * 128:c0 + (c + 1) * 128], rhs=hT[:, kc, :],
                        start=(kc == 0), stop=(kc == 7)), reads=[r_win, r_hT], writes=[rB])
            if half == 0:
                P.op("act", lambda e, B=B: e.activation(out=qbT[:].rearrange("p a b -> p (a b)"), in_=B[:], func=AF.Copy,
                                                        scale=0.125), reads=[rB], writes=[r_qbT])
                P.op("act", lambda e, B=B: e.activation(out=qbTn[:].rearrange("p a b -> p (a b)"), in_=B[:], func=AF.Copy,
                                                        scale=-0.125), reads=[rB], writes=[r_qbT])
            else:
                P.op("act", lambda e, B=B: e.activation(out=kbT[:, :, tt * 128:(tt + 1) * 128],
                                                        in_=B[:].rearrange("p (c t) -> p c t", t=128), func=AF.Copy),
                     reads=[rB], writes=[r_kbT])
            release(rB)

    def swa(gt, tt):
        yb = gt % 2
        kts = [1] if tt == 0 else [0, 1]
        rOA = OAb = None
        for hb in range(4):
            g = hb // 2
            rST, ST = psr.get()
            ptb = hb % 2
            for hh in range(2):
                h = 2 * hb + hh
                for kt in kts:
                    ktile = tt - 1 + kt
                    o = (hh * 2 + kt) * 128
                    P.op("pe", lambda e, ST=ST, o=o, g=g, h=h, ktile=ktile: e.matmul(
                        ST[:, o:o + 128], lhsT=kaT[0:64, g, ktile * 128:(ktile + 1) * 128], rhs=qaT[0:64, h, :],
                        start=True, stop=False), reads=[r_kaT, r_qaT], writes=[rST])
                    P.op("pe", lambda e, ST=ST, o=o, h=h, kt=kt: e.matmul(
                        ST[:, o:o + 128], lhsT=identb, rhs=swab[:, h, kt, :], start=False, stop=True),
                        reads=[r_const], writes=[rST])
            if tt == 0:
                for hh in range(2):
                    o = (hh * 2 + 1) * 128
                    P.op("act", lambda e, ST=ST, o=o, hh=hh, ptb=ptb: e.activation(
                        out=PT[ptb][:, hh, 1, :], in_=ST[:, o:o + 128], func=AF.Exp, scale=0.125),
                        reads=[rST], writes=[r_PT[ptb]])
            else:
                P.op("act", lambda e, ST=ST, ptb=ptb: e.activation(
                    out=PT[ptb][:].rearrange("p a b c -> p (a b c)"), in_=ST[:], func=AF.Exp, scale=0.125),
                    reads=[rST], writes=[r_PT[ptb]])
            release(rST)
            if hb % 2 == 0:
                rOA, OAb = psr.get()
            for hh in range(2):
                h = 2 * hb + hh
                o = (h % 4) * 65
                for ki, kt in enumerate(kts):
                    ktile = tt - 1 + kt
                    P.op("pe", lambda e, OAb=OAb, o=o, hh=hh, kt=kt, ktile=ktile, g=g, ptb=ptb, ki=ki: e.matmul(
                        OAb[:, o:o + 65], lhsT=PT[ptb][:, hh, kt, :], rhs=vaug[:, ktile, g, :],
                        start=(ki == 0), stop=(ki == len(kts) - 1)), reads=[r_PT[ptb], r_vaug], writes=[rOA])
            if hb % 2 == 1:
                hs = (hb // 2) * 4
                oav = OAb[:, 0:260].rearrange("p (h d) -> p h d", d=65)
                P.op("dve", lambda e, oav=oav, hs=hs: e.tensor_tensor(
                    out=dn[:], in0=oav[:, :, 64:65], in1=esink[:, hs:hs + 4].unsqueeze(2), op=ALU.add),
                    reads=[rOA, r_const], writes=[r_dn])
                P.op("dve", lambda e: e.reciprocal(out=dn[:], in_=dn[:]), reads=[r_dn], writes=[r_dn])
                P.op("dve", lambda e, oav=oav, hs=hs: e.tensor_tensor(
                    out=ya[:, hs * 64:(hs + 4) * 64].rearrange("p (h d) -> p h d", d=64), in0=oav[:, :, 0:64],
                    in1=dn[:].to_broadcast([128, 4, 64]), op=ALU.mult), reads=[rOA, r_dn], writes=[r_ya])
                release(rOA)
        for c in range(4):
            P.op("pe", lambda e, c=c: e.transpose(out=TR[:, c * 128:(c + 1) * 128], in_=ya[:, c * 128:(c + 1) * 128],
                                                  identity=identb), reads=[r_ya, r_const], writes=[r_TR])
        P.op("act", lambda e: e.activation(out=yaT[yb][:].rearrange("p a b -> p (a b)"), in_=TR[:, 0:512], func=AF.Copy),
             reads=[r_TR], writes=[r_yaT[yb]])
        P.dma("sp", lambda e: e.dma_start(out=dr["ysa"][gt], in_=yaT[yb][:].rearrange("p a b -> p (a b)")),
              reads=[r_yaT[yb]])

    def sbattn(gt, tt):
        yb = gt % 2
        for h in range(8):
            c = h // 2
            pb = (h % 2) * 64
            OUTb, rOUT = OUT[h % 2], r_OUT[h % 2]
            kbs = list(range(tt, -1, -1))
            batches = [kbs[i:i + BATCH] for i in range(0, len(kbs), BATCH)]
            nmm = len(kbs)
            mmi = 0
            for batch in batches:
                nb = len(batch)
                rZ, Z = psr.get()
                for b, kb in enumerate(batch):
                    P.op("pe", lambda e, Z=Z, b=b, kb=kb, c=c, pb=pb: e.matmul(
                        Z[:, b * 128:(b + 1) * 128], lhsT=kbT[pb:pb + 64, c, kb * 128:(kb + 1) * 128],
                        rhs=qbT[pb:pb + 64, c, :], start=True, stop=True), reads=[r_kbT, r_qbT], writes=[rZ])
                rE, E = ering.get()
                P.op("act", lambda e, Z=Z, E=E, nb=nb: e.activation(out=E[:, 0:nb * 128], in_=Z[:, 0:nb * 128], func=AF.Exp),
                     reads=[rZ], writes=[rE])
                release(rZ)
                rL, L = lring.get()
                P.op("act", lambda e, E=E, L=L, nb=nb: e.activation(out=L[:, 0:nb * 128], in_=E[:, 0:nb * 128], func=AF.Ln,
                                                                     bias=1.0), reads=[rE], writes=[rL])
                release(rE)
                for b, kb in enumerate(batch):
                    if kb == tt:
                        P.op("dve", lambda e, L=L, b=b: e.tensor_tensor(out=Sb[:, tt, :], in0=L[:, b * 128:(b + 1) * 128],
                                                                        in1=maskLb, op=ALU.mult),
                             reads=[rL, r_const], writes=[r_S[tt]])
                    elif kb >= 1:
                        P.op("dve", lambda e, L=L, b=b, kb=kb: e.tensor_tensor(out=Sb[:, kb, :], in0=Sb[:, kb + 1, :],
                                                                               in1=L[:, b * 128:(b + 1) * 128], op=ALU.add),
                             reads=[rL, r_S[kb + 1]], writes=[r_S[kb]])
                rC, C = psr.get()
                for b, kb in enumerate(batch):
                    o = b * 128
                    if kb == tt:
                        P.op("pe", lambda e, C=C, o=o: e.matmul(C[:, o:o + 128], lhsT=trib, rhs=Sb[:, tt, :], start=True, stop=False),
                             reads=[r_const, r_S[tt]], writes=[rC])
                    else:
                        P.op("pe", lambda e, C=C, o=o, L=L: e.matmul(C[:, o:o + 128], lhsT=trib, rhs=L[:, o:o + 128],
                                                                     start=True, stop=False), reads=[r_const, rL], writes=[rC])
                        P.op("pe", lambda e, C=C, o=o, kb=kb: e.matmul(C[:, o:o + 128], lhsT=onesb, rhs=Sb[:, kb + 1, :],
                                                                       start=False, stop=False),
                             reads=[r_const, r_S[kb + 1]], writes=[rC])
                    P.op("pe", lambda e, C=C, o=o, kb=kb, c=c, pb=pb: e.matmul(
                        C[:, o:o + 128], lhsT=kbT[pb:pb + 64, c, kb * 128:(kb + 1) * 128], rhs=qbTn[pb:pb + 64, c, :],
                        start=False, stop=(kb != tt)), reads=[r_kbT, r_qbT], writes=[rC])
                    if kb == tt:
                        P.op("pe", lambda e, C=C, o=o: e.matmul(C[:, o:o + 128], lhsT=identb, rhs=maskbb, start=False, stop=True),
                             reads=[r_const], writes=[rC])
                release(rL)
                rW, W = wring.get()
                P.op("act", lambda e, C=C, W=W, nb=nb: e.activation(out=W[:, 0:nb * 128], in_=C[:, 0:nb * 128], func=AF.Exp,
                                                                     scale=-1.0), reads=[rC], writes=[rW])
                release(rC)
                for b, kb in enumerate(batch):
                    P.op("pe", lambda e, W=W, b=b, kb=kb, h=h, OUTb=OUTb, mmi=mmi: e.matmul(
                        OUTb[0:64, 0:128], lhsT=vb[:, kb, h * 64:(h + 1) * 64], rhs=W[:, b * 128:(b + 1) * 128],
                        start=(mmi == 0), stop=(mmi == nmm - 1)), reads=[r_vb, rW], writes=[rOUT])
                    mmi += 1
                release(rW)
            P.op("act", lambda e, OUTb=OUTb, h=h: e.activation(out=ybT[yb][:, h, :], in_=OUTb[0:64, 0:128], func=AF.Copy),
                 reads=[rOUT], writes=[r_ybT[yb]])
        P.dma("sp", lambda e: e.dma_start(out=dr["ysb"][gt], in_=ybT[yb][:].rearrange("p a b -> p (a b)")),
              reads=[r_ybT[yb]])

    for sq_i in range(nseq):
        for tt in range(NT):
            gt = sq_i * NT + tt
            convert_chunks((128 + nseq * NT - 1) // (nseq * NT))
            front(sq_i, tt)
            front_k(tt)
            front_b(tt)
            if tt not in SKIP_SWA:
                swa(gt, tt)
            if tt not in SKIP_SB:
                sbattn(gt, tt)
            if tt in SKIP_SWA:
                pass


def phase_a2(P, nc, dr, ntiles):
    R = lambda n: Res(n)
    wg = sb(nc, "m_wg", [128, 8, 2048], BF16)
    wupa = sb(nc, "m_wupa", [128, 4, 1024], BF16)
    wupb = sb(nc, "m_wupb", [64, 8, 1024], BF16)
    wout = sb(nc, "m_wout", [128, 8, 1024], BF16)
    stg = [sb(nc, f"m_stg{i}", [128, 1024], F32) for i in range(2)]
    g1col = sb(nc, "m_g1col", [128, 8], F32)
    ngb = sb(nc, "m_ngb", [128, 16], F32)
    identf = sb(nc, "m_identf", [128, 128], F32)
    identb = sb(nc, "m_identb", [128, 128], BF16)
    epsc = sb(nc, "m_eps", [128, 1], F32)
    xa = [sb(nc, f"m_xa{i}", [128, D], F32) for i in range(2)]
    xs = sb(nc, "m_xs", [128, D], BF16)
    junk = sb(nc, "m_junk", [128, D], BF16)
    hT = sb(nc, "m_hT", [128, 8, 128], BF16)
    ss = sb(nc, "m_ss", [128, 1], F32)
    lnv = sb(nc, "m_lnv", [128, 1], F32)
    rstd = sb(nc, "m_rstd", [128, 1], F32)
    yaT = [sb(nc, f"m_yaT{i}", [128, 4, 128], BF16) for i in range(2)]
    ybT = [sb(nc, f"m_ybT{i}", [64, 8, 128], BF16) for i in range(2)]
    eg = [sb(nc, f"m_eg{i}", [128, 2, 128], F32) for i in range(2)]
    tmp = [sb(nc, f"m_tmp{i}", [128, 2, 128], F32) for i in range(2)]
    mT = sb(nc, "m_mT", [128, 8, 128], BF16)
    x1 = [sb(nc, f"m_x1{i}", [128, D], F32) for i in range(2)]
    TR = ps(nc, "m_TR", [128, 1024], BF16)
    PSR = [ps(nc, f"m_PS{i}", [128, 512], F32) for i in range(6)]

    r_w, r_const = R("w"), R("const")
    r_stg = [R("stg0"), R("stg1")]
    r_xa = [R("xa0"), R("xa1")]
    r_xs, r_junk, r_hT, r_norm, r_mT, r_TR = (R(n) for n in ("xs", "junk", "hT", "norm", "mT", "TR"))
    r_yaT = [R("yaT0"), R("yaT1")]
    r_ybT = [R("ybT0"), R("ybT1")]
    r_eg = [R("eg0"), R("eg1")]
    r_tmp = [R("tmp0"), R("tmp1")]
    r_x1 = [R("x10"), R("x11")]
    psr = Ring([(R(f"PS{i}"), PSR[i]) for i in range(6)])

    P.dma("sp", lambda e: e.dma_start(out=g1col[:], in_=dr["g1col"]), writes=[r_const])
    P.dma("sp", lambda e: e.dma_start(out=ngb[:], in_=dr["gbcol"]), writes=[r_const])
    P.dma("sp", lambda e: e.dma_start(out=identf[:], in_=dr["ident"]), writes=[r_const])
    P.op("dve", lambda e: e.tensor_copy(out=identb[:], in_=identf[:]), reads=[r_const], writes=[r_const])
    P.op("dve", lambda e: e.tensor_scalar(out=ngb[:], in0=ngb[:], scalar1=-1.0, scalar2=None, op0=ALU.mult),
         reads=[r_const], writes=[r_const])
    P.op("dve", lambda e: e.memset(epsc[:], 1e-6), writes=[r_const])
    k = 0

    def load(dst_ap, src_ap, npart, scalar=None):
        nonlocal k
        s = k % 2
        k += 1
        P.dma("sp", lambda e: e.dma_start(out=stg[s][0:npart, :], in_=src_ap), writes=[r_stg[s]])
        if scalar is None:
            P.op("dve", lambda e: e.tensor_copy(out=dst_ap, in_=stg[s][0:npart, :]), reads=[r_stg[s]], writes=[r_w])
        else:
            P.op("dve", lambda e: e.tensor_scalar(out=dst_ap, in0=stg[s][0:npart, :], scalar1=scalar, scalar2=None,
                                                  op0=ALU.mult), reads=[r_stg[s], r_const], writes=[r_w])
    for kc in range(8):
        for hf in range(2):
            load(wg[:, kc, hf * 1024:(hf + 1) * 1024], dr["w_in"][kc * 128:(kc + 1) * 128, 2304 + hf * 1024:2304 + (hf + 1) * 1024],
                 128, g1col[:, kc:kc + 1])
    for i in range(4):
        load(wupa[:, i, :], dr["w_up_swa"][i * 128:(i + 1) * 128, :], 128)
    for h in range(8):
        load(wupb[:, h, :], dr["w_up_sb"][h * 64:(h + 1) * 64, :], 64)
    for dc in range(8):
        load(wout[:, dc, :], dr["w_out"][dc * 128:(dc + 1) * 128, :], 128)

    toks = []
    for gt in range(ntiles):
        b = gt % 2
        xap = dr["x"][gt * 128:(gt + 1) * 128, :]
        P.dma("sp", lambda e, xap=xap, b=b: e.dma_start(out=xa[b][:], in_=xap), writes=[r_xa[b]])
        P.dma("sp", lambda e, gt=gt, b=b: e.dma_start(out=yaT[b][:].rearrange("p a b -> p (a b)"), in_=dr["ysa"][gt]),
              writes=[r_yaT[b]])
        P.dma("sp", lambda e, gt=gt, b=b: e.dma_start(out=ybT[b][:].rearrange("p a b -> p (a b)"), in_=dr["ysb"][gt]),
              writes=[r_ybT[b]])
        P.op("act", lambda e, b=b: e.activation(out=junk[:], in_=xa[b][:], func=AF.Square, accum_out=ss[:]),
             reads=[r_xa[b]], writes=[r_junk, r_norm])
        P.op("act", lambda e: e.activation(out=lnv[:], in_=ss[:], func=AF.Ln, scale=1.0 / D, bias=epsc[:]),
             reads=[r_norm, r_const], writes=[r_norm])
        P.op("act", lambda e: e.activation(out=rstd[:], in_=lnv[:], func=AF.Exp, scale=-0.5), reads=[r_norm], writes=[r_norm])
        P.op("dve", lambda e, b=b: e.tensor_scalar(out=xs[:], in0=xa[b][:], scalar1=rstd[:], scalar2=None, op0=ALU.mult),
             reads=[r_xa[b], r_norm], writes=[r_xs])
        for kc in range(8):
            P.op("pe", lambda e, kc=kc: e.transpose(out=TR[:, kc * 128:(kc + 1) * 128], in_=xs[:, kc * 128:(kc + 1) * 128],
                                                    identity=identb[:]), reads=[r_xs, r_const], writes=[r_TR])
        P.op("act", lambda e: e.activation(out=hT[:].rearrange("p a b -> p (a b)"), in_=TR[:], func=AF.Copy),
             reads=[r_TR], writes=[r_hT])
        for dc in range(8):
            rM, M = psr.get()
            cs = slice(dc * 128, (dc + 1) * 128)
            for i in range(4):
                P.op("pe", lambda e, M=M, i=i, cs=cs, b=b: e.matmul(M[:, 0:128], lhsT=wupa[:, i, cs], rhs=yaT[b][:, i, :],
                                                                   start=(i == 0), stop=(i == 3)),
                     reads=[r_w, r_yaT[b]], writes=[rM])
            for h in range(8):
                P.op("pe", lambda e, M=M, h=h, cs=cs, b=b: e.matmul(M[:, 128:256], lhsT=wupb[:, h, cs], rhs=ybT[b][:, h, :],
                                                                   start=(h == 0), stop=(h == 7)),
                     reads=[r_w, r_ybT[b]], writes=[rM])
            for gi in range(2):
                gs = slice(gi * 1024 + dc * 128, gi * 1024 + (dc + 1) * 128)
                for kc in range(8):
                    P.op("pe", lambda e, M=M, gi=gi, gs=gs, kc=kc: e.matmul(M[:, (2 + gi) * 128:(3 + gi) * 128], lhsT=wg[:, kc, gs],
                                                                           rhs=hT[:, kc, :], start=(kc == 0), stop=(kc == 7)),
                         reads=[r_w, r_hT], writes=[rM])
            eb = dc % 2
            for gi in range(2):
                P.op("act", lambda e, M=M, gi=gi, dc=dc, eb=eb: e.activation(
                    out=eg[eb][:, gi, :], in_=M[:, (2 + gi) * 128:(3 + gi) * 128], func=AF.Exp, scale=-1.0,
                    bias=ngb[:, gi * 8 + dc:gi * 8 + dc + 1]), reads=[rM, r_const], writes=[r_eg[eb]])
            P.op("dve", lambda e, eb=eb: e.tensor_scalar(out=eg[eb][:], in0=eg[eb][:], scalar1=1.0, scalar2=None, op0=ALU.add),
                 reads=[r_eg[eb]], writes=[r_eg[eb]])
            P.op("dve", lambda e, eb=eb: e.reciprocal(out=eg[eb][:], in_=eg[eb][:]), reads=[r_eg[eb]], writes=[r_eg[eb]])
            P.op("dve", lambda e, M=M, eb=eb: e.tensor_tensor(out=tmp[eb][:].rearrange("p a b -> p (a b)"),
                                                              in0=eg[eb][:].rearrange("p a b -> p (a b)"), in1=M[:, 0:256],
                                                              op=ALU.mult), reads=[r_eg[eb], rM], writes=[r_tmp[eb]])
            release(rM)
            P.op("dve", lambda e, eb=eb, dc=dc: e.tensor_tensor(out=mT[:, dc, :], in0=tmp[eb][:, 0, :], in1=tmp[eb][:, 1, :],
                                                                op=ALU.add), reads=[r_tmp[eb]], writes=[r_mT])
        for hf in range(2):
            rO, O = psr.get()
            for dc in range(8):
                P.op("pe", lambda e, O=O, dc=dc, hf=hf: e.matmul(O[:, 0:512], lhsT=mT[:, dc, :],
                                                                rhs=wout[:, dc, hf * 512:(hf + 1) * 512],
                                                                start=(dc == 0), stop=(dc == 7)), reads=[r_mT, r_w], writes=[rO])
            P.op("dve", lambda e, O=O, hf=hf, b=b: e.tensor_tensor(out=x1[b][:, hf * 512:(hf + 1) * 512],
                                                                   in0=xa[b][:, hf * 512:(hf + 1) * 512], in1=O[:, 0:512],
                                                                   op=ALU.add), reads=[r_xa[b], rO], writes=[r_x1[b]])
            release(rO)
        yap = dr["y"][gt * 128:(gt + 1) * 128, :]
        toks.append(P.dma("sp", lambda e, yap=yap, b=b: e.dma_start(out=yap, in_=x1[b][:]), reads=[r_x1[b]]))
    return toks


NG = 20


def phase_b(P, nc, dr, ntiles, x1_res):
    wq = sb(nc, "b_wq", [128, 8, 2048], BF16)
    skT = sb(nc, "b_skT", [128, 16, 128], F32)
    g2bc = sb(nc, "b_g2bc", [128, D], F32)
    g2col = sb(nc, "b_g2col", [128, 8], F32)
    iota16 = sb(nc, "b_iota", [128, 16], F32)
    identf = sb(nc, "b_identf", [128, 128], F32)
    identb = sb(nc, "b_identb", [128, 128], BF16)
    epsc = sb(nc, "b_eps", [128, 1], F32)
    xt = [sb(nc, f"b_xt{i}", [128, D], F32) for i in range(3)]
    xs = sb(nc, "b_xs", [128, D], BF16)
    junk = sb(nc, "b_junk", [128, D], BF16)
    h2 = [sb(nc, f"b_h2{i}", [128, D], F32) for i in range(2)]
    h2T = sb(nc, "b_h2T", [128, 8, 128], BF16)
    qT = sb(nc, "b_qT", [128, 16, 128], F32)
    sc = sb(nc, "b_sc", [128, 16, 128], F32)
    sc2 = sb(nc, "b_sc2", [128, 16, 128], F32)
    stg = [sc[:].rearrange("p a b -> p (a b)"), sc2[:].rearrange("p a b -> p (a b)")]
    cand = sb(nc, "b_cand", [128, 8, 256], F32)
    cand2 = sb(nc, "b_cand2", [128, 8, 256], F32)
    oh = sb(nc, "b_oh", [128, 8, 16, 16], F32)
    tops = sb(nc, "b_tops", [128, 16, 16], F32)
    topi = sb(nc, "b_topi", [128, 16, 16], U32)
    topif = sb(nc, "b_topif", [128, 16, 16], F32)
    bests = sb(nc, "b_bests", [128, 8, 16], F32)
    bestp = sb(nc, "b_bestp", [128, 8, 16], U32)
    hiu = sb(nc, "b_hiu", [128, 8, 16], U32)
    lou = sb(nc, "b_lou", [128, 8, 16], U32)
    hif = sb(nc, "b_hif", [128, 8, 16], F32)
    lof = sb(nc, "b_lof", [128, 8, 16], F32)
    i0s = sb(nc, "b_i0s", [128, 8, 16], F32)
    i1s = sb(nc, "b_i1s", [128, 8, 16], F32)
    expf = sb(nc, "b_expf", [128, 128], F32)
    idx = [sb(nc, f"b_idx{i}", [128, 128], I32) for i in range(2)]
    gd = sb(nc, "b_gd", [128, 8, 16], F32)
    gsum = sb(nc, "b_gsum", [128, 8], F32)
    gate = [sb(nc, f"b_gate{i}", [128, 8, 16], F32) for i in range(2)]
    dots = sb(nc, "b_dots", [128, 128], F32)
    act = sb(nc, "b_act", [128, 128], F32)
    wgt = sb(nc, "b_wgt", [128, 128], F32)
    ss = sb(nc, "b_ss", [128, 1], F32)
    lnv = sb(nc, "b_lnv", [128, 1], F32)
    rstd = sb(nc, "b_rstd", [128, 1], F32)
    G = [sb(nc, f"b_G{i}", [128, D], BF16) for i in range(NG)]
    acc = [sb(nc, f"b_acc{i}", [128, D], F32) for i in range(2)]
    scl = [sb(nc, f"b_scl{i}", [128, D], BF16) for i in range(4)]
    r_scl = [Res(f"scl{i}") for i in range(4)]
    ACC = [ps(nc, f"b_ACC{i}", [128, 512], F32) for i in range(2)]
    r_ACC = Res("ACC")
    TR = ps(nc, "b_TR", [128, 1024], BF16)
    PS = [ps(nc, f"b_PS{i}", [128, 512], F32) for i in range(4)]

    R = lambda n: Res(n)
    r_wq, r_skT, r_const = R("wq"), R("skT"), R("const")
    r_xt = [R("xt0"), R("xt1"), R("xt2")]
    r_xs, r_junk, r_h2T, r_qT, r_sc, r_sc2 = R("xs"), R("junk"), R("h2T"), R("qT"), R("sc"), R("sc2")
    r_stg = [r_sc, r_sc2]
    r_h2 = [R("h2a"), R("h2b")]
    r_cand, r_cand2, r_oh, r_top, r_best, r_sel = R("cand"), R("cand2"), R("oh"), R("top"), R("best"), R("sel")
    r_idx = [R("idx0"), R("idx1")]
    r_gate = [R("gate0"), R("gate1")]
    r_gtmp = R("gtmp")
    r_dots, r_wgt, r_norm = R("dots"), R("wgt"), R("norm")
    r_G = [R(f"G{i}") for i in range(NG)]
    r_acc = [R("acc0"), R("acc1")]
    r_TR = R("TR")
    r_PS = [R(f"PS{i}") for i in range(4)]
    r_y = [R(f"y{i}") for i in range(ntiles)]

    P.dma("sp", lambda e: e.dma_start(out=skT[:], in_=dr["skT"]), writes=[r_skT])
    P.dma("sp", lambda e: e.dma_start(out=g2bc[:], in_=dr["g2bc"]), writes=[r_const])
    P.dma("sp", lambda e: e.dma_start(out=g2col[:], in_=dr["g2col"]), writes=[r_const])
    P.dma("sp", lambda e: e.dma_start(out=iota16[:], in_=dr["iota16"]), writes=[r_const])
    P.dma("sp", lambda e: e.dma_start(out=identf[:], in_=dr["ident"]), writes=[r_const])
    P.op("dve", lambda e: e.tensor_copy(out=identb[:], in_=identf[:]), reads=[r_const], writes=[r_const])
    P.op("dve", lambda e: e.memset(epsc[:], 1e-6), writes=[r_const])
    for kc in range(8):
        s = kc % 2
        P.dma("sp", lambda e, kc=kc, s=s: e.dma_start(out=stg[s], in_=dr["wq"][kc * 128:(kc + 1) * 128, :]),
              writes=[r_stg[s]])
        P.op("dve", lambda e, kc=kc, s=s: e.tensor_scalar(out=wq[:, kc, :], in0=stg[s], scalar1=g2col[:, kc:kc + 1],
                                                         scalar2=None, op0=ALU.mult),
             reads=[r_stg[s], r_const], writes=[r_wq])

    def front(i):
        b = i % 2
        bx = i % 3
        x1ap = dr["x1"][i * 128:(i + 1) * 128, :]
        rd = [x1_res[i]] if x1_res is not None else []
        P.dma("sp", lambda e: e.dma_start(out=xt[bx][:], in_=x1ap), reads=rd, writes=[r_xt[bx]])
        P.op("act", lambda e: e.activation(out=junk[:], in_=xt[bx][:], func=AF.Square, accum_out=ss[:]),
             reads=[r_xt[bx]], writes=[r_junk, r_norm])
        P.op("act", lambda e: e.activation(out=lnv[:], in_=ss[:], func=AF.Ln, scale=1.0 / D, bias=epsc[:]),
             reads=[r_norm, r_const], writes=[r_norm])
        P.op("act", lambda e: e.activation(out=rstd[:], in_=lnv[:], func=AF.Exp, scale=-0.5),
             reads=[r_norm], writes=[r_norm])
        P.op("dve", lambda e: e.tensor_scalar(out=xs[:], in0=xt[bx][:], scalar1=rstd[:], scalar2=None, op0=ALU.mult),
             reads=[r_xt[bx], r_norm], writes=[r_xs])
        P.op("dve", lambda e: e.scalar_tensor_tensor(out=h2[b][:], in0=xt[bx][:], scalar=rstd[:], in1=g2bc[:],
                                                     op0=ALU.mult, op1=ALU.mult),
             reads=[r_xt[bx], r_norm, r_const], writes=[r_h2[b]])
        for kc in range(8):
            P.op("pe", lambda e, kc=kc: e.transpose(out=TR[:, kc * 128:(kc + 1) * 128], in_=xs[:, kc * 128:(kc + 1) * 128],
                                                    identity=identb[:]),
                 reads=[r_xs, r_const], writes=[r_TR])
        P.op("act", lambda e: e.activation(out=h2T[:].rearrange("p a b -> p (a b)"), in_=TR[:], func=AF.Copy),
             reads=[r_TR], writes=[r_h2T])
        for g in range(4):
            for j in range(4):
                hp = g * 4 + j
                for kc in range(8):
                    P.op("pe", lambda e, g=g, j=j, hp=hp, kc=kc: e.matmul(
                        PS[g][:, j * 128:(j + 1) * 128], lhsT=wq[:, kc, hp * 128:(hp + 1) * 128], rhs=h2T[:, kc, :],
                        start=(kc == 0), stop=(kc == 7)),
                        reads=[r_wq, r_h2T], writes=[r_PS[g]])
            P.op("act", lambda e, g=g: e.activation(out=qT[:, g * 4:(g + 1) * 4, :].rearrange("p a b -> p (a b)"),
                                                    in_=PS[g][:], func=AF.Copy),
                 reads=[r_PS[g]], writes=[r_qT])
        for g in range(4):
            for j in range(4):
                hp = g * 4 + j
                P.op("pe", lambda e, g=g, j=j, hp=hp: e.matmul(
                    PS[g][:, j * 128:(j + 1) * 128], lhsT=qT[:, hp, :], rhs=skT[:, hp, :], start=True, stop=True),
                    reads=[r_qT, r_skT], writes=[r_PS[g]])
            P.op("act", lambda e, g=g: e.activation(out=sc[:, g * 4:(g + 1) * 4, :].rearrange("p a b -> p (a b)"),
                                                    in_=PS[g][:], func=AF.Copy),
                 reads=[r_PS[g]], writes=[r_sc])
        for hp in range(16):
            P.op("dve", lambda e, hp=hp: e.max(out=tops[:, hp, 0:8], in_=sc[:, hp, :]), reads=[r_sc], writes=[r_top])
            P.op("dve", lambda e, hp=hp: e.max_index(out=topi[:, hp, 0:8], in_max=tops[:, hp, 0:8], in_values=sc[:, hp, :]),
                 reads=[r_sc, r_top], writes=[r_top])
            P.op("dve", lambda e, hp=hp: e.match_replace(out=sc2[:, hp, :], in_to_replace=tops[:, hp, 0:8],
                                                         in_values=sc[:, hp, :], imm_value=-1e30),
                 reads=[r_sc, r_top], writes=[r_sc2])
            P.op("dve", lambda e, hp=hp: e.max(out=tops[:, hp, 8:16], in_=sc2[:, hp, :]), reads=[r_sc2], writes=[r_top])
            P.op("dve", lambda e, hp=hp: e.max_index(out=topi[:, hp, 8:16], in_max=tops[:, hp, 8:16], in_values=sc2[:, hp, :]),
                 reads=[r_sc2, r_top], writes=[r_top])
        P.op("dve", lambda e: e.tensor_copy(out=topif[:], in_=topi[:]), reads=[r_top], writes=[r_top])
        tv = tops[:].rearrange("p (h t) k -> p h t k", t=2)
        in0 = tv[:, :, 0, :].unsqueeze(3).to_broadcast([128, 8, 16, 16])
        in1 = tv[:, :, 1, :].unsqueeze(2).to_broadcast([128, 8, 16, 16])
        candv = cand[:].rearrange("p h (i j) -> p h i j", j=16)
        P.op("dve", lambda e: e.tensor_tensor(out=candv, in0=in0, in1=in1, op=ALU.add), reads=[r_top], writes=[r_cand])
        for h in range(8):
            P.op("dve", lambda e, h=h: e.max(out=bests[:, h, 0:8], in_=cand[:, h, :]), reads=[r_cand], writes=[r_best])
            P.op("dve", lambda e, h=h: e.max_index(out=bestp[:, h, 0:8], in_max=bests[:, h, 0:8], in_values=cand[:, h, :]),
                 reads=[r_cand, r_best], writes=[r_best])
            P.op("dve", lambda e, h=h: e.match_replace(out=cand2[:, h, :], in_to_replace=bests[:, h, 0:8],
                                                       in_values=cand[:, h, :], imm_value=-1e30),
                 reads=[r_cand, r_best], writes=[r_cand2])
            P.op("dve", lambda e, h=h: e.max(out=bests[:, h, 8:16], in_=cand2[:, h, :]), reads=[r_cand2], writes=[r_best])
            P.op("dve", lambda e, h=h: e.max_index(out=bestp[:, h, 8:16], in_max=bests[:, h, 8:16], in_values=cand2[:, h, :]),
                 reads=[r_cand2, r_best], writes=[r_best])
        P.op("dve", lambda e: e.tensor_single_scalar(out=hiu[:], in_=bestp[:], scalar=4, op=ALU.logical_shift_right),
             reads=[r_best], writes=[r_sel])
        P.op("dve", lambda e: e.tensor_single_scalar(out=lou[:], in_=bestp[:], scalar=15, op=ALU.bitwise_and),
             reads=[r_best], writes=[r_sel])
        P.op("dve", lambda e: e.tensor_copy(out=hif[:], in_=hiu[:]), reads=[r_sel], writes=[r_sel])
        P.op("dve", lambda e: e.tensor_copy(out=lof[:], in_=lou[:]), reads=[r_sel], writes=[r_sel])
        iota_bc = iota16[:].unsqueeze(1).unsqueeze(1).to_broadcast([128, 8, 16, 16])
        tiv = topif[:].rearrange("p (h t) k -> p h t k", t=2)
        for (selin, tsel, outsel) in ((hif, 0, i0s), (lof, 1, i1s)):
            P.op("dve", lambda e, selin=selin: e.tensor_tensor(
                out=oh[:], in0=iota_bc, in1=selin[:].unsqueeze(3).to_broadcast([128, 8, 16, 16]), op=ALU.is_equal),
                reads=[r_sel, r_const], writes=[r_oh])
            P.op("dve", lambda e, tsel=tsel: e.tensor_tensor(
                out=oh[:], in0=oh[:], in1=tiv[:, :, tsel, :].unsqueeze(2).to_broadcast([128, 8, 16, 16]), op=ALU.mult),
                reads=[r_oh, r_top], writes=[r_oh])
            P.op("dve", lambda e, outsel=outsel: e.tensor_reduce(out=outsel[:], in_=oh[:], axis=AX.X, op=ALU.add),
                 reads=[r_oh], writes=[r_sel])
        P.op("dve", lambda e: e.scalar_tensor_tensor(out=expf[:].rearrange("p (h k) -> p h k", k=16), in0=i0s[:],
                                                     scalar=128.0, in1=i1s[:], op0=ALU.mult, op1=ALU.add),
             reads=[r_sel], writes=[r_sel])
        P.op("dve", lambda e: e.tensor_copy(out=idx[b][:], in_=expf[:]), reads=[r_sel], writes=[r_idx[b]])
        P.op("dve", lambda e: e.tensor_tensor(out=gd[:], in0=bests[:], in1=bests[:, :, 0:1].to_broadcast([128, 8, 16]),
                                              op=ALU.subtract), reads=[r_best], writes=[r_gtmp])
        P.op("act", lambda e: e.activation(out=gd[:], in_=gd[:], func=AF.Exp), reads=[r_gtmp], writes=[r_gtmp])
        P.op("dve", lambda e: e.tensor_reduce(out=gsum[:], in_=gd[:], axis=AX.X, op=ALU.add), reads=[r_gtmp], writes=[r_gtmp])
        P.op("dve", lambda e: e.reciprocal(out=gsum[:], in_=gsum[:]), reads=[r_gtmp], writes=[r_gtmp])
        P.op("dve", lambda e: e.tensor_tensor(out=gate[b][:], in0=gd[:], in1=gsum[:].unsqueeze(2).to_broadcast([128, 8, 16]),
                                              op=ALU.mult), reads=[r_gtmp], writes=[r_gate[b]])

    gring = {"i": 0}

    def back(i):
        b = i % 2
        for j in range(128):
            g = gring["i"] % NG
            gring["i"] += 1
            P.dma("pool", lambda e, g=g, j=j: e.indirect_dma_start(
                out=G[g][:], out_offset=None, in_=dr["ub"],
                in_offset=bass.IndirectOffsetOnAxis(ap=idx[b][:, j:j + 1], axis=0)),
                reads=[r_idx[b]], writes=[r_G[g]])
            P.op("dve", lambda e, g=g, j=j: e.scalar_tensor_tensor(
                out=junk[:], in0=G[g][:], scalar=1.0, in1=h2[b][:], op0=ALU.mult, op1=ALU.mult,
                accum_out=dots[:, j:j + 1]),
                reads=[r_G[g], r_h2[b]], writes=[r_junk, r_dots])
        P.op("act", lambda e: e.activation(out=act[:], in_=dots[:], func=AF.Gelu), reads=[r_dots], writes=[r_wgt])
        P.op("dve", lambda e: e.tensor_tensor(out=wgt[:], in0=act[:], in1=gate[b][:].rearrange("p h k -> p (h k)"),
                                              op=ALU.mult), reads=[r_wgt, r_gate[b]], writes=[r_wgt])
        if i == 0 and "dbg" in dr:
            dbg = dr["dbg"]
            P.dma("sp", lambda e: e.dma_start(out=dbg[0], in_=dots[:]), reads=[r_dots])
            P.dma("sp", lambda e: e.dma_start(out=dbg[1], in_=wgt[:]), reads=[r_wgt])
            P.dma("sp", lambda e: e.dma_start(out=dbg[2], in_=expf[:]), reads=[r_sel])
            P.dma("sp", lambda e: e.dma_start(out=dbg[3], in_=gate[b][:].rearrange("p h k -> p (h k)")), reads=[r_gate[b]])
            P.dma("sp", lambda e: e.dma_start(out=dbg[4], in_=bests[:].rearrange("p h k -> p (h k)")), reads=[r_best])
            P.dma("sp", lambda e: e.dma_start(out=dbg[5], in_=topif[:, 0:8, :].rearrange("p h k -> p (h k)")), reads=[r_top])
            P.dma("sp", lambda e: e.dma_start(out=dbg[6], in_=tops[:, 0:8, :].rearrange("p h k -> p (h k)")), reads=[r_top])
            P.dma("sp", lambda e: e.dma_start(out=dbg[7], in_=sc[:, 0, :]), reads=[r_sc])
        for j in range(128):
            g = gring["i"] % NG
            gring["i"] += 1
            P.dma("pool", lambda e, g=g, j=j: e.indirect_dma_start(
                out=G[g][:], out_offset=None, in_=dr["vb"],
                in_offset=bass.IndirectOffsetOnAxis(ap=idx[b][:, j:j + 1], axis=0)),
                reads=[r_idx[b]], writes=[r_G[g]])
            si = j % 4
            P.op("act", lambda e, g=g, j=j, si=si: e.activation(out=scl[si][:], in_=G[g][:], func=AF.Copy,
                                                               scale=wgt[:, j:j + 1]),
                 reads=[r_G[g], r_wgt], writes=[r_scl[si]])
            for hf in range(2):
                P.op("pe", lambda e, j=j, si=si, hf=hf: e.matmul(ACC[hf][:, 0:512], lhsT=identb[:],
                                                                 rhs=scl[si][:, hf * 512:(hf + 1) * 512],
                                                                 start=(j == 0), stop=(j == 127)),
                     reads=[r_scl[si], r_const], writes=[r_ACC])

    def back_fin(i):
        b = i % 2
        bx = i % 3
        for hf in range(2):
            P.op("dve", lambda e, hf=hf: e.tensor_tensor(out=acc[b][:, hf * 512:(hf + 1) * 512],
                                                         in0=xt[bx][:, hf * 512:(hf + 1) * 512], in1=ACC[hf][:, 0:512],
                                                         op=ALU.add), reads=[r_xt[bx], r_ACC], writes=[r_acc[b]])
        yap = dr["y"][i * 128:(i + 1) * 128, :]
        wr = [r_y[i]] + ([x1_res[i]] if x1_res is not None else [])
        return P.dma("sp", lambda e: e.dma_start(out=yap, in_=acc[b][:]), reads=[r_acc[b]], writes=wr)

    out_toks = []
    front(0)
    if ntiles > 1:
        front(1)
    for i in range(ntiles):
        back(i)
        if i + 2 < ntiles:
            front(i + 2)
        out_toks.append(back_fin(i))
    return out_toks


NCORES = 8
SEQ_PER_CORE = 4


def build_program(nseq):
    nc = bass.Bass("TRN2", target_bir_lowering=False)
    nt = nseq * 16

    def din(name, shape, dt=F32):
        return nc.dram_tensor(name, list(shape), dt, kind="ExternalInput").ap()
    dr = {"x": din("x", [nt * 128, 1024]), "w_in": din("w_in", [1024, 4352]), "g1col": din("g1col", [128, 8]),
          "gainqk": din("gainqk", [128, 640]), "sinks_bc": din("sinks_bc", [128, 8]), "cst": din("cst", [5, 128, 128]),
          "swab": din("swab", [128, 2048]), "gbcol": din("gbcol", [128, 16]), "w_up_swa": din("w_up_swa", [512, 1024]),
          "w_up_sb": din("w_up_sb", [512, 1024]), "w_out": din("w_out", [1024, 1024]),
          "wq": din("wq", [1024, 2048]), "g2col": din("g2col", [128, 8]), "g2bc": din("g2bc", [128, 1024]),
          "skT": din("skT", [128, 16, 128]), "u": din("u", [16384, 1024]), "v": din("v", [16384, 1024]),
          "iota16": din("iota16", [128, 16])}
    dr["ident"] = dr["cst"][0]
    dr["y"] = nc.dram_tensor("y", [nt * 128, 1024], F32, kind="ExternalOutput").ap()
    dr["x1"] = dr["y"]
    dr["ysa"] = nc.dram_tensor("ysa", [nt, 128, 512], BF16).ap()
    dr["ysb"] = nc.dram_tensor("ysb", [nt, 64, 1024], BF16).ap()
    dr["ub"] = nc.dram_tensor("ub16", [16384, 1024], BF16).ap()
    dr["vb"] = nc.dram_tensor("vb16", [16384, 1024], BF16).ap()
    P = Prog(nc)
    with ExitStack() as st:
        STACK[0] = st
        phase_a1(P, nc, dr, nseq)
        P.barrier()
        P.replay()
    with ExitStack() as st:
        STACK[0] = st
        phase_a2(P, nc, dr, nt)
        P.barrier()
        P.replay()
    with ExitStack() as st:
        STACK[0] = st
        toks = phase_b(P, nc, dr, nt, None)
        P.wait_tokens("sp", toks)
        P.barrier()
        P.replay()
    return nc


def host_inputs(inputs):
    f = lambda k: np.asarray(inputs[k], np.float32)
    cst, swab = consts()
    g1 = f("mix_norm_gain")[0]
    g2 = f("ffn_norm_gain")[0]
    sk = f("peer_sub_keys")[0]
    shared = {
        "w_in": np.ascontiguousarray(f("w_in")[0]), "g1col": col(g1, 8),
        "gainqk": bc(np.concatenate([np.tile(f("swa_q_gain")[0], 8), np.tile(f("swa_k_gain")[0], 2)])),
        "sinks_bc": bc(f("swa_sinks")[0]), "cst": cst, "swab": swab, "gbcol": col(f("gate_bias")[0], 16),
        "w_up_swa": np.ascontiguousarray(f("w_up_swa")[0]), "w_up_sb": np.ascontiguousarray(f("w_up_sb")[0]),
        "w_out": np.ascontiguousarray(f("w_out")[0]), "wq": np.ascontiguousarray(f("peer_w_q")[0]),
        "g2col": col(g2, 8), "g2bc": bc(g2),
        "skT": np.ascontiguousarray(sk.reshape(16, 128, 128).transpose(2, 0, 1)),
        "u": np.ascontiguousarray(f("peer_u")[0]), "v": np.ascontiguousarray(f("peer_v")[0]),
        "iota16": bc(np.arange(16, dtype=np.float32)),
    }
    return shared


def kernel(**inputs):
    x = np.asarray(inputs["x"], np.float32)
    B, S_, D_ = x.shape
    nseq = B // NCORES
    shared = host_inputs(inputs)
    nc = build_program(nseq)
    in_maps = []
    for c in range(NCORES):
        m = dict(shared)
        m["x"] = np.ascontiguousarray(x[c * nseq:(c + 1) * nseq].reshape(nseq * S_, D_))
        in_maps.append(m)
    res = run_bass_kernel_spmd(nc, in_maps, core_ids=list(range(NCORES)))
    out = np.concatenate([np.asarray(r["y"], np.float32).reshape(nseq, S_, D_) for r in res.results], axis=0)
    return out
```

```python
from contextlib import ExitStack
import numpy as np
import concourse.bass as bass
import concourse.mybir as mybir
from concourse.bass_utils import run_bass_kernel_spmd


F32 = mybir.dt.float32
BF16 = mybir.dt.bfloat16
I32 = mybir.dt.int32
U32 = mybir.dt.uint32
AF = mybir.ActivationFunctionType
ALU = mybir.AluOpType
AX = mybir.AxisListType

SAME_ENGINE_SYNC = True
N_DMA_SEMS = {"sp": 12, "pool": 24, "act": 2}


class Res:
    __slots__ = ("name", "w", "r", "open")

    def __init__(self, name):
        self.name = name
        self.w = None
        self.r = {}
        self.open = False


class Ring:
    def __init__(self, items):
        self.items = list(items)
        self.i = 0

    def get(self):
        it = self.items[self.i % len(self.items)]
        self.i += 1
        res = it[0] if isinstance(it, tuple) else it
        assert not res.open, f"ring slot {res.name} still open"
        res.open = True
        return it


def release(res):
    res.open = False


class Prog:
    ENG = ("pe", "dve", "act", "pool", "sp")

    def __init__(self, nc):
        self.nc = nc
        self.ops = {e: [] for e in self.ENG}
        self.cnt = {e: 0 for e in self.ENG}
        self.seen = {e: {} for e in self.ENG}
        self.sems = {}
        for e in ("pe", "dve", "act", "pool"):
            self.sems[e] = nc.alloc_semaphore(f"s_{e}")
        self.dma_pool = {}
        for q, n in N_DMA_SEMS.items():
            self.dma_pool[q] = {"i": 0, "sems": []}
            for k in range(n):
                key = ("dma", q, k)
                self.sems[key] = nc.alloc_semaphore(f"s_dma_{q}_{k}")
                self.dma_pool[q]["sems"].append([key, 0])
        self.n_wait = 0

    def _deps(self, eng, reads, writes):
        deps = {}

        def add(tok):
            if tok is None:
                return
            k, v = tok
            if deps.get(k, 0) < v:
                deps[k] = v
        for r in reads:
            add(r.w)
        for w in writes:
            add(w.w)
            for k, v in w.r.items():
                add((k, v))
        out = []
        for k, v in deps.items():
            if k == eng and (eng == "pe" or not SAME_ENGINE_SYNC):
                continue
            if self.seen[eng].get(k, 0) >= v:
                continue
            self.seen[eng][k] = v
            out.append((k, v))
        return out

    def _update(self, tok, reads, writes):
        k, v = tok
        for r in reads:
            if r.r.get(k, 0) < v:
                r.r[k] = v
        for w in writes:
            w.w = tok
            w.r = {}

    def op(self, eng, fn, reads=(), writes=()):
        waits = self._deps(eng, reads, writes)
        self.cnt[eng] += 1
        tok = (eng, self.cnt[eng])
        self.ops[eng].append((waits, fn, (eng, 1)))
        self._update(tok, reads, writes)
        self.n_wait += len(waits)
        return tok

    def dma(self, q, fn, reads=(), writes=()):
        pool = self.dma_pool[q]
        slot = pool["sems"][pool["i"] % len(pool["sems"])]
        pool["i"] += 1
        key, cnt = slot
        waits = self._deps(q, reads, writes)
        if cnt > 0 and self.seen[q].get(key, 0) < 16 * cnt:
            self.seen[q][key] = 16 * cnt
            waits.append((key, 16 * cnt))
        slot[1] = cnt + 1
        tok = (key, 16 * (cnt + 1))
        self.ops[q].append((waits, fn, (key, 16)))
        self._update(tok, reads, writes)
        return tok

    def wait_tokens(self, eng, toks):
        waits = []
        for k, v in toks:
            if self.seen[eng].get(k, 0) >= v:
                continue
            self.seen[eng][k] = v
            waits.append((k, v))
        if waits:
            self.ops[eng].append((waits, None, None))

    def all_tokens(self):
        toks = []
        for e in ("pe", "dve", "act", "pool"):
            if self.cnt[e] > 0:
                toks.append((e, self.cnt[e]))
        for q, pool in self.dma_pool.items():
            for key, cnt in pool["sems"]:
                if cnt > 0:
                    toks.append((key, 16 * cnt))
        return toks

    def barrier(self):
        toks = self.all_tokens()
        for e in self.ENG:
            self.wait_tokens(e, [t for t in toks if not (e == 'pe' and t[0] == 'pe')])

    def replay(self):
        nc = self.nc
        P = self
        with nc.Block() as block:
            def run(e, name):
                for waits, fn, inc in P.ops[name]:
                    for k, v in waits:
                        e.wait_ge(P.sems[k], v)
                    if fn is not None:
                        ins = fn(e)
                        ins.then_inc(P.sems[inc[0]], inc[1])

            @block.sync
            def _(e):
                run(e, "sp")

            @block.tensor
            def _(e):
                run(e, "pe")

            @block.vector
            def _(e):
                run(e, "dve")

            @block.scalar
            def _(e):
                run(e, "act")

            @block.gpsimd
            def _(e):
                run(e, "pool")
        for e in self.ENG:
            self.ops[e] = []


STACK = [None]


def sb(nc, name, shape, dt):
    return STACK[0].enter_context(nc.sbuf_tensor(name, list(shape), dt))


def ps(nc, name, shape, dt):
    return STACK[0].enter_context(nc.psum_tensor(name, list(shape), dt))


def consts():
    j = np.arange(128)[:, None]; t = np.arange(128)[None, :]
    ident = np.eye(128, dtype=np.float32)
    tri = (j >= t).astype(np.float32)
    ones = np.ones((128, 128), np.float32)
    maskL = (j < t).astype(np.float32)
    maskb = np.where(j < t, 0.0, 30000.0).astype(np.float32)
    cst = np.stack([ident, tri, ones, maskL, maskb]).astype(np.float32)
    slopes = 2.0 ** (-8.0 * np.arange(1, 9) / 8)
    k = np.arange(128)[:, None]; q = np.arange(128)[None, :]
    swab = np.zeros((128, 8, 2, 128), np.float32)
    for kt in range(2):
        spos = (kt - 1) * 128 + k
        tpos = q
        ck = spos // 64
        cq = tpos // 64
        vis = (ck <= cq) & (ck >= cq - 2)
        dist = np.abs(tpos - spos).astype(np.float64)
        for h in range(8):
            b = np.where(vis, -slopes[h] * dist * 8.0, -240000.0)
            swab[:, h, kt, :] = b
    return cst, np.ascontiguousarray(swab.reshape(128, 2048))
def col(v, n):
    return np.ascontiguousarray(np.asarray(v, np.float32).reshape(n, 128).T)
def bc(v):
    v = np.asarray(v, np.float32).reshape(1, -1)
    return np.ascontiguousarray(np.broadcast_to(v, (128, v.shape[1])))


D = 1024
S = 2048
NT = 16
BATCH = 2
SKIP_SB = set()
SKIP_SWA = set()


def phase_a1(P, nc, dr, nseq):
    R = lambda n: Res(n)
    win = sb(nc, "a_win", [128, 8, 2304], BF16)
    stg = [sb(nc, f"a_stg{i}", [128, 1152], F32) for i in range(2)]
    g1col = sb(nc, "a_g1col", [128, 8], F32)
    gainqk = sb(nc, "a_gainqk", [128, 640], F32)
    esink = sb(nc, "a_esink", [128, 8], F32)
    epsc = sb(nc, "a_eps", [128, 1], F32)
    cstf = sb(nc, "a_cstf", [128, 5, 128], F32)
    cstb = sb(nc, "a_cstb", [128, 5, 128], BF16)
    identb, trib, onesb, maskLb, maskbb = (cstb[:, k, :] for k in range(5))
    swabf = sb(nc, "a_swabf", [128, 2048], F32)
    swab = sb(nc, "a_swab", [128, 8, 2, 128], BF16)
    kaT = sb(nc, "a_kaT", [64, 2, S], BF16)
    kbT = sb(nc, "a_kbT", [128, 4, S], BF16)
    vb = sb(nc, "a_vb", [128, NT, 512], BF16)
    vaug = sb(nc, "a_vaug", [128, NT, 2, 65], BF16)
    xa = [sb(nc, f"a_xa{i}", [128, D], F32) for i in range(2)]
    xs = sb(nc, "a_xs", [128, D], BF16)
    junk = sb(nc, "a_junk", [128, D], BF16)
    hT = sb(nc, "a_hT", [128, 8, 128], BF16)
    sq = sb(nc, "a_sq", [128, 640], F32)
    qtmp = sb(nc, "a_qtmp", [128, 640], F32)
    qn = sb(nc, "a_qn", [128, 640], BF16)
    ssq = sb(nc, "a_ssq", [128, 10], F32)
    rq = sb(nc, "a_rq", [128, 10], F32)
    ss = sb(nc, "a_ss", [128, 1], F32)
    lnv = sb(nc, "a_lnv", [128, 1], F32)
    rstd = sb(nc, "a_rstd", [128, 1], F32)
    qaT = sb(nc, "a_qaT", [64, 8, 128], BF16)
    qbT = sb(nc, "a_qbT", [128, 4, 128], BF16)
    qbTn = sb(nc, "a_qbTn", [128, 4, 128], BF16)
    PT = [sb(nc, f"a_PT{i}", [128, 2, 2, 128], BF16) for i in range(2)]
    dn = sb(nc, "a_dn", [128, 4, 1], F32)
    ya = sb(nc, "a_ya", [128, 512], BF16)
    yaT = [sb(nc, f"a_yaT{i}", [128, 4, 128], BF16) for i in range(2)]
    ybT = [sb(nc, f"a_ybT{i}", [64, 8, 128], BF16) for i in range(2)]
    Eb = [sb(nc, f"a_E{i}", [128, 512], F32) for i in range(2)]
    Lb = [sb(nc, f"a_L{i}", [128, 512], BF16) for i in range(2)]
    Wb = [sb(nc, f"a_W{i}", [128, 512], BF16) for i in range(2)]
    Sb = sb(nc, "a_S", [128, 17, 128], BF16)
    TR = ps(nc, "a_TR", [128, 1024], BF16)
    PSR = [ps(nc, f"a_PS{i}", [128, 512], F32) for i in range(4)]
    OUT = [ps(nc, f"a_OUT{i}", [128, 512], F32) for i in range(2)]
    cin = [sb(nc, f"a_cin{i}", [128, 2048], F32) for i in range(2)]
    cout = [sb(nc, f"a_cout{i}", [128, 2048], BF16) for i in range(2)]
    r_cin = [R("cin0"), R("cin1")]
    r_cout = [R("cout0"), R("cout1")]
    conv_state = {"k": 0}

    def convert_chunks(n):
        for _ in range(n):
            k = conv_state["k"]
            if k >= 128:
                return
            conv_state["k"] = k + 1
            src, dst = (dr["u"], dr["ub"]) if k < 64 else (dr["v"], dr["vb"])
            r0 = (k % 64) * 256
            s_ap = src[r0:r0 + 256, :].rearrange("(p i) d -> p (i d)", i=2)
            d_ap = dst[r0:r0 + 256, :].rearrange("(p i) d -> p (i d)", i=2)
            bi = k % 2
            P.dma("pool", lambda e, s_ap=s_ap, bi=bi: e.dma_start(out=cin[bi][:], in_=s_ap), writes=[r_cin[bi]])
            P.op("pool", lambda e, bi=bi: e.tensor_copy(out=cout[bi][:], in_=cin[bi][:]), reads=[r_cin[bi]], writes=[r_cout[bi]])
            P.dma("pool", lambda e, d_ap=d_ap, bi=bi: e.dma_start(out=d_ap, in_=cout[bi][:]), reads=[r_cout[bi]])

    r_win, r_const = R("win"), R("const")
    r_stg = [R("stg0"), R("stg1")]
    r_kaT, r_kbT, r_vb, r_vaug = R("kaT"), R("kbT"), R("vb"), R("vaug")
    r_xa = [R("xa0"), R("xa1")]
    r_xs, r_junk, r_hT, r_sq, r_qtmp, r_qn, r_norm, r_qnorm = (R(n) for n in
                                                               ("xs", "junk", "hT", "sq", "qtmp", "qn", "norm", "qnorm"))
    r_qaT, r_qbT = R("qaT"), R("qbT")
    r_PT = [R("PT0"), R("PT1")]
    r_dn, r_ya = R("dn"), R("ya")
    r_yaT = [R("yaT0"), R("yaT1")]
    r_ybT = [R("ybT0"), R("ybT1")]
    r_S = [R(f"S{k}") for k in range(17)]
    r_TR = R("TR")
    r_OUT = [R("OUT0"), R("OUT1")]
    psr = Ring([(R(f"PS{i}"), PSR[i]) for i in range(4)])
    ering = Ring([(R(f"E{i}"), Eb[i]) for i in range(2)])
    lring = Ring([(R(f"L{i}"), Lb[i]) for i in range(2)])
    wring = Ring([(R(f"W{i}"), Wb[i]) for i in range(2)])

    P.dma("sp", lambda e: e.dma_start(out=g1col[:], in_=dr["g1col"]), writes=[r_const])
    P.dma("sp", lambda e: e.dma_start(out=gainqk[:], in_=dr["gainqk"]), writes=[r_const])
    P.dma("sp", lambda e: e.dma_start(out=esink[:], in_=dr["sinks_bc"]), writes=[r_const])
    P.dma("sp", lambda e: e.dma_start(out=cstf[:], in_=dr["cst"][0:5].rearrange("k p n -> p k n")), writes=[r_const])
    P.dma("sp", lambda e: e.dma_start(out=swabf[:], in_=dr["swab"]), writes=[r_const])
    P.op("act", lambda e: e.activation(out=esink[:], in_=esink[:], func=AF.Exp), reads=[r_const], writes=[r_const])
    P.op("dve", lambda e: e.tensor_copy(out=cstb[:], in_=cstf[:]), reads=[r_const], writes=[r_const])
    P.op("dve", lambda e: e.tensor_copy(out=swab[:].rearrange("p a b c -> p (a b c)"), in_=swabf[:]),
         reads=[r_const], writes=[r_const])
    P.op("dve", lambda e: e.memset(epsc[:], 1e-6), writes=[r_const])
    P.op("dve", lambda e: e.memset(vaug[:], 1.0), writes=[r_vaug])
    k = 0
    for kc in range(8):
        for hf in range(2):
            s = k % 2
            k += 1
            P.dma("sp", lambda e, kc=kc, hf=hf, s=s: e.dma_start(
                out=stg[s][:], in_=dr["w_in"][kc * 128:(kc + 1) * 128, hf * 1152:(hf + 1) * 1152]), writes=[r_stg[s]])
            P.op("dve", lambda e, kc=kc, hf=hf, s=s: e.tensor_scalar(
                out=win[:, kc, hf * 1152:(hf + 1) * 1152], in0=stg[s][:], scalar1=g1col[:, kc:kc + 1], scalar2=None,
                op0=ALU.mult), reads=[r_stg[s], r_const], writes=[r_win])

    def front(sq_i, tt):
        gt = sq_i * NT + tt
        b = gt % 2
        xap = dr["x"][gt * 128:(gt + 1) * 128, :]
        P.dma("sp", lambda e: e.dma_start(out=xa[b][:], in_=xap), writes=[r_xa[b]])
        P.op("act", lambda e: e.activation(out=junk[:], in_=xa[b][:], func=AF.Square, accum_out=ss[:]),
             reads=[r_xa[b]], writes=[r_junk, r_norm])
        P.op("act", lambda e: e.activation(out=lnv[:], in_=ss[:], func=AF.Ln, scale=1.0 / D, bias=epsc[:]),
             reads=[r_norm, r_const], writes=[r_norm])
        P.op("act", lambda e: e.activation(out=rstd[:], in_=lnv[:], func=AF.Exp, scale=-0.5), reads=[r_norm], writes=[r_norm])
        P.op("dve", lambda e: e.tensor_scalar(out=xs[:], in0=xa[b][:], scalar1=rstd[:], scalar2=None, op0=ALU.mult),
             reads=[r_xa[b], r_norm], writes=[r_xs])
        for kc in range(8):
            P.op("pe", lambda e, kc=kc: e.transpose(out=TR[:, kc * 128:(kc + 1) * 128], in_=xs[:, kc * 128:(kc + 1) * 128],
                                                    identity=identb), reads=[r_xs, r_const], writes=[r_TR])
        P.op("act", lambda e: e.activation(out=hT[:].rearrange("p a b -> p (a b)"), in_=TR[:], func=AF.Copy),
             reads=[r_TR], writes=[r_hT])
        rA1, A1 = psr.get()
        rA2, A2 = psr.get()
        rA3, A3 = psr.get()
        for (rr, bank, c0, n) in ((rA1, A1, 0, 512), (rA2, A2, 512, 256), (rA3, A3, 1792, 512)):
            for kc in range(8):
                P.op("pe", lambda e, bank=bank, c0=c0, n=n, kc=kc: e.matmul(
                    bank[:, 0:n], lhsT=hT[:, kc, :], rhs=win[:, kc, c0:c0 + n], start=(kc == 0), stop=(kc == 7)),
                    reads=[r_hT, r_win], writes=[rr])
        P.op("act", lambda e: e.activation(out=vb[:, tt, :], in_=A3[:, 0:512], func=AF.Copy), reads=[rA3], writes=[r_vb])
        release(rA3)
        P.op("act", lambda e: e.activation(out=vaug[:, tt, :, 0:64], in_=A2[:, 128:256].rearrange("p (g d) -> p g d", d=64),
                                           func=AF.Copy), reads=[rA2], writes=[r_vaug])
        P.op("act", lambda e: e.activation(out=sq[:, 0:512], in_=A1[:, 0:512], func=AF.Square), reads=[rA1], writes=[r_sq])
        P.op("act", lambda e: e.activation(out=sq[:, 512:640], in_=A2[:, 0:128], func=AF.Square), reads=[rA2], writes=[r_sq])
        P.op("dve", lambda e: e.tensor_reduce(out=ssq[:], in_=sq[:].rearrange("p (h d) -> p h d", d=64), axis=AX.X, op=ALU.add),
             reads=[r_sq], writes=[r_qnorm])
        P.op("act", lambda e: e.activation(out=rq[:], in_=ssq[:], func=AF.Ln, scale=1.0 / 64, bias=epsc[:]),
             reads=[r_qnorm, r_const], writes=[r_qnorm])
        P.op("act", lambda e: e.activation(out=rq[:], in_=rq[:], func=AF.Exp, scale=-0.5), reads=[r_qnorm], writes=[r_qnorm])
        P.op("dve", lambda e: e.tensor_tensor(out=qtmp[:, 0:512].rearrange("p (h d) -> p h d", d=64),
                                              in0=A1[:, 0:512].rearrange("p (h d) -> p h d", d=64),
                                              in1=rq[:, 0:8].unsqueeze(2).to_broadcast([128, 8, 64]), op=ALU.mult),
             reads=[rA1, r_qnorm], writes=[r_qtmp])
        P.op("dve", lambda e: e.tensor_tensor(out=qtmp[:, 512:640].rearrange("p (h d) -> p h d", d=64),
                                              in0=A2[:, 0:128].rearrange("p (h d) -> p h d", d=64),
                                              in1=rq[:, 8:10].unsqueeze(2).to_broadcast([128, 2, 64]), op=ALU.mult),
             reads=[rA2, r_qnorm], writes=[r_qtmp])
        release(rA1)
        release(rA2)
        P.op("dve", lambda e: e.tensor_tensor(out=qn[:], in0=qtmp[:], in1=gainqk[:], op=ALU.mult),
             reads=[r_qtmp, r_const], writes=[r_qn])
        for h in range(8):
            P.op("pe", lambda e, h=h: e.transpose(out=TR[0:64, h * 128:(h + 1) * 128], in_=qn[:, h * 64:(h + 1) * 64],
                                                  identity=identb), reads=[r_qn, r_const], writes=[r_TR])
        P.op("act", lambda e: e.activation(out=qaT[:].rearrange("p a b -> p (a b)"), in_=TR[0:64, 0:1024], func=AF.Copy),
             reads=[r_TR], writes=[r_qaT])

    def front_k(tt):
        for g in range(2):
            P.op("pe", lambda e, g=g: e.transpose(out=TR[0:64, g * 128:(g + 1) * 128], in_=qn[:, 512 + g * 64:512 + (g + 1) * 64],
                                                  identity=identb), reads=[r_qn, r_const], writes=[r_TR])
        P.op("act", lambda e: e.activation(out=kaT[:, :, tt * 128:(tt + 1) * 128],
                                           in_=TR[0:64, 0:256].rearrange("p (g t) -> p g t", t=128), func=AF.Copy),
             reads=[r_TR], writes=[r_kaT])

    def front_b(tt):
        for half, c0 in ((0, 768), (1, 1280)):
            rB, B = psr.get()
            for c in range(4):
                for kc in range(8):
                    P.op("pe", lambda e, B=B, c=c, kc=kc, c0=c0: e.matmul(
                        B[:, c * 128:(c + 1) * 128], lhsT=win[:, kc, c0 + c * 128:c0 + (c + 1) * 128], rhs=hT[:, kc, :],
                        start=(kc == 0), stop=(kc == 7)), reads=[r_win, r_hT], writes=[rB])
            if half == 0:
                P.op("act", lambda e, B=B: e.activation(out=qbT[:].rearrange("p a b -> p (a b)"), in_=B[:], func=AF.Copy,
                                                        scale=0.125), reads=[rB], writes=[r_qbT])
                P.op("act", lambda e, B=B: e.activation(out=qbTn[:].rearrange("p a b -> p (a b)"), in_=B[:], func=AF.Copy,
                                                        scale=-0.125), reads=[rB], writes=[r_qbT])
            else:
                P.op("act", lambda e, B=B: e.activation(out=kbT[:, :, tt * 128:(tt + 1) * 128],
                                                        in_=B[:].rearrange("p (c t) -> p c t", t=128), func=AF.Copy),
                     reads=[rB], writes=[r_kbT])
            release(rB)

    def swa(gt, tt):
        yb = gt % 2
        kts = [1] if tt == 0 else [0, 1]
        rOA = OAb = None
        for hb in range(4):
            g = hb // 2
            rST, ST = psr.get()
            ptb = hb % 2
            for hh in range(2):
                h = 2 * hb + hh
                for kt in kts:
                    ktile = tt - 1 + kt
                    o = (hh * 2 + kt) * 128
                    P.op("pe", lambda e, ST=ST, o=o, g=g, h=h, ktile=ktile: e.matmul(
                        ST[:, o:o + 128], lhsT=kaT[0:64, g, ktile * 128:(ktile + 1) * 128], rhs=qaT[0:64, h, :],
                        start=True, stop=False), reads=[r_kaT, r_qaT], writes=[rST])
                    P.op("pe", lambda e, ST=ST, o=o, h=h, kt=kt: e.matmul(
                        ST[:, o:o + 128], lhsT=identb, rhs=swab[:, h, kt, :], start=False, stop=True),
                        reads=[r_const], writes=[rST])
            if tt == 0:
                for hh in range(2):
                    o = (hh * 2 + 1) * 128
                    P.op("act", lambda e, ST=ST, o=o, hh=hh, ptb=ptb: e.activation(
                        out=PT[ptb][:, hh, 1, :], in_=ST[:, o:o + 128], func=AF.Exp, scale=0.125),
                        reads=[rST], writes=[r_PT[ptb]])
            else:
                P.op("act", lambda e, ST=ST, ptb=ptb: e.activation(
                    out=PT[ptb][:].rearrange("p a b c -> p (a b c)"), in_=ST[:], func=AF.Exp, scale=0.125),
                    reads=[rST], writes=[r_PT[ptb]])
            release(rST)
            if hb % 2 == 0:
                rOA, OAb = psr.get()
            for hh in range(2):
                h = 2 * hb + hh
                o = (h % 4) * 65
                for ki, kt in enumerate(kts):
                    ktile = tt - 1 + kt
                    P.op("pe", lambda e, OAb=OAb, o=o, hh=hh, kt=kt, ktile=ktile, g=g, ptb=ptb, ki=ki: e.matmul(
                        OAb[:, o:o + 65], lhsT=PT[ptb][:, hh, kt, :], rhs=vaug[:, ktile, g, :],
                        start=(ki == 0), stop=(ki == len(kts) - 1)), reads=[r_PT[ptb], r_vaug], writes=[rOA])
            if hb % 2 == 1:
                hs = (hb // 2) * 4
                oav = OAb[:, 0:260].rearrange("p (h d) -> p h d", d=65)
                P.op("dve", lambda e, oav=oav, hs=hs: e.tensor_tensor(
                    out=dn[:], in0=oav[:, :, 64:65], in1=esink[:, hs:hs + 4].unsqueeze(2), op=ALU.add),
                    reads=[rOA, r_const], writes=[r_dn])
                P.op("dve", lambda e: e.reciprocal(out=dn[:], in_=dn[:]), reads=[r_dn], writes=[r_dn])
                P.op("dve", lambda e, oav=oav, hs=hs: e.tensor_tensor(
                    out=ya[:, hs * 64:(hs + 4) * 64].rearrange("p (h d) -> p h d", d=64), in0=oav[:, :, 0:64],
                    in1=dn[:].to_broadcast([128, 4, 64]), op=ALU.mult), reads=[rOA, r_dn], writes=[r_ya])
                release(rOA)
        for c in range(4):
            P.op("pe", lambda e, c=c: e.transpose(out=TR[:, c * 128:(c + 1) * 128], in_=ya[:, c * 128:(c + 1) * 128],
                                                  identity=identb), reads=[r_ya, r_const], writes=[r_TR])
        P.op("act", lambda e: e.activation(out=yaT[yb][:].rearrange("p a b -> p (a b)"), in_=TR[:, 0:512], func=AF.Copy),
             reads=[r_TR], writes=[r_yaT[yb]])
        P.dma("sp", lambda e: e.dma_start(out=dr["ysa"][gt], in_=yaT[yb][:].rearrange("p a b -> p (a b)")),
              reads=[r_yaT[yb]])

    def sbattn(gt, tt):
        yb = gt % 2
        for h in range(8):
            c = h // 2
            pb = (h % 2) * 64
            OUTb, rOUT = OUT[h % 2], r_OUT[h % 2]
            kbs = list(range(tt, -1, -1))
            batches = [kbs[i:i + BATCH] for i in range(0, len(kbs), BATCH)]
            nmm = len(kbs)
            mmi = 0
            for batch in batches:
                nb = len(batch)
                rZ, Z = psr.get()
                for b, kb in enumerate(batch):
                    P.op("pe", lambda e, Z=Z, b=b, kb=kb, c=c, pb=pb: e.matmul(
                        Z[:, b * 128:(b + 1) * 128], lhsT=kbT[pb:pb + 64, c, kb * 128:(kb + 1) * 128],
                        rhs=qbT[pb:pb + 64, c, :], start=True, stop=True), reads=[r_kbT, r_qbT], writes=[rZ])
                rE, E = ering.get()
                P.op("act", lambda e, Z=Z, E=E, nb=nb: e.activation(out=E[:, 0:nb * 128], in_=Z[:, 0:nb * 128], func=AF.Exp),
                     reads=[rZ], writes=[rE])
                release(rZ)
                rL, L = lring.get()
                P.op("act", lambda e, E=E, L=L, nb=nb: e.activation(out=L[:, 0:nb * 128], in_=E[:, 0:nb * 128], func=AF.Ln,
                                                                     bias=1.0), reads=[rE], writes=[rL])
                release(rE)
                for b, kb in enumerate(batch):
                    if kb == tt:
                        P.op("dve", lambda e, L=L, b=b: e.tensor_tensor(out=Sb[:, tt, :], in0=L[:, b * 128:(b + 1) * 128],
                                                                        in1=maskLb, op=ALU.mult),
                             reads=[rL, r_const], writes=[r_S[tt]])
                    elif kb >= 1:
                        P.op("dve", lambda e, L=L, b=b, kb=kb: e.tensor_tensor(out=Sb[:, kb, :], in0=Sb[:, kb + 1, :],
                                                                               in1=L[:, b * 128:(b + 1) * 128], op=ALU.add),
                             reads=[rL, r_S[kb + 1]], writes=[r_S[kb]])
                rC, C = psr.get()
                for b, kb in enumerate(batch):
                    o = b * 128
                    if kb == tt:
                        P.op("pe", lambda e, C=C, o=o: e.matmul(C[:, o:o + 128], lhsT=trib, rhs=Sb[:, tt, :], start=True, stop=False),
                             reads=[r_const, r_S[tt]], writes=[rC])
                    else:
                        P.op("pe", lambda e, C=C, o=o, L=L: e.matmul(C[:, o:o + 128], lhsT=trib, rhs=L[:, o:o + 128],
                                                                     start=True, stop=False), reads=[r_const, rL], writes=[rC])
                        P.op("pe", lambda e, C=C, o=o, kb=kb: e.matmul(C[:, o:o + 128], lhsT=onesb, rhs=Sb[:, kb + 1, :],
                                                                       start=False, stop=False),
                             reads=[r_const, r_S[kb + 1]], writes=[rC])
                    P.op("pe", lambda e, C=C, o=o, kb=kb, c=c, pb=pb: e.matmul(
                        C[:, o:o + 128], lhsT=kbT[pb:pb + 64, c, kb * 128:(kb + 1) * 128], rhs=qbTn[pb:pb + 64, c, :],
                        start=False, stop=(kb != tt)), reads=[r_kbT, r_qbT], writes=[rC])
                    if kb == tt:
                        P.op("pe", lambda e, C=C, o=o: e.matmul(C[:, o:o + 128], lhsT=identb, rhs=maskbb, start=False, stop=True),
                             reads=[r_const], writes=[rC])
                release(rL)
                rW, W = wring.get()
                P.op("act", lambda e, C=C, W=W, nb=nb: e.activation(out=W[:, 0:nb * 128], in_=C[:, 0:nb * 128], func=AF.Exp,
                                                                     scale=-1.0), reads=[rC], writes=[rW])
                release(rC)
                for b, kb in enumerate(batch):
                    P.op("pe", lambda e, W=W, b=b, kb=kb, h=h, OUTb=OUTb, mmi=mmi: e.matmul(
                        OUTb[0:64, 0:128], lhsT=vb[:, kb, h * 64:(h + 1) * 64], rhs=W[:, b * 128:(b + 1) * 128],
                        start=(mmi == 0), stop=(mmi == nmm - 1)), reads=[r_vb, rW], writes=[rOUT])
                    mmi += 1
                release(rW)
            P.op("act", lambda e, OUTb=OUTb, h=h: e.activation(out=ybT[yb][:, h, :], in_=OUTb[0:64, 0:128], func=AF.Copy),
                 reads=[rOUT], writes=[r_ybT[yb]])
        P.dma("sp", lambda e: e.dma_start(out=dr["ysb"][gt], in_=ybT[yb][:].rearrange("p a b -> p (a b)")),
              reads=[r_ybT[yb]])

    for sq_i in range(nseq):
        for tt in range(NT):
            gt = sq_i * NT + tt
            convert_chunks((128 + nseq * NT - 1) // (nseq * NT))
            front(sq_i, tt)
            front_k(tt)
            front_b(tt)
            if tt not in SKIP_SWA:
                swa(gt, tt)
            if tt not in SKIP_SB:
                sbattn(gt, tt)
            if tt in SKIP_SWA:
                pass


def phase_a2(P, nc, dr, ntiles):
    R = lambda n: Res(n)
    wg = sb(nc, "m_wg", [128, 8, 2048], BF16)
    wupa = sb(nc, "m_wupa", [128, 4, 1024], BF16)
    wupb = sb(nc, "m_wupb", [64, 8, 1024], BF16)
    wout = sb(nc, "m_wout", [128, 8, 1024], BF16)
    stg = [sb(nc, f"m_stg{i}", [128, 1024], F32) for i in range(2)]
    g1col = sb(nc, "m_g1col", [128, 8], F32)
    ngb = sb(nc, "m_ngb", [128, 16], F32)
    identf = sb(nc, "m_identf", [128, 128], F32)
    identb = sb(nc, "m_identb", [128, 128], BF16)
    epsc = sb(nc, "m_eps", [128, 1], F32)
    xa = [sb(nc, f"m_xa{i}", [128, D], F32) for i in range(2)]
    xs = sb(nc, "m_xs", [128, D], BF16)
    junk = sb(nc, "m_junk", [128, D], BF16)
    hT = sb(nc, "m_hT", [128, 8, 128], BF16)
    ss = sb(nc, "m_ss", [128, 1], F32)
    lnv = sb(nc, "m_lnv", [128, 1], F32)
    rstd = sb(nc, "m_rstd", [128, 1], F32)
    yaT = [sb(nc, f"m_yaT{i}", [128, 4, 128], BF16) for i in range(2)]
    ybT = [sb(nc, f"m_ybT{i}", [64, 8, 128], BF16) for i in range(2)]
    eg = [sb(nc, f"m_eg{i}", [128, 2, 128], F32) for i in range(2)]
    tmp = [sb(nc, f"m_tmp{i}", [128, 2, 128], F32) for i in range(2)]
    mT = sb(nc, "m_mT", [128, 8, 128], BF16)
    x1 = [sb(nc, f"m_x1{i}", [128, D], F32) for i in range(2)]
    TR = ps(nc, "m_TR", [128, 1024], BF16)
    PSR = [ps(nc, f"m_PS{i}", [128, 512], F32) for i in range(6)]

    r_w, r_const = R("w"), R("const")
    r_stg = [R("stg0"), R("stg1")]
    r_xa = [R("xa0"), R("xa1")]
    r_xs, r_junk, r_hT, r_norm, r_mT, r_TR = (R(n) for n in ("xs", "junk", "hT", "norm", "mT", "TR"))
    r_yaT = [R("yaT0"), R("yaT1")]
    r_ybT = [R("ybT0"), R("ybT1")]
    r_eg = [R("eg0"), R("eg1")]
    r_tmp = [R("tmp0"), R("tmp1")]
    r_x1 = [R("x10"), R("x11")]
    psr = Ring([(R(f"PS{i}"), PSR[i]) for i in range(6)])

    P.dma("sp", lambda e: e.dma_start(out=g1col[:], in_=dr["g1col"]), writes=[r_const])
    P.dma("sp", lambda e: e.dma_start(out=ngb[:], in_=dr["gbcol"]), writes=[r_const])
    P.dma("sp", lambda e: e.dma_start(out=identf[:], in_=dr["ident"]), writes=[r_const])
    P.op("dve", lambda e: e.tensor_copy(out=identb[:], in_=identf[:]), reads=[r_const], writes=[r_const])
    P.op("dve", lambda e: e.tensor_scalar(out=ngb[:], in0=ngb[:], scalar1=-1.0, scalar2=None, op0=ALU.mult),
         reads=[r_const], writes=[r_const])
    P.op("dve", lambda e: e.memset(epsc[:], 1e-6), writes=[r_const])
    k = 0

    def load(dst_ap, src_ap, npart, scalar=None):
        nonlocal k
        s = k % 2
        k += 1
        P.dma("sp", lambda e: e.dma_start(out=stg[s][0:npart, :], in_=src_ap), writes=[r_stg[s]])
        if scalar is None:
            P.op("dve", lambda e: e.tensor_copy(out=dst_ap, in_=stg[s][0:npart, :]), reads=[r_stg[s]], writes=[r_w])
        else:
            P.op("dve", lambda e: e.tensor_scalar(out=dst_ap, in0=stg[s][0:npart, :], scalar1=scalar, scalar2=None,
                                                  op0=ALU.mult), reads=[r_stg[s], r_const], writes=[r_w])
    for kc in range(8):
        for hf in range(2):
            load(wg[:, kc, hf * 1024:(hf + 1) * 1024], dr["w_in"][kc * 128:(kc + 1) * 128, 2304 + hf * 1024:2304 + (hf + 1) * 1024],
                 128, g1col[:, kc:kc + 1])
    for i in range(4):
        load(wupa[:, i, :], dr["w_up_swa"][i * 128:(i + 1) * 128, :], 128)
    for h in range(8):
        load(wupb[:, h, :], dr["w_up_sb"][h * 64:(h + 1) * 64, :], 64)
    for dc in range(8):
        load(wout[:, dc, :], dr["w_out"][dc * 128:(dc + 1) * 128, :], 128)

    toks = []
    for gt in range(ntiles):
        b = gt % 2
        xap = dr["x"][gt * 128:(gt + 1) * 128, :]
        P.dma("sp", lambda e, xap=xap, b=b: e.dma_start(out=xa[b][:], in_=xap), writes=[r_xa[b]])
        P.dma("sp", lambda e, gt=gt, b=b: e.dma_start(out=yaT[b][:].rearrange("p a b -> p (a b)"), in_=dr["ysa"][gt]),
              writes=[r_yaT[b]])
        P.dma("sp", lambda e, gt=gt, b=b: e.dma_start(out=ybT[b][:].rearrange("p a b -> p (a b)"), in_=dr["ysb"][gt]),
              writes=[r_ybT[b]])
        P.op("act", lambda e, b=b: e.activation(out=junk[:], in_=xa[b][:], func=AF.Square, accum_out=ss[:]),
             reads=[r_xa[b]], writes=[r_junk, r_norm])
        P.op("act", lambda e: e.activation(out=lnv[:], in_=ss[:], func=AF.Ln, scale=1.0 / D, bias=epsc[:]),
             reads=[r_norm, r_const], writes=[r_norm])
        P.op("act", lambda e: e.activation(out=rstd[:], in_=lnv[:], func=AF.Exp, scale=-0.5), reads=[r_norm], writes=[r_norm])
        P.op("dve", lambda e, b=b: e.tensor_scalar(out=xs[:], in0=xa[b][:], scalar1=rstd[:], scalar2=None, op0=ALU.mult),
             reads=[r_xa[b], r_norm], writes=[r_xs])
        for kc in range(8):
            P.op("pe", lambda e, kc=kc: e.transpose(out=TR[:, kc * 128:(kc + 1) * 128], in_=xs[:, kc * 128:(kc + 1) * 128],
                                                    identity=identb[:]), reads=[r_xs, r_const], writes=[r_TR])
        P.op("act", lambda e: e.activation(out=hT[:].rearrange("p a b -> p (a b)"), in_=TR[:], func=AF.Copy),
             reads=[r_TR], writes=[r_hT])
        for dc in range(8):
            rM, M = psr.get()
            cs = slice(dc * 128, (dc + 1) * 128)
            for i in range(4):
                P.op("pe", lambda e, M=M, i=i, cs=cs, b=b: e.matmul(M[:, 0:128], lhsT=wupa[:, i, cs], rhs=yaT[b][:, i, :],
                                                                   start=(i == 0), stop=(i == 3)),
                     reads=[r_w, r_yaT[b]], writes=[rM])
            for h in range(8):
                P.op("pe", lambda e, M=M, h=h, cs=cs, b=b: e.matmul(M[:, 128:256], lhsT=wupb[:, h, cs], rhs=ybT[b][:, h, :],
                                                                   start=(h == 0), stop=(h == 7)),
                     reads=[r_w, r_ybT[b]], writes=[rM])
            for gi in range(2):
                gs = slice(gi * 1024 + dc * 128, gi * 1024 + (dc + 1) * 128)
                for kc in range(8):
                    P.op("pe", lambda e, M=M, gi=gi, gs=gs, kc=kc: e.matmul(M[:, (2 + gi) * 128:(3 + gi) * 128], lhsT=wg[:, kc, gs],
                                                                           rhs=hT[:, kc, :], start=(kc == 0), stop=(kc == 7)),
                         reads=[r_w, r_hT], writes=[rM])
            eb = dc % 2
            for gi in range(2):
                P.op("act", lambda e, M=M, gi=gi, dc=dc, eb=eb: e.activation(
                    out=eg[eb][:, gi, :], in_=M[:, (2 + gi) * 128:(3 + gi) * 128], func=AF.Exp, scale=-1.0,
                    bias=ngb[:, gi * 8 + dc:gi * 8 + dc + 1]), reads=[rM, r_const], writes=[r_eg[eb]])
            P.op("dve", lambda e, eb=eb: e.tensor_scalar(out=eg[eb][:], in0=eg[eb][:], scalar1=1.0, scalar2=None, op0=ALU.add),
                 reads=[r_eg[eb]], writes=[r_eg[eb]])
            P.op("dve", lambda e, eb=eb: e.reciprocal(out=eg[eb][:], in_=eg[eb][:]), reads=[r_eg[eb]], writes=[r_eg[eb]])
            P.op("dve", lambda e, M=M, eb=eb: e.tensor_tensor(out=tmp[eb][:].rearrange("p a b -> p (a b)"),
                                                              in0=eg[eb][:].rearrange("p a b -> p (a b)"), in1=M[:, 0:256],
                                                              op=ALU.mult), reads=[r_eg[eb], rM], writes=[r_tmp[eb]])
            release(rM)
            P.op("dve", lambda e, eb=eb, dc=dc: e.tensor_tensor(out=mT[:, dc, :], in0=tmp[eb][:, 0, :], in1=tmp[eb][:, 1, :],
                                                                op=ALU.add), reads=[r_tmp[eb]], writes=[r_mT])
        for hf in range(2):
            rO, O = psr.get()
            for dc in range(8):
                P.op("pe", lambda e, O=O, dc=dc, hf=hf: e.matmul(O[:, 0:512], lhsT=mT[:, dc, :],
                                                                rhs=wout[:, dc, hf * 512:(hf + 1) * 512],
                                                                start=(dc == 0), stop=(dc == 7)), reads=[r_mT, r_w], writes=[rO])
            P.op("dve", lambda e, O=O, hf=hf, b=b: e.tensor_tensor(out=x1[b][:, hf * 512:(hf + 1) * 512],
                                                                   in0=xa[b][:, hf * 512:(hf + 1) * 512], in1=O[:, 0:512],
                                                                   op=ALU.add), reads=[r_xa[b], rO], writes=[r_x1[b]])
            release(rO)
        yap = dr["y"][gt * 128:(gt + 1) * 128, :]
        toks.append(P.dma("sp", lambda e, yap=yap, b=b: e.dma_start(out=yap, in_=x1[b][:]), reads=[r_x1[b]]))
    return toks


NG = 20


def phase_b(P, nc, dr, ntiles, x1_res):
    wq = sb(nc, "b_wq", [128, 8, 2048], BF16)
    skT = sb(nc, "b_skT", [128, 16, 128], F32)
    g2bc = sb(nc, "b_g2bc", [128, D], F32)
    g2col = sb(nc, "b_g2col", [128, 8], F32)
    iota16 = sb(nc, "b_iota", [128, 16], F32)
    identf = sb(nc, "b_identf", [128, 128], F32)
    identb = sb(nc, "b_identb", [128, 128], BF16)
    epsc = sb(nc, "b_eps", [128, 1], F32)
    xt = [sb(nc, f"b_xt{i}", [128, D], F32) for i in range(3)]
    xs = sb(nc, "b_xs", [128, D], BF16)
    junk = sb(nc, "b_junk", [128, D], BF16)
    h2 = [sb(nc, f"b_h2{i}", [128, D], F32) for i in range(2)]
    h2T = sb(nc, "b_h2T", [128, 8, 128], BF16)
    qT = sb(nc, "b_qT", [128, 16, 128], F32)
    sc = sb(nc, "b_sc", [128, 16, 128], F32)
    sc2 = sb(nc, "b_sc2", [128, 16, 128], F32)
    stg = [sc[:].rearrange("p a b -> p (a b)"), sc2[:].rearrange("p a b -> p (a b)")]
    cand = sb(nc, "b_cand", [128, 8, 256], F32)
    cand2 = sb(nc, "b_cand2", [128, 8, 256], F32)
    oh = sb(nc, "b_oh", [128, 8, 16, 16], F32)
    tops = sb(nc, "b_tops", [128, 16, 16], F32)
    topi = sb(nc, "b_topi", [128, 16, 16], U32)
    topif = sb(nc, "b_topif", [128, 16, 16], F32)
    bests = sb(nc, "b_bests", [128, 8, 16], F32)
    bestp = sb(nc, "b_bestp", [128, 8, 16], U32)
    hiu = sb(nc, "b_hiu", [128, 8, 16], U32)
    lou = sb(nc, "b_lou", [128, 8, 16], U32)
    hif = sb(nc, "b_hif", [128, 8, 16], F32)
    lof = sb(nc, "b_lof", [128, 8, 16], F32)
    i0s = sb(nc, "b_i0s", [128, 8, 16], F32)
    i1s = sb(nc, "b_i1s", [128, 8, 16], F32)
    expf = sb(nc, "b_expf", [128, 128], F32)
    idx = [sb(nc, f"b_idx{i}", [128, 128], I32) for i in range(3)]
    gd = [sb(nc, f"b_gd{i}", [128, 8, 16], F32) for i in range(2)]
    gsum = sb(nc, "b_gsum", [128, 8], F32)
    gate = [sb(nc, f"b_gate{i}", [128, 8, 16], F32) for i in range(2)]
    dots = sb(nc, "b_dots", [128, 128], F32)
    act = sb(nc, "b_act", [128, 128], F32)
    wgt = sb(nc, "b_wgt", [128, 128], F32)
    ss = sb(nc, "b_ss", [128, 1], F32)
    lnv = sb(nc, "b_lnv", [128, 1], F32)
    rstd = sb(nc, "b_rstd", [128, 1], F32)
    G = [sb(nc, f"b_G{i}", [128, D], BF16) for i in range(NG)]
    acc = [sb(nc, f"b_acc{i}", [128, D], F32) for i in range(2)]
    scl = [sb(nc, f"b_scl{i}", [128, D], BF16) for i in range(4)]
    r_scl = [Res(f"scl{i}") for i in range(4)]
    ACC = [ps(nc, f"b_ACC{i}", [128, 512], F32) for i in range(2)]
    r_ACC = Res("ACC")
    TR = ps(nc, "b_TR", [128, 1024], BF16)
    PS = [ps(nc, f"b_PS{i}", [128, 512], F32) for i in range(4)]

    R = lambda n: Res(n)
    r_wq, r_skT, r_const = R("wq"), R("skT"), R("const")
    r_xt = [R("xt0"), R("xt1"), R("xt2")]
    r_xs, r_junk, r_h2T, r_qT, r_sc, r_sc2 = R("xs"), R("junk"), R("h2T"), R("qT"), R("sc"), R("sc2")
    r_stg = [r_sc, r_sc2]
    r_h2 = [R("h2a"), R("h2b")]
    r_cand, r_cand2, r_oh, r_top, r_best, r_sel = R("cand"), R("cand2"), R("oh"), R("top"), R("best"), R("sel")
    r_idx = [R("idx0"), R("idx1"), R("idx2")]
    r_gate = [R("gate0"), R("gate1")]
    r_gtmp = R("gtmp")
    r_gd = [R("gd0"), R("gd1")]
    r_dots, r_wgt, r_norm = R("dots"), R("wgt"), R("norm")
    r_G = [R(f"G{i}") for i in range(NG)]
    r_acc = [R("acc0"), R("acc1")]
    r_TR = R("TR")
    r_PS = [R(f"PS{i}") for i in range(4)]
    r_y = [R(f"y{i}") for i in range(ntiles)]

    P.dma("sp", lambda e: e.dma_start(out=skT[:], in_=dr["skT"]), writes=[r_skT])
    P.dma("sp", lambda e: e.dma_start(out=g2bc[:], in_=dr["g2bc"]), writes=[r_const])
    P.dma("sp", lambda e: e.dma_start(out=g2col[:], in_=dr["g2col"]), writes=[r_const])
    P.dma("sp", lambda e: e.dma_start(out=iota16[:], in_=dr["iota16"]), writes=[r_const])
    P.dma("sp", lambda e: e.dma_start(out=identf[:], in_=dr["ident"]), writes=[r_const])
    P.op("dve", lambda e: e.tensor_copy(out=identb[:], in_=identf[:]), reads=[r_const], writes=[r_const])
    P.op("dve", lambda e: e.memset(epsc[:], 1e-6), writes=[r_const])
    for kc in range(8):
        s = kc % 2
        P.dma("sp", lambda e, kc=kc, s=s: e.dma_start(out=stg[s], in_=dr["wq"][kc * 128:(kc + 1) * 128, :]),
              writes=[r_stg[s]])
        P.op("dve", lambda e, kc=kc, s=s: e.tensor_scalar(out=wq[:, kc, :], in0=stg[s], scalar1=g2col[:, kc:kc + 1],
                                                         scalar2=None, op0=ALU.mult),
             reads=[r_stg[s], r_const], writes=[r_wq])

    def front(i):
        b = i % 2
        bx = i % 3
        x1ap = dr["x1"][i * 128:(i + 1) * 128, :]
        rd = [x1_res[i]] if x1_res is not None else []
        P.dma("sp", lambda e: e.dma_start(out=xt[bx][:], in_=x1ap), reads=rd, writes=[r_xt[bx]])
        P.op("act", lambda e: e.activation(out=junk[:], in_=xt[bx][:], func=AF.Square, accum_out=ss[:]),
             reads=[r_xt[bx]], writes=[r_junk, r_norm])
        P.op("act", lambda e: e.activation(out=lnv[:], in_=ss[:], func=AF.Ln, scale=1.0 / D, bias=epsc[:]),
             reads=[r_norm, r_const], writes=[r_norm])
        P.op("act", lambda e: e.activation(out=rstd[:], in_=lnv[:], func=AF.Exp, scale=-0.5),
             reads=[r_norm], writes=[r_norm])
        P.op("dve", lambda e: e.tensor_scalar(out=xs[:], in0=xt[bx][:], scalar1=rstd[:], scalar2=None, op0=ALU.mult),
             reads=[r_xt[bx], r_norm], writes=[r_xs])
        P.op("dve", lambda e: e.scalar_tensor_tensor(out=h2[b][:], in0=xt[bx][:], scalar=rstd[:], in1=g2bc[:],
                                                     op0=ALU.mult, op1=ALU.mult),
             reads=[r_xt[bx], r_norm, r_const], writes=[r_h2[b]])
        for kc in range(8):
            P.op("pe", lambda e, kc=kc: e.transpose(out=TR[:, kc * 128:(kc + 1) * 128], in_=xs[:, kc * 128:(kc + 1) * 128],
                                                    identity=identb[:]),
                 reads=[r_xs, r_const], writes=[r_TR])
        P.op("act", lambda e: e.activation(out=h2T[:].rearrange("p a b -> p (a b)"), in_=TR[:], func=AF.Copy),
             reads=[r_TR], writes=[r_h2T])
        for g in range(4):
            for j in range(4):
                hp = g * 4 + j
                for kc in range(8):
                    P.op("pe", lambda e, g=g, j=j, hp=hp, kc=kc: e.matmul(
                        PS[g][:, j * 128:(j + 1) * 128], lhsT=wq[:, kc, hp * 128:(hp + 1) * 128], rhs=h2T[:, kc, :],
                        start=(kc == 0), stop=(kc == 7)),
                        reads=[r_wq, r_h2T], writes=[r_PS[g]])
            P.op("act", lambda e, g=g: e.activation(out=qT[:, g * 4:(g + 1) * 4, :].rearrange("p a b -> p (a b)"),
                                                    in_=PS[g][:], func=AF.Copy),
                 reads=[r_PS[g]], writes=[r_qT])
        for g in range(4):
            for j in range(4):
                hp = g * 4 + j
                P.op("pe", lambda e, g=g, j=j, hp=hp: e.matmul(
                    PS[g][:, j * 128:(j + 1) * 128], lhsT=qT[:, hp, :], rhs=skT[:, hp, :], start=True, stop=True),
                    reads=[r_qT, r_skT], writes=[r_PS[g]])
            P.op("act", lambda e, g=g: e.activation(out=sc[:, g * 4:(g + 1) * 4, :].rearrange("p a b -> p (a b)"),
                                                    in_=PS[g][:], func=AF.Copy),
                 reads=[r_PS[g]], writes=[r_sc])
        for hp in range(16):
            P.op("dve", lambda e, hp=hp: e.max(out=tops[:, hp, 0:8], in_=sc[:, hp, :]), reads=[r_sc], writes=[r_top])
            P.op("dve", lambda e, hp=hp: e.max_index(out=topi[:, hp, 0:8], in_max=tops[:, hp, 0:8], in_values=sc[:, hp, :]),
                 reads=[r_sc, r_top], writes=[r_top])
            P.op("dve", lambda e, hp=hp: e.match_replace(out=sc2[:, hp, :], in_to_replace=tops[:, hp, 0:8],
                                                         in_values=sc[:, hp, :], imm_value=-1e30),
                 reads=[r_sc, r_top], writes=[r_sc2])
            P.op("dve", lambda e, hp=hp: e.max(out=tops[:, hp, 8:16], in_=sc2[:, hp, :]), reads=[r_sc2], writes=[r_top])
            P.op("dve", lambda e, hp=hp: e.max_index(out=topi[:, hp, 8:16], in_max=tops[:, hp, 8:16], in_values=sc2[:, hp, :]),
                 reads=[r_sc2, r_top], writes=[r_top])
        P.op("dve", lambda e: e.tensor_copy(out=topif[:], in_=topi[:]), reads=[r_top], writes=[r_top])
        tv = tops[:].rearrange("p (h t) k -> p h t k", t=2)
        in0 = tv[:, :, 0, :].unsqueeze(3).to_broadcast([128, 8, 16, 16])
        in1 = tv[:, :, 1, :].unsqueeze(2).to_broadcast([128, 8, 16, 16])
        candv = cand[:].rearrange("p h (i j) -> p h i j", j=16)
        P.op("dve", lambda e: e.tensor_tensor(out=candv, in0=in0, in1=in1, op=ALU.add), reads=[r_top], writes=[r_cand])
        for h in range(8):
            P.op("dve", lambda e, h=h: e.max(out=bests[:, h, 0:8], in_=cand[:, h, :]), reads=[r_cand], writes=[r_best])
            P.op("dve", lambda e, h=h: e.max_index(out=bestp[:, h, 0:8], in_max=bests[:, h, 0:8], in_values=cand[:, h, :]),
                 reads=[r_cand, r_best], writes=[r_best])
            P.op("dve", lambda e, h=h: e.match_replace(out=cand2[:, h, :], in_to_replace=bests[:, h, 0:8],
                                                       in_values=cand[:, h, :], imm_value=-1e30),
                 reads=[r_cand, r_best], writes=[r_cand2])
            P.op("dve", lambda e, h=h: e.max(out=bests[:, h, 8:16], in_=cand2[:, h, :]), reads=[r_cand2], writes=[r_best])
            P.op("dve", lambda e, h=h: e.max_index(out=bestp[:, h, 8:16], in_max=bests[:, h, 8:16], in_values=cand2[:, h, :]),
                 reads=[r_cand2, r_best], writes=[r_best])
        P.op("dve", lambda e: e.tensor_single_scalar(out=hiu[:], in_=bestp[:], scalar=4, op=ALU.logical_shift_right),
             reads=[r_best], writes=[r_sel])
        P.op("dve", lambda e: e.tensor_single_scalar(out=lou[:], in_=bestp[:], scalar=15, op=ALU.bitwise_and),
             reads=[r_best], writes=[r_sel])
        P.op("dve", lambda e: e.tensor_copy(out=hif[:], in_=hiu[:]), reads=[r_sel], writes=[r_sel])
        P.op("dve", lambda e: e.tensor_copy(out=lof[:], in_=lou[:]), reads=[r_sel], writes=[r_sel])
        iota_bc = iota16[:].unsqueeze(1).unsqueeze(1).to_broadcast([128, 8, 16, 16])
        tiv = topif[:].rearrange("p (h t) k -> p h t k", t=2)
        for (selin, tsel, outsel) in ((hif, 0, i0s), (lof, 1, i1s)):
            P.op("dve", lambda e, selin=selin: e.tensor_tensor(
                out=oh[:], in0=iota_bc, in1=selin[:].unsqueeze(3).to_broadcast([128, 8, 16, 16]), op=ALU.is_equal),
                reads=[r_sel, r_const], writes=[r_oh])
            P.op("dve", lambda e, tsel=tsel: e.tensor_tensor(
                out=oh[:], in0=oh[:], in1=tiv[:, :, tsel, :].unsqueeze(2).to_broadcast([128, 8, 16, 16]), op=ALU.mult),
                reads=[r_oh, r_top], writes=[r_oh])
            P.op("dve", lambda e, outsel=outsel: e.tensor_reduce(out=outsel[:], in_=oh[:], axis=AX.X, op=ALU.add),
                 reads=[r_oh], writes=[r_sel])
        P.op("dve", lambda e: e.scalar_tensor_tensor(out=expf[:].rearrange("p (h k) -> p h k", k=16), in0=i0s[:],
                                                     scalar=128.0, in1=i1s[:], op0=ALU.mult, op1=ALU.add),
             reads=[r_sel], writes=[r_sel])
        P.op("dve", lambda e: e.tensor_copy(out=idx[bx][:], in_=expf[:]), reads=[r_sel], writes=[r_idx[bx]])
        P.op("dve", lambda e: e.tensor_tensor(out=gd[b][:], in0=bests[:], in1=bests[:, :, 0:1].to_broadcast([128, 8, 16]),
                                              op=ALU.subtract), reads=[r_best], writes=[r_gd[b]])

    gring = {"i": 0}

    def back(i):
        b = i % 2
        bx = i % 3
        for j in range(128):
            g = gring["i"] % NG
            gring["i"] += 1
            P.dma("pool", lambda e, g=g, j=j: e.indirect_dma_start(
                out=G[g][:], out_offset=None, in_=dr["ub"],
                in_offset=bass.IndirectOffsetOnAxis(ap=idx[bx][:, j:j + 1], axis=0)),
                reads=[r_idx[bx]], writes=[r_G[g]])
            P.op("dve", lambda e, g=g, j=j: e.scalar_tensor_tensor(
                out=junk[:], in0=G[g][:], scalar=1.0, in1=h2[b][:], op0=ALU.mult, op1=ALU.mult,
                accum_out=dots[:, j:j + 1]),
                reads=[r_G[g], r_h2[b]], writes=[r_junk, r_dots])
        P.op("act", lambda e: e.activation(out=gd[b][:], in_=gd[b][:], func=AF.Exp), reads=[r_gd[b]], writes=[r_gd[b]])
        P.op("dve", lambda e: e.tensor_reduce(out=gsum[:], in_=gd[b][:], axis=AX.X, op=ALU.add), reads=[r_gd[b]], writes=[r_gtmp])
        P.op("dve", lambda e: e.reciprocal(out=gsum[:], in_=gsum[:]), reads=[r_gtmp], writes=[r_gtmp])
        P.op("dve", lambda e: e.tensor_tensor(out=gate[b][:], in0=gd[b][:], in1=gsum[:].unsqueeze(2).to_broadcast([128, 8, 16]),
                                              op=ALU.mult), reads=[r_gtmp, r_gd[b]], writes=[r_gate[b]])
        P.op("act", lambda e: e.activation(out=act[:], in_=dots[:], func=AF.Gelu), reads=[r_dots], writes=[r_wgt])
        P.op("dve", lambda e: e.tensor_tensor(out=wgt[:], in0=act[:], in1=gate[b][:].rearrange("p h k -> p (h k)"),
                                              op=ALU.mult), reads=[r_wgt, r_gate[b]], writes=[r_wgt])
        if i == 0 and "dbg" in dr:
            dbg = dr["dbg"]
            P.dma("sp", lambda e: e.dma_start(out=dbg[0], in_=dots[:]), reads=[r_dots])
            P.dma("sp", lambda e: e.dma_start(out=dbg[1], in_=wgt[:]), reads=[r_wgt])
            P.dma("sp", lambda e: e.dma_start(out=dbg[2], in_=expf[:]), reads=[r_sel])
            P.dma("sp", lambda e: e.dma_start(out=dbg[3], in_=gate[b][:].rearrange("p h k -> p (h k)")), reads=[r_gate[b]])
            P.dma("sp", lambda e: e.dma_start(out=dbg[4], in_=bests[:].rearrange("p h k -> p (h k)")), reads=[r_best])
            P.dma("sp", lambda e: e.dma_start(out=dbg[5], in_=topif[:, 0:8, :].rearrange("p h k -> p (h k)")), reads=[r_top])
            P.dma("sp", lambda e: e.dma_start(out=dbg[6], in_=tops[:, 0:8, :].rearrange("p h k -> p (h k)")), reads=[r_top])
            P.dma("sp", lambda e: e.dma_start(out=dbg[7], in_=sc[:, 0, :]), reads=[r_sc])

    def back_v(i):
        b = i % 2
        bx = i % 3
        for j in range(128):
            g = gring["i"] % NG
            gring["i"] += 1
            P.dma("pool", lambda e, g=g, j=j: e.indirect_dma_start(
                out=G[g][:], out_offset=None, in_=dr["vb"],
                in_offset=bass.IndirectOffsetOnAxis(ap=idx[bx][:, j:j + 1], axis=0)),
                reads=[r_idx[bx]], writes=[r_G[g]])
            si = j % 4
            P.op("act", lambda e, g=g, j=j, si=si: e.activation(out=scl[si][:], in_=G[g][:], func=AF.Copy,
                                                               scale=wgt[:, j:j + 1]),
                 reads=[r_G[g], r_wgt], writes=[r_scl[si]])
            for hf in range(2):
                P.op("pe", lambda e, j=j, si=si, hf=hf: e.matmul(ACC[hf][:, 0:512], lhsT=identb[:],
                                                                 rhs=scl[si][:, hf * 512:(hf + 1) * 512],
                                                                 start=(j == 0), stop=(j == 127)),
                     reads=[r_scl[si], r_const], writes=[r_ACC])

    def back_fin(i):
        b = i % 2
        bx = i % 3
        for hf in range(2):
            P.op("dve", lambda e, hf=hf: e.tensor_tensor(out=acc[b][:, hf * 512:(hf + 1) * 512],
                                                         in0=xt[bx][:, hf * 512:(hf + 1) * 512], in1=ACC[hf][:, 0:512],
                                                         op=ALU.add), reads=[r_xt[bx], r_ACC], writes=[r_acc[b]])
        yap = dr["y"][i * 128:(i + 1) * 128, :]
        wr = [r_y[i]] + ([x1_res[i]] if x1_res is not None else [])
        return P.dma("sp", lambda e: e.dma_start(out=yap, in_=acc[b][:]), reads=[r_acc[b]], writes=wr)

    out_toks = []
    front(0)
    if ntiles > 1:
        front(1)
    for i in range(ntiles):
        back(i)
        if i + 2 < ntiles:
            front(i + 2)
        back_v(i)
        out_toks.append(back_fin(i))
    return out_toks


NCORES = 8
SEQ_PER_CORE = 4


def build_program(nseq):
    nc = bass.Bass("TRN2", target_bir_lowering=False)
    nt = nseq * 16

    def din(name, shape, dt=F32):
        return nc.dram_tensor(name, list(shape), dt, kind="ExternalInput").ap()
    dr = {"x": din("x", [nt * 128, 1024]), "w_in": din("w_in", [1024, 4352]), "g1col": din("g1col", [128, 8]),
          "gainqk": din("gainqk", [128, 640]), "sinks_bc": din("sinks_bc", [128, 8]), "cst": din("cst", [5, 128, 128]),
          "swab": din("swab", [128, 2048]), "gbcol": din("gbcol", [128, 16]), "w_up_swa": din("w_up_swa", [512, 1024]),
          "w_up_sb": din("w_up_sb", [512, 1024]), "w_out": din("w_out", [1024, 1024]),
          "wq": din("wq", [1024, 2048]), "g2col": din("g2col", [128, 8]), "g2bc": din("g2bc", [128, 1024]),
          "skT": din("skT", [128, 16, 128]), "u": din("u", [16384, 1024]), "v": din("v", [16384, 1024]),
          "iota16": din("iota16", [128, 16])}
    dr["ident"] = dr["cst"][0]
    dr["y"] = nc.dram_tensor("y", [nt * 128, 1024], F32, kind="ExternalOutput").ap()
    dr["x1"] = dr["y"]
    dr["ysa"] = nc.dram_tensor("ysa", [nt, 128, 512], BF16).ap()
    dr["ysb"] = nc.dram_tensor("ysb", [nt, 64, 1024], BF16).ap()
    dr["ub"] = nc.dram_tensor("ub16", [16384, 1024], BF16).ap()
    dr["vb"] = nc.dram_tensor("vb16", [16384, 1024], BF16).ap()
    P = Prog(nc)
    with ExitStack() as st:
        STACK[0] = st
        phase_a1(P, nc, dr, nseq)
        P.barrier()
        P.replay()
    with ExitStack() as st:
        STACK[0] = st
        phase_a2(P, nc, dr, nt)
        P.barrier()
        P.replay()
    with ExitStack() as st:
        STACK[0] = st
        toks = phase_b(P, nc, dr, nt, None)
        P.wait_tokens("sp", toks)
        P.barrier()
        P.replay()
    return nc


def host_inputs(inputs):
    f = lambda k: np.asarray(inputs[k], np.float32)
    cst, swab = consts()
    g1 = f("mix_norm_gain")[0]
    g2 = f("ffn_norm_gain")[0]
    sk = f("peer_sub_keys")[0]
    shared = {
        "w_in": np.ascontiguousarray(f("w_in")[0]), "g1col": col(g1, 8),
        "gainqk": bc(np.concatenate([np.tile(f("swa_q_gain")[0], 8), np.tile(f("swa_k_gain")[0], 2)])),
        "sinks_bc": bc(f("swa_sinks")[0]), "cst": cst, "swab": swab, "gbcol": col(f("gate_bias")[0], 16),
        "w_up_swa": np.ascontiguousarray(f("w_up_swa")[0]), "w_up_sb": np.ascontiguousarray(f("w_up_sb")[0]),
        "w_out": np.ascontiguousarray(f("w_out")[0]), "wq": np.ascontiguousarray(f("peer_w_q")[0]),
        "g2col": col(g2, 8), "g2bc": bc(g2),
        "skT": np.ascontiguousarray(sk.reshape(16, 128, 128).transpose(2, 0, 1)),
        "u": np.ascontiguousarray(f("peer_u")[0]), "v": np.ascontiguousarray(f("peer_v")[0]),
        "iota16": bc(np.arange(16, dtype=np.float32)),
    }
    return shared


def kernel(**inputs):
    x = np.asarray(inputs["x"], np.float32)
    B, S_, D_ = x.shape
    nseq = B // NCORES
    shared = host_inputs(inputs)
    nc = build_program(nseq)
    in_maps = []
    for c in range(NCORES):
        m = dict(shared)
        m["x"] = np.ascontiguousarray(x[c * nseq:(c + 1) * nseq].reshape(nseq * S_, D_))
        in_maps.append(m)
    res = run_bass_kernel_spmd(nc, in_maps, core_ids=list(range(NCORES)))
    out = np.concatenate([np.asarray(r["y"], np.float32).reshape(nseq, S_, D_) for r in res.results], axis=0)
    return out
```

```python
from contextlib import ExitStack
import numpy as np
import concourse.bass as bass
import concourse.mybir as mybir
from concourse.bass_utils import run_bass_kernel_spmd


F32 = mybir.dt.float32
BF16 = mybir.dt.bfloat16
I32 = mybir.dt.int32
U32 = mybir.dt.uint32
AF = mybir.ActivationFunctionType
ALU = mybir.AluOpType
AX = mybir.AxisListType

SAME_ENGINE_SYNC = True
N_DMA_SEMS = {"sp": 12, "pool": 24, "act": 2}


class Res:
    __slots__ = ("name", "w", "r", "open")

    def __init__(self, name):
        self.name = name
        self.w = None
        self.r = {}
        self.open = False


class Ring:
    def __init__(self, items):
        self.items = list(items)
        self.i = 0

    def get(self):
        it = self.items[self.i % len(self.items)]
        self.i += 1
        res = it[0] if isinstance(it, tuple) else it
        assert not res.open, f"ring slot {res.name} still open"
        res.open = True
        return it


def release(res):
    res.open = False


class Prog:
    ENG = ("pe", "dve", "act", "pool", "sp")

    def __init__(self, nc):
        self.nc = nc
        self.ops = {e: [] for e in self.ENG}
        self.cnt = {e: 0 for e in self.ENG}
        self.seen = {e: {} for e in self.ENG}
        self.sems = {}
        for e in ("pe", "dve", "act", "pool"):
            self.sems[e] = nc.alloc_semaphore(f"s_{e}")
        self.dma_pool = {}
        for q, n in N_DMA_SEMS.items():
            self.dma_pool[q] = {"i": 0, "sems": []}
            for k in range(n):
                key = ("dma", q, k)
                self.sems[key] = nc.alloc_semaphore(f"s_dma_{q}_{k}")
                self.dma_pool[q]["sems"].append([key, 0])
        self.n_wait = 0

    def _deps(self, eng, reads, writes):
        deps = {}

        def add(tok):
            if tok is None:
                return
            k, v = tok
            if deps.get(k, 0) < v:
                deps[k] = v
        for r in reads:
            add(r.w)
        for w in writes:
            add(w.w)
            for k, v in w.r.items():
                add((k, v))
        out = []
        for k, v in deps.items():
            if k == eng and (eng == "pe" or not SAME_ENGINE_SYNC):
                continue
            if self.seen[eng].get(k, 0) >= v:
                continue
            self.seen[eng][k] = v
            out.append((k, v))
        return out

    def _update(self, tok, reads, writes):
        k, v = tok
        for r in reads:
            if r.r.get(k, 0) < v:
                r.r[k] = v
        for w in writes:
            w.w = tok
            w.r = {}

    def op(self, eng, fn, reads=(), writes=()):
        waits = self._deps(eng, reads, writes)
        self.cnt[eng] += 1
        tok = (eng, self.cnt[eng])
        self.ops[eng].append((waits, fn, (eng, 1)))
        self._update(tok, reads, writes)
        self.n_wait += len(waits)
        return tok

    def dma(self, q, fn, reads=(), writes=()):
        pool = self.dma_pool[q]
        slot = pool["sems"][pool["i"] % len(pool["sems"])]
        pool["i"] += 1
        key, cnt = slot
        waits = self._deps(q, reads, writes)
        if cnt > 0 and self.seen[q].get(key, 0) < 16 * cnt:
            self.seen[q][key] = 16 * cnt
            waits.append((key, 16 * cnt))
        slot[1] = cnt + 1
        tok = (key, 16 * (cnt + 1))
        self.ops[q].append((waits, fn, (key, 16)))
        self._update(tok, reads, writes)
        return tok

    def wait_tokens(self, eng, toks):
        waits = []
        for k, v in toks:
            if self.seen[eng].get(k, 0) >= v:
                continue
            self.seen[eng][k] = v
            waits.append((k, v))
        if waits:
            self.ops[eng].append((waits, None, None))

    def all_tokens(self):
        toks = []
        for e in ("pe", "dve", "act", "pool"):
            if self.cnt[e] > 0:
                toks.append((e, self.cnt[e]))
        for q, pool in self.dma_pool.items():
            for key, cnt in pool["sems"]:
                if cnt > 0:
                    toks.append((key, 16 * cnt))
        return toks

    def barrier(self):
        toks = self.all_tokens()
        for e in self.ENG:
            self.wait_tokens(e, [t for t in toks if not (e == 'pe' and t[0] == 'pe')])

    def replay(self):
        nc = self.nc
        P = self
        with nc.Block() as block:
            def run(e, name):
                for waits, fn, inc in P.ops[name]:
                    for k, v in waits:
                        e.wait_ge(P.sems[k], v)
                    if fn is not None:
                        ins = fn(e)
                        ins.then_inc(P.sems[inc[0]], inc[1])

            @block.sync
            def _(e):
                run(e, "sp")

            @block.tensor
            def _(e):
                run(e, "pe")

            @block.vector
            def _(e):
                run(e, "dve")

            @block.scalar
            def _(e):
                run(e, "act")

            @block.gpsimd
            def _(e):
                run(e, "pool")
        for e in self.ENG:
            self.ops[e] = []


STACK = [None]


def sb(nc, name, shape, dt):
    return STACK[0].enter_context(nc.sbuf_tensor(name, list(shape), dt))


def ps(nc, name, shape, dt):
    return STACK[0].enter_context(nc.psum_tensor(name, list(shape), dt))


def consts():
    j = np.arange(128)[:, None]; t = np.arange(128)[None, :]
    ident = np.eye(128, dtype=np.float32)
    tri = (j >= t).astype(np.float32)
    ones = np.ones((128, 128), np.float32)
    maskL = (j < t).astype(np.float32)
    maskb = np.where(j < t, 0.0, 30000.0).astype(np.float32)
    cst = np.stack([ident, tri, ones, maskL, maskb]).astype(np.float32)
    slopes = 2.0 ** (-8.0 * np.arange(1, 9) / 8)
    k = np.arange(128)[:, None]; q = np.arange(128)[None, :]
    swab = np.zeros((128, 8, 2, 128), np.float32)
    for kt in range(2):
        spos = (kt - 1) * 128 + k
        tpos = q
        ck = spos // 64
        cq = tpos // 64
        vis = (ck <= cq) & (ck >= cq - 2)
        dist = np.abs(tpos - spos).astype(np.float64)
        for h in range(8):
            b = np.where(vis, -slopes[h] * dist * 8.0, -240000.0)
            swab[:, h, kt, :] = b
    return cst, np.ascontiguousarray(swab.reshape(128, 2048))
def col(v, n):
    return np.ascontiguousarray(np.asarray(v, np.float32).reshape(n, 128).T)
def bc(v):
    v = np.asarray(v, np.float32).reshape(1, -1)
    return np.ascontiguousarray(np.broadcast_to(v, (128, v.shape[1])))


D = 1024
S = 2048
NT = 16
BATCH = 2
SKIP_SB = set()
SKIP_SWA = set()


def phase_a1(P, nc, dr, nseq):
    R = lambda n: Res(n)
    win = sb(nc, "a_win", [128, 8, 2304], BF16)
    stg = [sb(nc, f"a_stg{i}", [128, 1152], F32) for i in range(2)]
    g1col = sb(nc, "a_g1col", [128, 8], F32)
    gainqk = sb(nc, "a_gainqk", [128, 640], F32)
    esink = sb(nc, "a_esink", [128, 8], F32)
    epsc = sb(nc, "a_eps", [128, 1], F32)
    cstf = sb(nc, "a_cstf", [128, 5, 128], F32)
    cstb = sb(nc, "a_cstb", [128, 5, 128], BF16)
    identb, trib, onesb, maskLb, maskbb = (cstb[:, k, :] for k in range(5))
    swabf = sb(nc, "a_swabf", [128, 2048], F32)
    swab = sb(nc, "a_swab", [128, 8, 2, 128], BF16)
    kaT = sb(nc, "a_kaT", [64, 2, S], BF16)
    kbT = sb(nc, "a_kbT", [128, 4, S], BF16)
    vb = sb(nc, "a_vb", [128, NT, 512], BF16)
    vaug = sb(nc, "a_vaug", [128, NT, 2, 65], BF16)
    xa = [sb(nc, f"a_xa{i}", [128, D], F32) for i in range(2)]
    xs = sb(nc, "a_xs", [128, D], BF16)
    junk = sb(nc, "a_junk", [128, D], BF16)
    hT = sb(nc, "a_hT", [128, 8, 128], BF16)
    sq = sb(nc, "a_sq", [128, 640], F32)
    qtmp = sb(nc, "a_qtmp", [128, 640], F32)
    qn = sb(nc, "a_qn", [128, 640], BF16)
    ssq = sb(nc, "a_ssq", [128, 10], F32)
    rq = sb(nc, "a_rq", [128, 10], F32)
    ss = sb(nc, "a_ss", [128, 1], F32)
    lnv = sb(nc, "a_lnv", [128, 1], F32)
    rstd = sb(nc, "a_rstd", [128, 1], F32)
    qaT = sb(nc, "a_qaT", [64, 8, 128], BF16)
    qbT = sb(nc, "a_qbT", [128, 4, 128], BF16)
    qbTn = sb(nc, "a_qbTn", [128, 4, 128], BF16)
    PT = [sb(nc, f"a_PT{i}", [128, 2, 2, 128], BF16) for i in range(2)]
    dn = sb(nc, "a_dn", [128, 4, 1], F32)
    ya = sb(nc, "a_ya", [128, 512], BF16)
    yaT = [sb(nc, f"a_yaT{i}", [128, 4, 128], BF16) for i in range(2)]
    ybT = [sb(nc, f"a_ybT{i}", [64, 8, 128], BF16) for i in range(2)]
    Eb = [sb(nc, f"a_E{i}", [128, 512], F32) for i in range(2)]
    Lb = [sb(nc, f"a_L{i}", [128, 512], BF16) for i in range(2)]
    Wb = [sb(nc, f"a_W{i}", [128, 512], BF16) for i in range(2)]
    Sb = sb(nc, "a_S", [128, 17, 128], BF16)
    TR = ps(nc, "a_TR", [128, 1024], BF16)
    PSR = [ps(nc, f"a_PS{i}", [128, 512], F32) for i in range(4)]
    OUT = [ps(nc, f"a_OUT{i}", [128, 512], F32) for i in range(2)]
    cin = [sb(nc, f"a_cin{i}", [128, 2048], F32) for i in range(2)]
    cout = [sb(nc, f"a_cout{i}", [128, 2048], BF16) for i in range(2)]
    r_cin = [R("cin0"), R("cin1")]
    r_cout = [R("cout0"), R("cout1")]
    conv_state = {"k": 0}

    def convert_chunks(n):
        for _ in range(n):
            k = conv_state["k"]
            if k >= 128:
                return
            conv_state["k"] = k + 1
            src, c0 = (dr["u"], 0) if k < 64 else (dr["v"], 1024)
            r0 = (k % 64) * 256
            s_ap = src[r0:r0 + 256, :].rearrange("(p i) d -> p (i d)", i=2)
            d_ap = dr["uvb"][r0:r0 + 256, c0:c0 + 1024].rearrange("(p i) d -> p i d", i=2)
            bi = k % 2
            P.dma("pool", lambda e, s_ap=s_ap, bi=bi: e.dma_start(out=cin[bi][:], in_=s_ap), writes=[r_cin[bi]])
            P.op("pool", lambda e, bi=bi: e.tensor_copy(out=cout[bi][:], in_=cin[bi][:]), reads=[r_cin[bi]], writes=[r_cout[bi]])
            P.dma("pool", lambda e, d_ap=d_ap, bi=bi: e.dma_start(out=d_ap, in_=cout[bi][:].rearrange("p (i d) -> p i d", i=2)), reads=[r_cout[bi]])

    r_win, r_const = R("win"), R("const")
    r_stg = [R("stg0"), R("stg1")]
    r_kaT, r_kbT, r_vb, r_vaug = R("kaT"), R("kbT"), R("vb"), R("vaug")
    r_xa = [R("xa0"), R("xa1")]
    r_xs, r_junk, r_hT, r_sq, r_qtmp, r_qn, r_norm, r_qnorm = (R(n) for n in
                                                               ("xs", "junk", "hT", "sq", "qtmp", "qn", "norm", "qnorm"))
    r_qaT, r_qbT = R("qaT"), R("qbT")
    r_PT = [R("PT0"), R("PT1")]
    r_dn, r_ya = R("dn"), R("ya")
    r_yaT = [R("yaT0"), R("yaT1")]
    r_ybT = [R("ybT0"), R("ybT1")]
    r_S = [R(f"S{k}") for k in range(17)]
    r_TR = R("TR")
    r_OUT = [R("OUT0"), R("OUT1")]
    psr = Ring([(R(f"PS{i}"), PSR[i]) for i in range(4)])
    ering = Ring([(R(f"E{i}"), Eb[i]) for i in range(2)])
    lring = Ring([(R(f"L{i}"), Lb[i]) for i in range(2)])
    wring = Ring([(R(f"W{i}"), Wb[i]) for i in range(2)])

    P.dma("sp", lambda e: e.dma_start(out=g1col[:], in_=dr["g1col"]), writes=[r_const])
    P.dma("sp", lambda e: e.dma_start(out=gainqk[:], in_=dr["gainqk"]), writes=[r_const])
    P.dma("sp", lambda e: e.dma_start(out=esink[:], in_=dr["sinks_bc"]), writes=[r_const])
    P.dma("sp", lambda e: e.dma_start(out=cstf[:], in_=dr["cst"][0:5].rearrange("k p n -> p k n")), writes=[r_const])
    P.dma("sp", lambda e: e.dma_start(out=swabf[:], in_=dr["swab"]), writes=[r_const])
    P.op("act", lambda e: e.activation(out=esink[:], in_=esink[:], func=AF.Exp), reads=[r_const], writes=[r_const])
    P.op("dve", lambda e: e.tensor_copy(out=cstb[:], in_=cstf[:]), reads=[r_const], writes=[r_const])
    P.op("dve", lambda e: e.tensor_copy(out=swab[:].rearrange("p a b c -> p (a b c)"), in_=swabf[:]),
         reads=[r_const], writes=[r_const])
    P.op("dve", lambda e: e.memset(epsc[:], 1e-6), writes=[r_const])
    P.op("dve", lambda e: e.memset(vaug[:], 1.0), writes=[r_vaug])
    k = 0
    for kc in range(8):
        for hf in range(2):
            s = k % 2
            k += 1
            P.dma("sp", lambda e, kc=kc, hf=hf, s=s: e.dma_start(
                out=stg[s][:], in_=dr["w_in"][kc * 128:(kc + 1) * 128, hf * 1152:(hf + 1) * 1152]), writes=[r_stg[s]])
            P.op("dve", lambda e, kc=kc, hf=hf, s=s: e.tensor_scalar(
                out=win[:, kc, hf * 1152:(hf + 1) * 1152], in0=stg[s][:], scalar1=g1col[:, kc:kc + 1], scalar2=None,
                op0=ALU.mult), reads=[r_stg[s], r_const], writes=[r_win])

    def front(sq_i, tt):
        gt = sq_i * NT + tt
        b = gt % 2
        xap = dr["x"][gt * 128:(gt + 1) * 128, :]
        P.dma("sp", lambda e: e.dma_start(out=xa[b][:], in_=xap), writes=[r_xa[b]])
        P.op("act", lambda e: e.activation(out=junk[:], in_=xa[b][:], func=AF.Square, accum_out=ss[:]),
             reads=[r_xa[b]], writes=[r_junk, r_norm])
        P.op("act", lambda e: e.activation(out=lnv[:], in_=ss[:], func=AF.Ln, scale=1.0 / D, bias=epsc[:]),
             reads=[r_norm, r_const], writes=[r_norm])
        P.op("act", lambda e: e.activation(out=rstd[:], in_=lnv[:], func=AF.Exp, scale=-0.5), reads=[r_norm], writes=[r_norm])
        P.op("dve", lambda e: e.tensor_scalar(out=xs[:], in0=xa[b][:], scalar1=rstd[:], scalar2=None, op0=ALU.mult),
             reads=[r_xa[b], r_norm], writes=[r_xs])
        for kc in range(8):
            P.op("pe", lambda e, kc=kc: e.transpose(out=TR[:, kc * 128:(kc + 1) * 128], in_=xs[:, kc * 128:(kc + 1) * 128],
                                                    identity=identb), reads=[r_xs, r_const], writes=[r_TR])
        P.op("act", lambda e: e.activation(out=hT[:].rearrange("p a b -> p (a b)"), in_=TR[:], func=AF.Copy),
             reads=[r_TR], writes=[r_hT])
        rA1, A1 = psr.get()
        rA2, A2 = psr.get()
        rA3, A3 = psr.get()
        for (rr, bank, c0, n) in ((rA1, A1, 0, 512), (rA2, A2, 512, 256), (rA3, A3, 1792, 512)):
            for kc in range(8):
                P.op("pe", lambda e, bank=bank, c0=c0, n=n, kc=kc: e.matmul(
                    bank[:, 0:n], lhsT=hT[:, kc, :], rhs=win[:, kc, c0:c0 + n], start=(kc == 0), stop=(kc == 7)),
                    reads=[r_hT, r_win], writes=[rr])
        P.op("act", lambda e: e.activation(out=vb[:, tt, :], in_=A3[:, 0:512], func=AF.Copy), reads=[rA3], writes=[r_vb])
        release(rA3)
        P.op("act", lambda e: e.activation(out=vaug[:, tt, :, 0:64], in_=A2[:, 128:256].rearrange("p (g d) -> p g d", d=64),
                                           func=AF.Copy), reads=[rA2], writes=[r_vaug])
        P.op("act", lambda e: e.activation(out=sq[:, 0:512], in_=A1[:, 0:512], func=AF.Square), reads=[rA1], writes=[r_sq])
        P.op("act", lambda e: e.activation(out=sq[:, 512:640], in_=A2[:, 0:128], func=AF.Square), reads=[rA2], writes=[r_sq])
        P.op("dve", lambda e: e.tensor_reduce(out=ssq[:], in_=sq[:].rearrange("p (h d) -> p h d", d=64), axis=AX.X, op=ALU.add),
             reads=[r_sq], writes=[r_qnorm])
        P.op("act", lambda e: e.activation(out=rq[:], in_=ssq[:], func=AF.Ln, scale=1.0 / 64, bias=epsc[:]),
             reads=[r_qnorm, r_const], writes=[r_qnorm])
        P.op("act", lambda e: e.activation(out=rq[:], in_=rq[:], func=AF.Exp, scale=-0.5), reads=[r_qnorm], writes=[r_qnorm])
        P.op("dve", lambda e: e.tensor_tensor(out=qtmp[:, 0:512].rearrange("p (h d) -> p h d", d=64),
                                              in0=A1[:, 0:512].rearrange("p (h d) -> p h d", d=64),
                                              in1=rq[:, 0:8].unsqueeze(2).to_broadcast([128, 8, 64]), op=ALU.mult),
             reads=[rA1, r_qnorm], writes=[r_qtmp])
        P.op("dve", lambda e: e.tensor_tensor(out=qtmp[:, 512:640].rearrange("p (h d) -> p h d", d=64),
                                              in0=A2[:, 0:128].rearrange("p (h d) -> p h d", d=64),
                                              in1=rq[:, 8:10].unsqueeze(2).to_broadcast([128, 2, 64]), op=ALU.mult),
             reads=[rA2, r_qnorm], writes=[r_qtmp])
        release(rA1)
        release(rA2)
        P.op("dve", lambda e: e.tensor_tensor(out=qn[:], in0=qtmp[:], in1=gainqk[:], op=ALU.mult),
             reads=[r_qtmp, r_const], writes=[r_qn])
        for h in range(8):
            P.op("pe", lambda e, h=h: e.transpose(out=TR[0:64, h * 128:(h + 1) * 128], in_=qn[:, h * 64:(h + 1) * 64],
                                                  identity=identb), reads=[r_qn, r_const], writes=[r_TR])
        P.op("act", lambda e: e.activation(out=qaT[:].rearrange("p a b -> p (a b)"), in_=TR[0:64, 0:1024], func=AF.Copy),
             reads=[r_TR], writes=[r_qaT])

    def front_k(tt):
        for g in range(2):
            P.op("pe", lambda e, g=g: e.transpose(out=TR[0:64, g * 128:(g + 1) * 128], in_=qn[:, 512 + g * 64:512 + (g + 1) * 64],
                                                  identity=identb), reads=[r_qn, r_const], writes=[r_TR])
        P.op("act", lambda e: e.activation(out=kaT[:, :, tt * 128:(tt + 1) * 128],
                                           in_=TR[0:64, 0:256].rearrange("p (g t) -> p g t", t=128), func=AF.Copy),
             reads=[r_TR], writes=[r_kaT])

    def front_b(tt):
        for half, c0 in ((0, 768), (1, 1280)):
            rB, B = psr.get()
            for c in range(4):
                for kc in range(8):
                    P.op("pe", lambda e, B=B, c=c, kc=kc, c0=c0: e.matmul(
                        B[:, c * 128:(c + 1) * 128], lhsT=win[:, kc, c0 + c * 128:c0 + (c + 1) * 128], rhs=hT[:, kc, :],
                        start=(kc == 0), stop=(kc == 7)), reads=[r_win, r_hT], writes=[rB])
            if half == 0:
                P.op("act", lambda e, B=B: e.activation(out=qbT[:].rearrange("p a b -> p (a b)"), in_=B[:], func=AF.Copy,
                                                        scale=0.125), reads=[rB], writes=[r_qbT])
                P.op("act", lambda e, B=B: e.activation(out=qbTn[:].rearrange("p a b -> p (a b)"), in_=B[:], func=AF.Copy,
                                                        scale=-0.125), reads=[rB], writes=[r_qbT])
            else:
                P.op("act", lambda e, B=B: e.activation(out=kbT[:, :, tt * 128:(tt + 1) * 128],
                                                        in_=B[:].rearrange("p (c t) -> p c t", t=128), func=AF.Copy),
                     reads=[rB], writes=[r_kbT])
            release(rB)

    def swa(gt, tt):
        yb = gt % 2
        kts = [1] if tt == 0 else [0, 1]
        rOA = OAb = None
        for hb in range(4):
            g = hb // 2
            rST, ST = psr.get()
            ptb = hb % 2
            for hh in range(2):
                h = 2 * hb + hh
                for kt in kts:
                    ktile = tt - 1 + kt
                    o = (hh * 2 + kt) * 128
                    P.op("pe", lambda e, ST=ST, o=o, g=g, h=h, ktile=ktile: e.matmul(
                        ST[:, o:o + 128], lhsT=kaT[0:64, g, ktile * 128:(ktile + 1) * 128], rhs=qaT[0:64, h, :],
                        start=True, stop=False), reads=[r_kaT, r_qaT], writes=[rST])
                    P.op("pe", lambda e, ST=ST, o=o, h=h, kt=kt: e.matmul(
                        ST[:, o:o + 128], lhsT=identb, rhs=swab[:, h, kt, :], start=False, stop=True),
                        reads=[r_const], writes=[rST])
            if tt == 0:
                for hh in range(2):
                    o = (hh * 2 + 1) * 128
                    P.op("act", lambda e, ST=ST, o=o, hh=hh, ptb=ptb: e.activation(
                        out=PT[ptb][:, hh, 1, :], in_=ST[:, o:o + 128], func=AF.Exp, scale=0.125),
                        reads=[rST], writes=[r_PT[ptb]])
            else:
                P.op("act", lambda e, ST=ST, ptb=ptb: e.activation(
                    out=PT[ptb][:].rearrange("p a b c -> p (a b c)"), in_=ST[:], func=AF.Exp, scale=0.125),
                    reads=[rST], writes=[r_PT[ptb]])
            release(rST)
            if hb % 2 == 0:
                rOA, OAb = psr.get()
            for hh in range(2):
                h = 2 * hb + hh
                o = (h % 4) * 65
                for ki, kt in enumerate(kts):
                    ktile = tt - 1 + kt
                    P.op("pe", lambda e, OAb=OAb, o=o, hh=hh, kt=kt, ktile=ktile, g=g, ptb=ptb, ki=ki: e.matmul(
                        OAb[:, o:o + 65], lhsT=PT[ptb][:, hh, kt, :], rhs=vaug[:, ktile, g, :],
                        start=(ki == 0), stop=(ki == len(kts) - 1)), reads=[r_PT[ptb], r_vaug], writes=[rOA])
            if hb % 2 == 1:
                hs = (hb // 2) * 4
                oav = OAb[:, 0:260].rearrange("p (h d) -> p h d", d=65)
                P.op("dve", lambda e, oav=oav, hs=hs: e.tensor_tensor(
                    out=dn[:], in0=oav[:, :, 64:65], in1=esink[:, hs:hs + 4].unsqueeze(2), op=ALU.add),
                    reads=[rOA, r_const], writes=[r_dn])
                P.op("dve", lambda e: e.reciprocal(out=dn[:], in_=dn[:]), reads=[r_dn], writes=[r_dn])
                P.op("dve", lambda e, oav=oav, hs=hs: e.tensor_tensor(
                    out=ya[:, hs * 64:(hs + 4) * 64].rearrange("p (h d) -> p h d", d=64), in0=oav[:, :, 0:64],
                    in1=dn[:].to_broadcast([128, 4, 64]), op=ALU.mult), reads=[rOA, r_dn], writes=[r_ya])
                release(rOA)
        for c in range(4):
            P.op("pe", lambda e, c=c: e.transpose(out=TR[:, c * 128:(c + 1) * 128], in_=ya[:, c * 128:(c + 1) * 128],
                                                  identity=identb), reads=[r_ya, r_const], writes=[r_TR])
        P.op("act", lambda e: e.activation(out=yaT[yb][:].rearrange("p a b -> p (a b)"), in_=TR[:, 0:512], func=AF.Copy),
             reads=[r_TR], writes=[r_yaT[yb]])
        P.dma("sp", lambda e: e.dma_start(out=dr["ysa"][gt], in_=yaT[yb][:].rearrange("p a b -> p (a b)")),
              reads=[r_yaT[yb]])

    def sbattn(gt, tt):
        yb = gt % 2
        for h in range(8):
            c = h // 2
            pb = (h % 2) * 64
            OUTb, rOUT = OUT[h % 2], r_OUT[h % 2]
            kbs = list(range(tt, -1, -1))
            batches = [kbs[i:i + BATCH] for i in range(0, len(kbs), BATCH)]
            nmm = len(kbs)
            mmi = 0
            for batch in batches:
                nb = len(batch)
                rZ, Z = psr.get()
                for b, kb in enumerate(batch):
                    P.op("pe", lambda e, Z=Z, b=b, kb=kb, c=c, pb=pb: e.matmul(
                        Z[:, b * 128:(b + 1) * 128], lhsT=kbT[pb:pb + 64, c, kb * 128:(kb + 1) * 128],
                        rhs=qbT[pb:pb + 64, c, :], start=True, stop=True), reads=[r_kbT, r_qbT], writes=[rZ])
                rE, E = ering.get()
                P.op("act", lambda e, Z=Z, E=E, nb=nb: e.activation(out=E[:, 0:nb * 128], in_=Z[:, 0:nb * 128], func=AF.Exp),
                     reads=[rZ], writes=[rE])
                release(rZ)
                rL, L = lring.get()
                P.op("act", lambda e, E=E, L=L, nb=nb: e.activation(out=L[:, 0:nb * 128], in_=E[:, 0:nb * 128], func=AF.Ln,
                                                                     bias=1.0), reads=[rE], writes=[rL])
                release(rE)
                for b, kb in enumerate(batch):
                    if kb == tt:
                        P.op("dve", lambda e, L=L, b=b: e.tensor_tensor(out=Sb[:, tt, :], in0=L[:, b * 128:(b + 1) * 128],
                                                                        in1=maskLb, op=ALU.mult),
                             reads=[rL, r_const], writes=[r_S[tt]])
                    elif kb >= 1:
                        P.op("dve", lambda e, L=L, b=b, kb=kb: e.tensor_tensor(out=Sb[:, kb, :], in0=Sb[:, kb + 1, :],
                                                                               in1=L[:, b * 128:(b + 1) * 128], op=ALU.add),
                             reads=[rL, r_S[kb + 1]], writes=[r_S[kb]])
                rC, C = psr.get()
                for b, kb in enumerate(batch):
                    o = b * 128
                    if kb == tt:
                        P.op("pe", lambda e, C=C, o=o: e.matmul(C[:, o:o + 128], lhsT=trib, rhs=Sb[:, tt, :], start=True, stop=False),
                             reads=[r_const, r_S[tt]], writes=[rC])
                    else:
                        P.op("pe", lambda e, C=C, o=o, L=L: e.matmul(C[:, o:o + 128], lhsT=trib, rhs=L[:, o:o + 128],
                                                                     start=True, stop=False), reads=[r_const, rL], writes=[rC])
                        P.op("pe", lambda e, C=C, o=o, kb=kb: e.matmul(C[:, o:o + 128], lhsT=onesb, rhs=Sb[:, kb + 1, :],
                                                                       start=False, stop=False),
                             reads=[r_const, r_S[kb + 1]], writes=[rC])
                    P.op("pe", lambda e, C=C, o=o, kb=kb, c=c, pb=pb: e.matmul(
                        C[:, o:o + 128], lhsT=kbT[pb:pb + 64, c, kb * 128:(kb + 1) * 128], rhs=qbTn[pb:pb + 64, c, :],
                        start=False, stop=(kb != tt)), reads=[r_kbT, r_qbT], writes=[rC])
                    if kb == tt:
                        P.op("pe", lambda e, C=C, o=o: e.matmul(C[:, o:o + 128], lhsT=identb, rhs=maskbb, start=False, stop=True),
                             reads=[r_const], writes=[rC])
                release(rL)
                rW, W = wring.get()
                P.op("act", lambda e, C=C, W=W, nb=nb: e.activation(out=W[:, 0:nb * 128], in_=C[:, 0:nb * 128], func=AF.Exp,
                                                                     scale=-1.0), reads=[rC], writes=[rW])
                release(rC)
                for b, kb in enumerate(batch):
                    P.op("pe", lambda e, W=W, b=b, kb=kb, h=h, OUTb=OUTb, mmi=mmi: e.matmul(
                        OUTb[0:64, 0:128], lhsT=vb[:, kb, h * 64:(h + 1) * 64], rhs=W[:, b * 128:(b + 1) * 128],
                        start=(mmi == 0), stop=(mmi == nmm - 1)), reads=[r_vb, rW], writes=[rOUT])
                    mmi += 1
                release(rW)
            P.op("act", lambda e, OUTb=OUTb, h=h: e.activation(out=ybT[yb][:, h, :], in_=OUTb[0:64, 0:128], func=AF.Copy),
                 reads=[rOUT], writes=[r_ybT[yb]])
        P.dma("sp", lambda e: e.dma_start(out=dr["ysb"][gt], in_=ybT[yb][:].rearrange("p a b -> p (a b)")),
              reads=[r_ybT[yb]])

    for sq_i in range(nseq):
        for tt in range(NT):
            gt = sq_i * NT + tt
            convert_chunks((128 + nseq * NT - 1) // (nseq * NT))
            front(sq_i, tt)
            front_k(tt)
            front_b(tt)
            if tt not in SKIP_SWA:
                swa(gt, tt)
            if tt not in SKIP_SB:
                sbattn(gt, tt)
            if tt in SKIP_SWA:
                pass


def phase_a2(P, nc, dr, ntiles):
    R = lambda n: Res(n)
    wg = sb(nc, "m_wg", [128, 8, 2048], BF16)
    wupa = sb(nc, "m_wupa", [128, 4, 1024], BF16)
    wupb = sb(nc, "m_wupb", [64, 8, 1024], BF16)
    wout = sb(nc, "m_wout", [128, 8, 1024], BF16)
    stg = [sb(nc, f"m_stg{i}", [128, 1024], F32) for i in range(2)]
    g1col = sb(nc, "m_g1col", [128, 8], F32)
    ngb = sb(nc, "m_ngb", [128, 16], F32)
    identf = sb(nc, "m_identf", [128, 128], F32)
    identb = sb(nc, "m_identb", [128, 128], BF16)
    epsc = sb(nc, "m_eps", [128, 1], F32)
    xa = [sb(nc, f"m_xa{i}", [128, D], F32) for i in range(2)]
    xs = sb(nc, "m_xs", [128, D], BF16)
    junk = sb(nc, "m_junk", [128, D], BF16)
    hT = sb(nc, "m_hT", [128, 8, 128], BF16)
    ss = sb(nc, "m_ss", [128, 1], F32)
    lnv = sb(nc, "m_lnv", [128, 1], F32)
    rstd = sb(nc, "m_rstd", [128, 1], F32)
    yaT = [sb(nc, f"m_yaT{i}", [128, 4, 128], BF16) for i in range(2)]
    ybT = [sb(nc, f"m_ybT{i}", [64, 8, 128], BF16) for i in range(2)]
    eg = [sb(nc, f"m_eg{i}", [128, 2, 128], F32) for i in range(2)]
    tmp = [sb(nc, f"m_tmp{i}", [128, 2, 128], F32) for i in range(2)]
    mT = sb(nc, "m_mT", [128, 8, 128], BF16)
    x1 = [sb(nc, f"m_x1{i}", [128, D], F32) for i in range(2)]
    TR = ps(nc, "m_TR", [128, 1024], BF16)
    PSR = [ps(nc, f"m_PS{i}", [128, 512], F32) for i in range(6)]

    r_w, r_const = R("w"), R("const")
    r_stg = [R("stg0"), R("stg1")]
    r_xa = [R("xa0"), R("xa1")]
    r_xs, r_junk, r_hT, r_norm, r_mT, r_TR = (R(n) for n in ("xs", "junk", "hT", "norm", "mT", "TR"))
    r_yaT = [R("yaT0"), R("yaT1")]
    r_ybT = [R("ybT0"), R("ybT1")]
    r_eg = [R("eg0"), R("eg1")]
    r_tmp = [R("tmp0"), R("tmp1")]
    r_x1 = [R("x10"), R("x11")]
    psr = Ring([(R(f"PS{i}"), PSR[i]) for i in range(6)])

    P.dma("sp", lambda e: e.dma_start(out=g1col[:], in_=dr["g1col"]), writes=[r_const])
    P.dma("sp", lambda e: e.dma_start(out=ngb[:], in_=dr["gbcol"]), writes=[r_const])
    P.dma("sp", lambda e: e.dma_start(out=identf[:], in_=dr["ident"]), writes=[r_const])
    P.op("dve", lambda e: e.tensor_copy(out=identb[:], in_=identf[:]), reads=[r_const], writes=[r_const])
    P.op("dve", lambda e: e.tensor_scalar(out=ngb[:], in0=ngb[:], scalar1=-1.0, scalar2=None, op0=ALU.mult),
         reads=[r_const], writes=[r_const])
    P.op("dve", lambda e: e.memset(epsc[:], 1e-6), writes=[r_const])
    k = 0

    def load(dst_ap, src_ap, npart, scalar=None):
        nonlocal k
        s = k % 2
        k += 1
        P.dma("sp", lambda e: e.dma_start(out=stg[s][0:npart, :], in_=src_ap), writes=[r_stg[s]])
        if scalar is None:
            P.op("dve", lambda e: e.tensor_copy(out=dst_ap, in_=stg[s][0:npart, :]), reads=[r_stg[s]], writes=[r_w])
        else:
            P.op("dve", lambda e: e.tensor_scalar(out=dst_ap, in0=stg[s][0:npart, :], scalar1=scalar, scalar2=None,
                                                  op0=ALU.mult), reads=[r_stg[s], r_const], writes=[r_w])
    for kc in range(8):
        for hf in range(2):
            load(wg[:, kc, hf * 1024:(hf + 1) * 1024], dr["w_in"][kc * 128:(kc + 1) * 128, 2304 + hf * 1024:2304 + (hf + 1) * 1024],
                 128, g1col[:, kc:kc + 1])
    for i in range(4):
        load(wupa[:, i, :], dr["w_up_swa"][i * 128:(i + 1) * 128, :], 128)
    for h in range(8):
        load(wupb[:, h, :], dr["w_up_sb"][h * 64:(h + 1) * 64, :], 64)
    for dc in range(8):
        load(wout[:, dc, :], dr["w_out"][dc * 128:(dc + 1) * 128, :], 128)

    toks = []
    for gt in range(ntiles):
        b = gt % 2
        xap = dr["x"][gt * 128:(gt + 1) * 128, :]
        P.dma("sp", lambda e, xap=xap, b=b: e.dma_start(out=xa[b][:], in_=xap), writes=[r_xa[b]])
        P.dma("sp", lambda e, gt=gt, b=b: e.dma_start(out=yaT[b][:].rearrange("p a b -> p (a b)"), in_=dr["ysa"][gt]),
              writes=[r_yaT[b]])
        P.dma("sp", lambda e, gt=gt, b=b: e.dma_start(out=ybT[b][:].rearrange("p a b -> p (a b)"), in_=dr["ysb"][gt]),
              writes=[r_ybT[b]])
        P.op("act", lambda e, b=b: e.activation(out=junk[:], in_=xa[b][:], func=AF.Square, accum_out=ss[:]),
             reads=[r_xa[b]], writes=[r_junk, r_norm])
        P.op("act", lambda e: e.activation(out=lnv[:], in_=ss[:], func=AF.Ln, scale=1.0 / D, bias=epsc[:]),
             reads=[r_norm, r_const], writes=[r_norm])
        P.op("act", lambda e: e.activation(out=rstd[:], in_=lnv[:], func=AF.Exp, scale=-0.5), reads=[r_norm], writes=[r_norm])
        P.op("dve", lambda e, b=b: e.tensor_scalar(out=xs[:], in0=xa[b][:], scalar1=rstd[:], scalar2=None, op0=ALU.mult),
             reads=[r_xa[b], r_norm], writes=[r_xs])
        for kc in range(8):
            P.op("pe", lambda e, kc=kc: e.transpose(out=TR[:, kc * 128:(kc + 1) * 128], in_=xs[:, kc * 128:(kc + 1) * 128],
                                                    identity=identb[:]), reads=[r_xs, r_const], writes=[r_TR])
        P.op("act", lambda e: e.activation(out=hT[:].rearrange("p a b -> p (a b)"), in_=TR[:], func=AF.Copy),
             reads=[r_TR], writes=[r_hT])
        for dc in range(8):
            rM, M = psr.get()
            cs = slice(dc * 128, (dc + 1) * 128)
            for i in range(4):
                P.op("pe", lambda e, M=M, i=i, cs=cs, b=b: e.matmul(M[:, 0:128], lhsT=wupa[:, i, cs], rhs=yaT[b][:, i, :],
                                                                   start=(i == 0), stop=(i == 3)),
                     reads=[r_w, r_yaT[b]], writes=[rM])
            for h in range(8):
                P.op("pe", lambda e, M=M, h=h, cs=cs, b=b: e.matmul(M[:, 128:256], lhsT=wupb[:, h, cs], rhs=ybT[b][:, h, :],
                                                                   start=(h == 0), stop=(h == 7)),
                     reads=[r_w, r_ybT[b]], writes=[rM])
            for gi in range(2):
                gs = slice(gi * 1024 + dc * 128, gi * 1024 + (dc + 1) * 128)
                for kc in range(8):
                    P.op("pe", lambda e, M=M, gi=gi, gs=gs, kc=kc: e.matmul(M[:, (2 + gi) * 128:(3 + gi) * 128], lhsT=wg[:, kc, gs],
                                                                           rhs=hT[:, kc, :], start=(kc == 0), stop=(kc == 7)),
                         reads=[r_w, r_hT], writes=[rM])
            eb = dc % 2
            for gi in range(2):
                P.op("act", lambda e, M=M, gi=gi, dc=dc, eb=eb: e.activation(
                    out=eg[eb][:, gi, :], in_=M[:, (2 + gi) * 128:(3 + gi) * 128], func=AF.Exp, scale=-1.0,
                    bias=ngb[:, gi * 8 + dc:gi * 8 + dc + 1]), reads=[rM, r_const], writes=[r_eg[eb]])
            P.op("dve", lambda e, eb=eb: e.tensor_scalar(out=eg[eb][:], in0=eg[eb][:], scalar1=1.0, scalar2=None, op0=ALU.add),
                 reads=[r_eg[eb]], writes=[r_eg[eb]])
            P.op("dve", lambda e, eb=eb: e.reciprocal(out=eg[eb][:], in_=eg[eb][:]), reads=[r_eg[eb]], writes=[r_eg[eb]])
            P.op("dve", lambda e, M=M, eb=eb: e.tensor_tensor(out=tmp[eb][:].rearrange("p a b -> p (a b)"),
                                                              in0=eg[eb][:].rearrange("p a b -> p (a b)"), in1=M[:, 0:256],
                                                              op=ALU.mult), reads=[r_eg[eb], rM], writes=[r_tmp[eb]])
            release(rM)
            P.op("dve", lambda e, eb=eb, dc=dc: e.tensor_tensor(out=mT[:, dc, :], in0=tmp[eb][:, 0, :], in1=tmp[eb][:, 1, :],
                                                                op=ALU.add), reads=[r_tmp[eb]], writes=[r_mT])
        for hf in range(2):
            rO, O = psr.get()
            for dc in range(8):
                P.op("pe", lambda e, O=O, dc=dc, hf=hf: e.matmul(O[:, 0:512], lhsT=mT[:, dc, :],
                                                                rhs=wout[:, dc, hf * 512:(hf + 1) * 512],
                                                                start=(dc == 0), stop=(dc == 7)), reads=[r_mT, r_w], writes=[rO])
            P.op("dve", lambda e, O=O, hf=hf, b=b: e.tensor_tensor(out=x1[b][:, hf * 512:(hf + 1) * 512],
                                                                   in0=xa[b][:, hf * 512:(hf + 1) * 512], in1=O[:, 0:512],
                                                                   op=ALU.add), reads=[r_xa[b], rO], writes=[r_x1[b]])
            release(rO)
        yap = dr["y"][gt * 128:(gt + 1) * 128, :]
        toks.append(P.dma("sp", lambda e, yap=yap, b=b: e.dma_start(out=yap, in_=x1[b][:]), reads=[r_x1[b]]))
    return toks


NG = 16


def phase_b(P, nc, dr, ntiles, x1_res):
    wq = sb(nc, "b_wq", [128, 8, 2048], BF16)
    skT = sb(nc, "b_skT", [128, 16, 128], F32)
    g2bc = sb(nc, "b_g2bc", [128, D], F32)
    g2col = sb(nc, "b_g2col", [128, 8], F32)
    iota16 = sb(nc, "b_iota", [128, 16], F32)
    identf = sb(nc, "b_identf", [128, 128], F32)
    identb = sb(nc, "b_identb", [128, 128], BF16)
    epsc = sb(nc, "b_eps", [128, 1], F32)
    xt = [sb(nc, f"b_xt{i}", [128, D], F32) for i in range(3)]
    xs = sb(nc, "b_xs", [128, D], BF16)
    junk = sb(nc, "b_junk", [128, D], BF16)
    h2 = [sb(nc, f"b_h2{i}", [128, D], F32) for i in range(3)]
    h2T = sb(nc, "b_h2T", [128, 8, 128], BF16)
    qT = sb(nc, "b_qT", [128, 16, 128], F32)
    sc = sb(nc, "b_sc", [128, 16, 128], F32)
    sc2 = sb(nc, "b_sc2", [128, 16, 128], F32)
    stg = [sc[:].rearrange("p a b -> p (a b)"), sc2[:].rearrange("p a b -> p (a b)")]
    cand = sc[:].rearrange("p a b -> p (a b)").rearrange("p (h c) -> p h c", c=256)
    cand2 = sc2[:].rearrange("p a b -> p (a b)").rearrange("p (h c) -> p h c", c=256)
    oh = sb(nc, "b_oh", [128, 8, 16, 16], F32)
    tops = sb(nc, "b_tops", [128, 16, 16], F32)
    topi = sb(nc, "b_topi", [128, 16, 16], U32)
    topif = sb(nc, "b_topif", [128, 16, 16], F32)
    bests = sb(nc, "b_bests", [128, 8, 16], F32)
    bestp = sb(nc, "b_bestp", [128, 8, 16], U32)
    hiu = sb(nc, "b_hiu", [128, 8, 16], U32)
    lou = sb(nc, "b_lou", [128, 8, 16], U32)
    hif = sb(nc, "b_hif", [128, 8, 16], F32)
    lof = sb(nc, "b_lof", [128, 8, 16], F32)
    i0s = sb(nc, "b_i0s", [128, 8, 16], F32)
    i1s = sb(nc, "b_i1s", [128, 8, 16], F32)
    expf = sb(nc, "b_expf", [128, 128], F32)
    idx = [sb(nc, f"b_idx{i}", [128, 128], I32) for i in range(3)]
    gd = [sb(nc, f"b_gd{i}", [128, 8, 16], F32) for i in range(2)]
    gsum = sb(nc, "b_gsum", [128, 8], F32)
    gate = [sb(nc, f"b_gate{i}", [128, 8, 16], F32) for i in range(2)]
    dots = sb(nc, "b_dots", [128, 128], F32)
    act = sb(nc, "b_act", [128, 128], F32)
    wgt = sb(nc, "b_wgt", [128, 128], F32)
    ss = sb(nc, "b_ss", [128, 1], F32)
    lnv = sb(nc, "b_lnv", [128, 1], F32)
    rstd = sb(nc, "b_rstd", [128, 1], F32)
    G = [sb(nc, f"b_G{i}", [128, 2 * D], BF16) for i in range(NG)]
    acc = [sb(nc, f"b_acc{i}", [128, D], F32) for i in range(2)]
    scl = [sb(nc, f"b_scl{i}", [128, D], BF16) for i in range(4)]
    r_scl = [Res(f"scl{i}") for i in range(4)]
    ACC = [ps(nc, f"b_ACC{i}", [128, 512], F32) for i in range(2)]
    r_ACC = Res("ACC")
    TR = ps(nc, "b_TR", [128, 1024], BF16)
    PS = [ps(nc, f"b_PS{i}", [128, 512], F32) for i in range(4)]

    R = lambda n: Res(n)
    r_wq, r_skT, r_const = R("wq"), R("skT"), R("const")
    r_xt = [R("xt0"), R("xt1"), R("xt2")]
    r_xs, r_junk, r_h2T, r_qT, r_sc, r_sc2 = R("xs"), R("junk"), R("h2T"), R("qT"), R("sc"), R("sc2")
    r_stg = [r_sc, r_sc2]
    r_cand, r_cand2 = r_sc, r_sc2
    r_h2 = [R("h2a"), R("h2b"), R("h2c")]
    r_oh, r_top, r_best, r_sel = R("oh"), R("top"), R("best"), R("sel")
    r_idx = [R("idx0"), R("idx1"), R("idx2")]
    r_gate = [R("gate0"), R("gate1")]
    r_gtmp = R("gtmp")
    r_gd = [R("gd0"), R("gd1")]
    r_dots, r_wgt, r_norm = R("dots"), R("wgt"), R("norm")
    r_G = [R(f"G{i}") for i in range(NG)]
    r_acc = [R("acc0"), R("acc1")]
    r_TR = R("TR")
    r_PS = [R(f"PS{i}") for i in range(4)]
    r_y = [R(f"y{i}") for i in range(ntiles)]

    P.dma("sp", lambda e: e.dma_start(out=skT[:], in_=dr["skT"]), writes=[r_skT])
    P.dma("sp", lambda e: e.dma_start(out=g2bc[:], in_=dr["g2bc"]), writes=[r_const])
    P.dma("sp", lambda e: e.dma_start(out=g2col[:], in_=dr["g2col"]), writes=[r_const])
    P.dma("sp", lambda e: e.dma_start(out=iota16[:], in_=dr["iota16"]), writes=[r_const])
    P.dma("sp", lambda e: e.dma_start(out=identf[:], in_=dr["ident"]), writes=[r_const])
    P.op("dve", lambda e: e.tensor_copy(out=identb[:], in_=identf[:]), reads=[r_const], writes=[r_const])
    P.op("dve", lambda e: e.memset(epsc[:], 1e-6), writes=[r_const])
    for kc in range(8):
        s = kc % 2
        P.dma("sp", lambda e, kc=kc, s=s: e.dma_start(out=stg[s], in_=dr["wq"][kc * 128:(kc + 1) * 128, :]),
              writes=[r_stg[s]])
        P.op("dve", lambda e, kc=kc, s=s: e.tensor_scalar(out=wq[:, kc, :], in0=stg[s], scalar1=g2col[:, kc:kc + 1],
                                                         scalar2=None, op0=ALU.mult),
             reads=[r_stg[s], r_const], writes=[r_wq])

    def front(i):
        b = i % 2
        bx = i % 3
        x1ap = dr["x1"][i * 128:(i + 1) * 128, :]
        rd = [x1_res[i]] if x1_res is not None else []
        P.dma("sp", lambda e: e.dma_start(out=xt[bx][:], in_=x1ap), reads=rd, writes=[r_xt[bx]])
        P.op("act", lambda e: e.activation(out=junk[:], in_=xt[bx][:], func=AF.Square, accum_out=ss[:]),
             reads=[r_xt[bx]], writes=[r_junk, r_norm])
        P.op("act", lambda e: e.activation(out=lnv[:], in_=ss[:], func=AF.Ln, scale=1.0 / D, bias=epsc[:]),
             reads=[r_norm, r_const], writes=[r_norm])
        P.op("act", lambda e: e.activation(out=rstd[:], in_=lnv[:], func=AF.Exp, scale=-0.5),
             reads=[r_norm], writes=[r_norm])
        P.op("dve", lambda e: e.tensor_scalar(out=xs[:], in0=xt[bx][:], scalar1=rstd[:], scalar2=None, op0=ALU.mult),
             reads=[r_xt[bx], r_norm], writes=[r_xs])
        P.op("dve", lambda e: e.scalar_tensor_tensor(out=h2[bx][:], in0=xt[bx][:], scalar=rstd[:], in1=g2bc[:],
                                                     op0=ALU.mult, op1=ALU.mult),
             reads=[r_xt[bx], r_norm, r_const], writes=[r_h2[bx]])
        for kc in range(8):
            P.op("pe", lambda e, kc=kc: e.transpose(out=TR[:, kc * 128:(kc + 1) * 128], in_=xs[:, kc * 128:(kc + 1) * 128],
                                                    identity=identb[:]),
                 reads=[r_xs, r_const], writes=[r_TR])
        P.op("act", lambda e: e.activation(out=h2T[:].rearrange("p a b -> p (a b)"), in_=TR[:], func=AF.Copy),
             reads=[r_TR], writes=[r_h2T])
        for g in range(4):
            for j in range(4):
                hp = g * 4 + j
                for kc in range(8):
                    P.op("pe", lambda e, g=g, j=j, hp=hp, kc=kc: e.matmul(
                        PS[g][:, j * 128:(j + 1) * 128], lhsT=wq[:, kc, hp * 128:(hp + 1) * 128], rhs=h2T[:, kc, :],
                        start=(kc == 0), stop=(kc == 7)),
                        reads=[r_wq, r_h2T], writes=[r_PS[g]])
            P.op("act", lambda e, g=g: e.activation(out=qT[:, g * 4:(g + 1) * 4, :].rearrange("p a b -> p (a b)"),
                                                    in_=PS[g][:], func=AF.Copy),
                 reads=[r_PS[g]], writes=[r_qT])
        for g in range(4):
            for j in range(4):
                hp = g * 4 + j
                P.op("pe", lambda e, g=g, j=j, hp=hp: e.matmul(
                    PS[g][:, j * 128:(j + 1) * 128], lhsT=qT[:, hp, :], rhs=skT[:, hp, :], start=True, stop=True),
                    reads=[r_qT, r_skT], writes=[r_PS[g]])
            P.op("act", lambda e, g=g: e.activation(out=sc[:, g * 4:(g + 1) * 4, :].rearrange("p a b -> p (a b)"),
                                                    in_=PS[g][:], func=AF.Copy),
                 reads=[r_PS[g]], writes=[r_sc])
        for hp in range(16):
            P.op("dve", lambda e, hp=hp: e.max(out=tops[:, hp, 0:8], in_=sc[:, hp, :]), reads=[r_sc], writes=[r_top])
            P.op("dve", lambda e, hp=hp: e.max_index(out=topi[:, hp, 0:8], in_max=tops[:, hp, 0:8], in_values=sc[:, hp, :]),
                 reads=[r_sc, r_top], writes=[r_top])
            P.op("dve", lambda e, hp=hp: e.match_replace(out=sc2[:, hp, :], in_to_replace=tops[:, hp, 0:8],
                                                         in_values=sc[:, hp, :], imm_value=-1e30),
                 reads=[r_sc, r_top], writes=[r_sc2])
            P.op("dve", lambda e, hp=hp: e.max(out=tops[:, hp, 8:16], in_=sc2[:, hp, :]), reads=[r_sc2], writes=[r_top])
            P.op("dve", lambda e, hp=hp: e.max_index(out=topi[:, hp, 8:16], in_max=tops[:, hp, 8:16], in_values=sc2[:, hp, :]),
                 reads=[r_sc2, r_top], writes=[r_top])
        P.op("dve", lambda e: e.tensor_copy(out=topif[:], in_=topi[:]), reads=[r_top], writes=[r_top])
        tv = tops[:].rearrange("p (h t) k -> p h t k", t=2)
        in0 = tv[:, :, 0, :].unsqueeze(3).to_broadcast([128, 8, 16, 16])
        in1 = tv[:, :, 1, :].unsqueeze(2).to_broadcast([128, 8, 16, 16])
        candv = cand.rearrange("p h (i j) -> p h i j", j=16)
        P.op("dve", lambda e: e.tensor_tensor(out=candv, in0=in0, in1=in1, op=ALU.add), reads=[r_top], writes=[r_cand])
        for h in range(8):
            P.op("dve", lambda e, h=h: e.max(out=bests[:, h, 0:8], in_=cand[:, h, :]), reads=[r_cand], writes=[r_best])
            P.op("dve", lambda e, h=h: e.max_index(out=bestp[:, h, 0:8], in_max=bests[:, h, 0:8], in_values=cand[:, h, :]),
                 reads=[r_cand, r_best], writes=[r_best])
            P.op("dve", lambda e, h=h: e.match_replace(out=cand2[:, h, :], in_to_replace=bests[:, h, 0:8],
                                                       in_values=cand[:, h, :], imm_value=-1e30),
                 reads=[r_cand, r_best], writes=[r_cand2])
            P.op("dve", lambda e, h=h: e.max(out=bests[:, h, 8:16], in_=cand2[:, h, :]), reads=[r_cand2], writes=[r_best])
            P.op("dve", lambda e, h=h: e.max_index(out=bestp[:, h, 8:16], in_max=bests[:, h, 8:16], in_values=cand2[:, h, :]),
                 reads=[r_cand2, r_best], writes=[r_best])
        P.op("dve", lambda e: e.tensor_single_scalar(out=hiu[:], in_=bestp[:], scalar=4, op=ALU.logical_shift_right),
             reads=[r_best], writes=[r_sel])
        P.op("dve", lambda e: e.tensor_single_scalar(out=lou[:], in_=bestp[:], scalar=15, op=ALU.bitwise_and),
             reads=[r_best], writes=[r_sel])
        P.op("dve", lambda e: e.tensor_copy(out=hif[:], in_=hiu[:]), reads=[r_sel], writes=[r_sel])
        P.op("dve", lambda e: e.tensor_copy(out=lof[:], in_=lou[:]), reads=[r_sel], writes=[r_sel])
        iota_bc = iota16[:].unsqueeze(1).unsqueeze(1).to_broadcast([128, 8, 16, 16])
        tiv = topif[:].rearrange("p (h t) k -> p h t k", t=2)
        for (selin, tsel, outsel) in ((hif, 0, i0s), (lof, 1, i1s)):
            P.op("dve", lambda e, selin=selin: e.tensor_tensor(
                out=oh[:], in0=iota_bc, in1=selin[:].unsqueeze(3).to_broadcast([128, 8, 16, 16]), op=ALU.is_equal),
                reads=[r_sel, r_const], writes=[r_oh])
            P.op("dve", lambda e, tsel=tsel: e.tensor_tensor(
                out=oh[:], in0=oh[:], in1=tiv[:, :, tsel, :].unsqueeze(2).to_broadcast([128, 8, 16, 16]), op=ALU.mult),
                reads=[r_oh, r_top], writes=[r_oh])
            P.op("dve", lambda e, outsel=outsel: e.tensor_reduce(out=outsel[:], in_=oh[:], axis=AX.X, op=ALU.add),
                 reads=[r_oh], writes=[r_sel])
        P.op("dve", lambda e: e.scalar_tensor_tensor(out=expf[:].rearrange("p (h k) -> p h k", k=16), in0=i0s[:],
                                                     scalar=128.0, in1=i1s[:], op0=ALU.mult, op1=ALU.add),
             reads=[r_sel], writes=[r_sel])
        P.op("dve", lambda e: e.tensor_copy(out=idx[bx][:], in_=expf[:]), reads=[r_sel], writes=[r_idx[bx]])
        P.op("dve", lambda e: e.tensor_tensor(out=gd[b][:], in0=bests[:], in1=bests[:, :, 0:1].to_broadcast([128, 8, 16]),
                                              op=ALU.subtract), reads=[r_best], writes=[r_gd[b]])

    gring = {"i": 0}

    GRP = 4
    NGRP = 128 // GRP
    r_dg = [Res(f"dg{k}") for k in range(NGRP)]
    r_wg = [Res(f"wg{k}") for k in range(NGRP)]

    def back(i):
        b = i % 2
        bx = i % 3
        P.op("act", lambda e: e.activation(out=gd[b][:], in_=gd[b][:], func=AF.Exp), reads=[r_gd[b]], writes=[r_gd[b]])
        P.op("dve", lambda e: e.tensor_reduce(out=gsum[:], in_=gd[b][:], axis=AX.X, op=ALU.add), reads=[r_gd[b]], writes=[r_gtmp])
        P.op("dve", lambda e: e.reciprocal(out=gsum[:], in_=gsum[:]), reads=[r_gtmp], writes=[r_gtmp])
        P.op("dve", lambda e: e.tensor_tensor(out=gate[b][:], in0=gd[b][:], in1=gsum[:].unsqueeze(2).to_broadcast([128, 8, 16]),
                                              op=ALU.mult), reads=[r_gtmp, r_gd[b]], writes=[r_gate[b]])
        gatef = gate[b][:].rearrange("p h k -> p (h k)")
        slots = {}

        def vpart(grp):
            cs = slice(grp * GRP, (grp + 1) * GRP)
            P.op("dve", lambda e, cs=cs: e.tensor_tensor(out=wgt[:, cs], in0=act[:, cs], in1=gatef[:, cs], op=ALU.mult),
                 reads=[r_dg[grp], r_gate[b]], writes=[r_wg[grp]])
            for j in range(grp * GRP, (grp + 1) * GRP):
                g = slots[j]
                si = j % 4
                P.op("act", lambda e, g=g, j=j, si=si: e.activation(out=scl[si][:], in_=G[g][:, D:2 * D], func=AF.Copy,
                                                                   scale=wgt[:, j:j + 1]),
                     reads=[r_G[g], r_wg[grp]], writes=[r_scl[si]])
                for hf in range(2):
                    P.op("pe", lambda e, j=j, si=si, hf=hf: e.matmul(ACC[hf][:, 0:512], lhsT=identb[:],
                                                                     rhs=scl[si][:, hf * 512:(hf + 1) * 512],
                                                                     start=(j == 0), stop=(j == 127)),
                         reads=[r_scl[si], r_const], writes=[r_ACC])

        for grp in range(NGRP):
            cs = slice(grp * GRP, (grp + 1) * GRP)
            for j in range(grp * GRP, (grp + 1) * GRP):
                g = gring["i"] % NG
                gring["i"] += 1
                slots[j] = g
                P.dma("pool", lambda e, g=g, j=j: e.indirect_dma_start(
                    out=G[g][:], out_offset=None, in_=dr["uvb"],
                    in_offset=bass.IndirectOffsetOnAxis(ap=idx[bx][:, j:j + 1], axis=0)),
                    reads=[r_idx[bx]], writes=[r_G[g]])
                P.op("dve", lambda e, g=g, j=j: e.scalar_tensor_tensor(
                    out=junk[:], in0=G[g][:, 0:D], scalar=1.0, in1=h2[bx][:], op0=ALU.mult, op1=ALU.mult,
                    accum_out=dots[:, j:j + 1]),
                    reads=[r_G[g], r_h2[bx]], writes=[r_junk, r_dg[grp]])
            P.op("act", lambda e, cs=cs: e.activation(out=act[:, cs], in_=dots[:, cs], func=AF.Gelu),
                 reads=[r_dg[grp]], writes=[r_dg[grp]])
            if grp >= 1:
                vpart(grp - 1)
            if grp == NGRP // 2 - 1 and i + 2 < ntiles:
                front(i + 2)
        vpart(NGRP - 1)

    def back_fin(i):
        b = i % 2
        bx = i % 3
        for hf in range(2):
            P.op("dve", lambda e, hf=hf: e.tensor_tensor(out=acc[b][:, hf * 512:(hf + 1) * 512],
                                                         in0=xt[bx][:, hf * 512:(hf + 1) * 512], in1=ACC[hf][:, 0:512],
                                                         op=ALU.add), reads=[r_xt[bx], r_ACC], writes=[r_acc[b]])
        yap = dr["y"][i * 128:(i + 1) * 128, :]
        wr = [r_y[i]] + ([x1_res[i]] if x1_res is not None else [])
        return P.dma("sp", lambda e: e.dma_start(out=yap, in_=acc[b][:]), reads=[r_acc[b]], writes=wr)

    out_toks = []
    front(0)
    if ntiles > 1:
        front(1)
    for i in range(ntiles):
        back(i)
        out_toks.append(back_fin(i))
    return out_toks


NCORES = 8
SEQ_PER_CORE = 4


def build_program(nseq):
    nc = bass.Bass("TRN2", target_bir_lowering=False)
    nt = nseq * 16

    def din(name, shape, dt=F32):
        return nc.dram_tensor(name, list(shape), dt, kind="ExternalInput").ap()
    dr = {"x": din("x", [nt * 128, 1024]), "w_in": din("w_in", [1024, 4352]), "g1col": din("g1col", [128, 8]),
          "gainqk": din("gainqk", [128, 640]), "sinks_bc": din("sinks_bc", [128, 8]), "cst": din("cst", [5, 128, 128]),
          "swab": din("swab", [128, 2048]), "gbcol": din("gbcol", [128, 16]), "w_up_swa": din("w_up_swa", [512, 1024]),
          "w_up_sb": din("w_up_sb", [512, 1024]), "w_out": din("w_out", [1024, 1024]),
          "wq": din("wq", [1024, 2048]), "g2col": din("g2col", [128, 8]), "g2bc": din("g2bc", [128, 1024]),
          "skT": din("skT", [128, 16, 128]), "u": din("u", [16384, 1024]), "v": din("v", [16384, 1024]),
          "iota16": din("iota16", [128, 16])}
    dr["ident"] = dr["cst"][0]
    dr["y"] = nc.dram_tensor("y", [nt * 128, 1024], F32, kind="ExternalOutput").ap()
    dr["x1"] = dr["y"]
    dr["ysa"] = nc.dram_tensor("ysa", [nt, 128, 512], BF16).ap()
    dr["ysb"] = nc.dram_tensor("ysb", [nt, 64, 1024], BF16).ap()
    dr["uvb"] = nc.dram_tensor("uvb16", [16384, 2048], BF16).ap()
    P = Prog(nc)
    with ExitStack() as st:
        STACK[0] = st
        phase_a1(P, nc, dr, nseq)
        P.barrier()
        P.replay()
    with ExitStack() as st:
        STACK[0] = st
        phase_a2(P, nc, dr, nt)
        P.barrier()
        P.replay()
    with ExitStack() as st:
        STACK[0] = st
        toks = phase_b(P, nc, dr, nt, None)
        P.wait_tokens("sp", toks)
        P.barrier()
        P.replay()
    return nc


def host_inputs(inputs):
    f = lambda k: np.asarray(inputs[k], np.float32)
    cst, swab = consts()
    g1 = f("mix_norm_gain")[0]
    g2 = f("ffn_norm_gain")[0]
    sk = f("peer_sub_keys")[0]
    shared = {
        "w_in": np.ascontiguousarray(f("w_in")[0]), "g1col": col(g1, 8),
        "gainqk": bc(np.concatenate([np.tile(f("swa_q_gain")[0], 8), np.tile(f("swa_k_gain")[0], 2)])),
        "sinks_bc": bc(f("swa_sinks")[0]), "cst": cst, "swab": swab, "gbcol": col(f("gate_bias")[0], 16),
        "w_up_swa": np.ascontiguousarray(f("w_up_swa")[0]), "w_up_sb": np.ascontiguousarray(f("w_up_sb")[0]),
        "w_out": np.ascontiguousarray(f("w_out")[0]), "wq": np.ascontiguousarray(f("peer_w_q")[0]),
        "g2col": col(g2, 8), "g2bc": bc(g2),
        "skT": np.ascontiguousarray(sk.reshape(16, 128, 128).transpose(2, 0, 1)),
        "u": np.ascontiguousarray(f("peer_u")[0]), "v": np.ascontiguousarray(f("peer_v")[0]),
        "iota16": bc(np.arange(16, dtype=np.float32)),
    }
    return shared


def kernel(**inputs):
    x = np.asarray(inputs["x"], np.float32)
    B, S_, D_ = x.shape
    nseq = B // NCORES
    shared = host_inputs(inputs)
    nc = build_program(nseq)
    in_maps = []
    for c in range(NCORES):
        m = dict(shared)
        m["x"] = np.ascontiguousarray(x[c * nseq:(c + 1) * nseq].reshape(nseq * S_, D_))
        in_maps.append(m)
    res = run_bass_kernel_spmd(nc, in_maps, core_ids=list(range(NCORES)))
    out = np.concatenate([np.asarray(r["y"], np.float32).reshape(nseq, S_, D_) for r in res.results], axis=0)
    return out
```
